# Optimizing a Trainium2 kernel written in Bass

```python
import math
import jax, jax.numpy as jnp
from jax import lax
import numpy as np

D_MODEL = 1024
BATCH = 2
SEQ = 16384
DEPTH = 4

N_MIXERS = 2
N_CONV_LAYERS = (DEPTH + 1) // 2
N_ATTN_LAYERS = DEPTH // 2
N_HEADS = 16
HEAD_DIM = D_MODEL // N_HEADS
MOBA_BLOCK = 256
MOBA_TOP_K = 3
Q_CHUNK = 64
CONV_WIDTH = 31
D_FF = 4 * D_MODEL
N_MOD = 6
EPS = 1e-6

kernel_name = "hybrid_conv_moba_adaln_trunk"


def rms_norm(x, g):
    xf = x.astype(jnp.float32)
    y = xf * lax.rsqrt(jnp.mean(xf * xf, axis=-1, keepdims=True) + EPS)
    return (y * g.astype(jnp.float32)).astype(x.dtype)


def layer_norm(x, g, b):
    xf = x.astype(jnp.float32)
    mu = jnp.mean(xf, axis=-1, keepdims=True)
    xc = xf - mu
    var = jnp.mean(xc * xc, axis=-1, keepdims=True)
    y = xc * lax.rsqrt(var + EPS) * g.astype(jnp.float32) + b.astype(jnp.float32)
    return y.astype(x.dtype)


def alibi_slopes(n_heads):
    return jnp.asarray(2.0 ** (-8.0 * np.arange(1, n_heads + 1) / n_heads), dtype=jnp.float32)


def conformer_conv(h, w_in, b_in, dw, dw_b, ln_g, ln_b, w_out, b_out):
    d = h.shape[-1]
    a, g = jnp.split(h @ w_in + b_in, 2, axis=-1)
    u = a * jax.nn.sigmoid(g)
    u = lax.conv_general_dilated(
        u, dw[:, None, :].astype(u.dtype), window_strides=(1,),
        padding=((CONV_WIDTH - 1, 0),),
        dimension_numbers=('NWC', 'WIO', 'NWC'),
        feature_group_count=d) + dw_b
    u = jax.nn.silu(layer_norm(u, ln_g, ln_b))
    return u @ w_out + b_out


def moba_attention(h, w_qkv, q_norm_g, k_norm_g, w_o):
    B, S, D = h.shape
    H, DH = N_HEADS, HEAD_DIM
    q, k, v = jnp.split(h @ w_qkv, 3, axis=-1)
    to_heads = lambda z: z.reshape(B, S, H, DH).transpose(0, 2, 1, 3)
    q = rms_norm(to_heads(q), q_norm_g)
    k = rms_norm(to_heads(k), k_norm_g)
    v = to_heads(v)

    nb = -(-S // MOBA_BLOCK)
    s_pad = nb * MOBA_BLOCK
    pad = ((0, 0), (0, 0), (0, s_pad - S), (0, 0))
    q, k, v = jnp.pad(q, pad), jnp.pad(k, pad), jnp.pad(v, pad)
    k_blocks = k.reshape(B, H, nb, MOBA_BLOCK, DH)
    v_blocks = v.reshape(B, H, nb, MOBA_BLOCK, DH)
    k_mean = jnp.mean(k_blocks.astype(jnp.float32), axis=3)

    topk = min(MOBA_TOP_K, nb)
    slopes = alibi_slopes(H)
    scale = 1.0 / math.sqrt(DH)
    n_chunks = s_pad // Q_CHUNK
    q_chunks = q.reshape(B, H, n_chunks, Q_CHUNK, DH).transpose(2, 0, 1, 3, 4)
    bi = jnp.arange(B)[:, None, None, None]
    hi = jnp.arange(H)[None, :, None, None]
    blk_ids = jnp.arange(nb)
    in_block = jnp.arange(MOBA_BLOCK)

    def chunk_fn(args):
        qc, ci = args
        t = ci * Q_CHUNK + jnp.arange(Q_CHUNK)
        blk = (ci * Q_CHUNK) // MOBA_BLOCK
        gate = jnp.einsum('bhqd,bhnd->bhqn', qc.astype(jnp.float32), k_mean)
        gate = jnp.where(blk_ids < blk, gate, -jnp.inf)
        gate_val, sel = lax.top_k(gate, topk)
        sel_ok = jnp.isfinite(gate_val)
        k_sel = k_blocks[bi, hi, sel]
        v_sel = v_blocks[bi, hi, sel]
        s_sel = jnp.einsum('bhqd,bhqcsd->bhqcs', qc, k_sel).astype(jnp.float32) * scale
        pos_sel = sel[..., None] * MOBA_BLOCK + in_block
        dist_sel = t[None, None, :, None, None] - pos_sel
        s_sel = s_sel - slopes[None, :, None, None, None] * dist_sel
        s_sel = jnp.where(sel_ok[..., None], s_sel, -jnp.inf)
        k_own = lax.dynamic_slice_in_dim(k, blk * MOBA_BLOCK, MOBA_BLOCK, axis=2)
        v_own = lax.dynamic_slice_in_dim(v, blk * MOBA_BLOCK, MOBA_BLOCK, axis=2)
        s_own = jnp.einsum('bhqd,bhsd->bhqs', qc, k_own).astype(jnp.float32) * scale
        dist_own = t[:, None] - (blk * MOBA_BLOCK + in_block)[None, :]
        s_own = jnp.where(dist_own >= 0, s_own - slopes[None, :, None, None] * dist_own, -jnp.inf)
        scores = jnp.concatenate([s_sel.reshape(B, H, Q_CHUNK, topk * MOBA_BLOCK), s_own], axis=-1)
        p = jax.nn.softmax(scores, axis=-1)
        p_sel = p[..., :topk * MOBA_BLOCK].reshape(B, H, Q_CHUNK, topk, MOBA_BLOCK).astype(v.dtype)
        p_own = p[..., topk * MOBA_BLOCK:].astype(v.dtype)
        return (jnp.einsum('bhqcs,bhqcsd->bhqd', p_sel, v_sel)
                + jnp.einsum('bhqs,bhsd->bhqd', p_own, v_own))

    out = lax.map(chunk_fn, (q_chunks, jnp.arange(n_chunks)))
    out = out.transpose(1, 2, 0, 3, 4).reshape(B, H, s_pad, DH)[:, :, :S]
    out = out.transpose(0, 2, 1, 3).reshape(B, S, D)
    return out @ w_o


def setup_inputs(seed: int = 0) -> dict:
    key = jax.random.key(seed)
    ks = iter(jax.random.split(key, 32))
    nrm = lambda shape, s: jax.random.normal(next(ks), shape, jnp.float32) * s
    D = D_MODEL
    return {
        "x": nrm((BATCH, SEQ, D), 1.0),
        "c": nrm((BATCH, D), 1.0),
        "ada_w": nrm((DEPTH, D, N_MOD * D), 0.5 * D ** -0.5),
        "ada_b": nrm((DEPTH, N_MOD * D), 0.02),
        "mix_norm_g": 1.0 + nrm((DEPTH, D), 0.1),
        "mlp_norm_g": 1.0 + nrm((DEPTH, D), 0.1),
        "conv_w_in": nrm((N_CONV_LAYERS, D, 2 * D), D ** -0.5),
        "conv_b_in": nrm((N_CONV_LAYERS, 2 * D), 0.02),
        "conv_dw": nrm((N_CONV_LAYERS, CONV_WIDTH, D), CONV_WIDTH ** -0.5),
        "conv_dw_b": nrm((N_CONV_LAYERS, D), 0.02),
        "conv_ln_g": 1.0 + nrm((N_CONV_LAYERS, D), 0.1),
        "conv_ln_b": nrm((N_CONV_LAYERS, D), 0.02),
        "conv_w_out": nrm((N_CONV_LAYERS, D, D), D ** -0.5),
        "conv_b_out": nrm((N_CONV_LAYERS, D), 0.02),
        "attn_w_qkv": nrm((N_ATTN_LAYERS, D, 3 * D), D ** -0.5),
        "attn_q_norm_g": 1.0 + nrm((N_ATTN_LAYERS, HEAD_DIM), 0.1),
        "attn_k_norm_g": 1.0 + nrm((N_ATTN_LAYERS, HEAD_DIM), 0.1),
        "attn_w_o": nrm((N_ATTN_LAYERS, D, D), D ** -0.5),
        "mlp_w1": nrm((DEPTH, D, D_FF), D ** -0.5),
        "mlp_w2": nrm((DEPTH, D_FF, D), D_FF ** -0.5),
    }


def reference(x, c, ada_w, ada_b, mix_norm_g, mlp_norm_g,
              conv_w_in, conv_b_in, conv_dw, conv_dw_b, conv_ln_g, conv_ln_b, conv_w_out, conv_b_out,
              attn_w_qkv, attn_q_norm_g, attn_k_norm_g, attn_w_o,
              mlp_w1, mlp_w2):
    cond = jax.nn.silu(c)
    for i in range(DEPTH):
        mod = cond @ ada_w[i] + ada_b[i]
        sh1, sc1, g1, sh2, sc2, g2 = [m[:, None, :] for m in jnp.split(mod, N_MOD, axis=-1)]
        h = rms_norm(x, mix_norm_g[i]) * (1.0 + sc1) + sh1
        j = i // N_MIXERS
        if i % N_MIXERS == 0:
            y = conformer_conv(h, conv_w_in[j], conv_b_in[j], conv_dw[j], conv_dw_b[j],
                               conv_ln_g[j], conv_ln_b[j], conv_w_out[j], conv_b_out[j])
        else:
            y = moba_attention(h, attn_w_qkv[j], attn_q_norm_g[j], attn_k_norm_g[j], attn_w_o[j])
        x = x + g1 * y
        h = rms_norm(x, mlp_norm_g[i]) * (1.0 + sc2) + sh2
        x = x + g2 * (jnp.square(jax.nn.relu(h @ mlp_w1[i])) @ mlp_w2[i])
    return x
```

```python
import numpy as np
import concourse.bass as bass
import concourse.mybir as mybir
from concourse.bass_utils import run_bass_kernel_spmd

F32 = mybir.dt.float32
BF16 = mybir.dt.bfloat16
ALU = mybir.AluOpType
AF = mybir.ActivationFunctionType
AX = mybir.AxisListType

EPOCH = 20000
ENGS = ("pe", "act", "dve", "pool", "sp")

D = 1024
NCH = 8
S = 16384
NB = 2
TOK = 4096
T = 512
NT = TOK // T
CW = 31
HALO = CW - 1
EPS = 1e-6
NEG = -30000.0


class Buf:
    __slots__ = ("name", "w", "r", "dcount")

    def __init__(self, name):
        self.name = name
        self.w = None
        self.r = []
        self.dcount = 0


class Op:
    __slots__ = ("eng", "fn", "reads", "writes", "idx", "waits", "sig", "key", "ev")

    def __init__(self, eng, fn, reads, writes, key=None):
        self.eng = eng
        self.fn = fn
        self.reads = reads
        self.writes = writes
        self.key = key
        self.waits = []
        self.sig = False
        self.ev = None


class Prog:
    def __init__(self, nc):
        self.nc = nc
        self.ops = []
        self.sems = {}

    def op(self, eng, fn, reads=(), writes=()):
        o = Op(eng, fn, tuple(reads), tuple(writes))
        self.ops.append(o)
        return o

    def dma(self, q, fn, reads=(), writes=(), key=None):
        o = Op(q, fn, tuple(reads), tuple(writes), key=key)
        self.ops.append(o)
        return o

    def _sem(self, name):
        if name not in self.sems:
            self.sems[name] = self.nc.alloc_semaphore(name=name)
        return self.sems[name]

    def finalize(self, final_eng="sp"):
        nc = self.nc
        for i, o in enumerate(self.ops):
            o.idx = i
        deps_of = []
        for o in self.ops:
            deps = set()
            for b in o.reads:
                if b.w is not None:
                    deps.add(b.w)
            for b in o.writes:
                if b.w is not None:
                    deps.add(b.w)
                for r in b.r:
                    deps.add(r)
            deps.discard(o)
            for b in o.reads:
                b.r.append(o)
            for b in o.writes:
                b.w = o
                b.r = []
            best = {}
            dma_deps = []
            for d in deps:
                if d.key is not None:
                    dma_deps.append(d)
                else:
                    if d.eng == "pe" and o.eng == "pe":
                        continue
                    if d.eng not in best or best[d.eng].idx < d.idx:
                        best[d.eng] = d
            deps_of.append((list(best.values()), dma_deps))
            for d in best.values():
                d.sig = True
        cnt = {e: 0 for e in ENGS}
        dma_total = {}
        for o in self.ops:
            if o.key is not None:
                b = o.key
                b.dcount += 16
                o.ev = ("dma_" + b.name, b.dcount)
                dma_total[o.ev[0]] = b.dcount
            elif o.sig:
                c = cnt[o.eng]
                cnt[o.eng] = c + 1
                o.ev = ("%s_%d" % (o.eng, c // EPOCH), c % EPOCH + 1)
        running = {}
        known = {e: {} for e in ENGS}
        for o, (cdeps, ddeps) in zip(self.ops, deps_of):
            w = {}
            for d in cdeps:
                s, v = d.ev
                if w.get(s, 0) < v:
                    w[s] = v
            for d in ddeps:
                s = d.ev[0]
                v = running[s]
                if w.get(s, 0) < v:
                    w[s] = v
            kn = known[o.eng]
            for s, v in w.items():
                if kn.get(s, 0) < v:
                    kn[s] = v
                    o.waits.append((s, v))
            if o.key is not None:
                running[o.ev[0]] = o.ev[1]
        for o in self.ops:
            for s, v in o.waits:
                self._sem(s)
            if o.ev is not None:
                self._sem(o.ev[0])
        per_eng = {e: [o for o in self.ops if o.eng == e] for e in ENGS}
        sems = self.sems
        finals = sorted(dma_total.items())

        def emit(engobj, ename):
            for o in per_eng[ename]:
                for s, v in o.waits:
                    engobj.wait_ge(sems[s], v)
                ins = o.fn(engobj)
                if o.ev is not None:
                    ins.then_inc(sems[o.ev[0]], 16 if o.key is not None else 1)
            if ename == final_eng:
                for s, v in finals:
                    engobj.wait_ge(sems[s], v)

        with nc.Block() as block:
            @block.tensor
            def _(e):
                emit(e, "pe")

            @block.scalar
            def _(e):
                emit(e, "act")

            @block.vector
            def _(e):
                emit(e, "dve")

            @block.gpsimd
            def _(e):
                emit(e, "pool")

            @block.sync
            def _(e):
                emit(e, "sp")
        return len(self.ops)


class KB:
    def __init__(self):
        self.nc = bass.Bass("TRN2", target_bir_lowering=False)
        self.P = Prog(self.nc)
        self._u = 0

    def din(self, name, shape, dt=F32):
        return self.nc.dram_tensor(name, list(shape), dt, kind="ExternalInput").ap()

    def dout(self, name, shape, dt=F32):
        return self.nc.dram_tensor(name, list(shape), dt, kind="ExternalOutput").ap()

    def sb(self, name, shape, dt=F32):
        return self.nc.alloc_sbuf_tensor(name, list(shape), dt)

    def bank(self, name):
        return self.nc.alloc_psum_tensor(name, [128, 512], F32)

    def bufs(self, name, n):
        return [Buf("%s%d" % (name, i)) for i in range(n)]

    def buf(self, name):
        return Buf(name)


def emit_consts(k):
    P = k.P
    c = {}
    c["ones"] = k.sb("c_ones", [128, 128], BF16)
    c["eps"] = k.sb("c_eps", [128, 1], F32)
    c["Bones"] = k.buf("c_ones")
    c["Beps"] = k.buf("c_eps")
    P.op("pool", lambda e: e.memset(c["ones"][:, :], 1.0), writes=[c["Bones"]])
    P.op("pool", lambda e: e.memset(c["eps"][:, :], EPS), writes=[c["Beps"]])
    return c


def emit_mod(k, cb, adaT, adab, nv, R, BR):
    P = k.P
    n = nv * 8
    Rf = R[:, :, :].rearrange("p c t -> p (c t)")
    wch = [Rf[:, 0:D], Rf[:, D:2 * D]]
    Bw = [[BR[0], BR[1]], [BR[2], BR[3]]]
    junk = Rf[:, 2 * D:3 * D]
    Bj = [BR[4], BR[5]]
    scb = Rf[:, 3 * D:4 * D]
    Bscb = [BR[6], BR[7]]
    mod = k.sb("m_mod", [128, n], F32)
    Bmod = k.buf("m_mod")
    ab = k.sb("m_ab", [128, n], F32)
    Bab = k.buf("m_ab")
    P.dma("sp", lambda e: e.dma_start(out=scb, in_=cb[:, :]), writes=Bscb, key=BR[6])
    P.dma("sp", lambda e: e.dma_start(out=ab[:, :], in_=adab[:, :]), writes=[Bab], key=Bab)
    P.op("act", lambda e: e.activation(out=junk, in_=scb, func=AF.Sigmoid), reads=Bscb, writes=Bj)
    P.op("dve", lambda e: e.tensor_tensor(out=scb, in0=scb, in1=junk, op=ALU.mult),
         reads=Bscb + Bj, writes=Bscb)
    for j in range(n):
        s = j % 2
        P.dma("sp", lambda e, s=s, j=j: e.dma_start(out=wch[s], in_=adaT[j, :, :]), writes=Bw[s], key=Bw[s][0])
        P.op("dve", lambda e, s=s: e.tensor_tensor(out=junk, in0=wch[s], in1=scb, op=ALU.mult),
             reads=Bw[s] + Bscb, writes=Bj)
        P.op("dve", lambda e, j=j: e.tensor_reduce(out=mod[:, j:j + 1], in_=junk, axis=AX.X, op=ALU.add),
             reads=Bj, writes=[Bmod])
    P.op("dve", lambda e: e.tensor_tensor(out=mod[:, :], in0=mod[:, :], in1=ab[:, :], op=ALU.add),
         reads=[Bmod, Bab], writes=[Bmod])
    return mod, Bmod


def emit_scale(k, name, mod, Bmod, sc_lo, g_ap, Bg):
    P = k.P
    sc = k.sb(name, [128, NCH], F32)
    Bsc = k.buf(name)
    P.op("dve", lambda e: e.tensor_scalar(out=sc[:, :], in0=mod[:, sc_lo:sc_lo + NCH], scalar1=1.0, scalar2=None,
                                          op0=ALU.add), reads=[Bmod], writes=[Bsc])
    P.op("dve", lambda e: e.tensor_tensor(out=sc[:, :], in0=sc[:, :], in1=g_ap, op=ALU.mult),
         reads=[Bsc, Bg], writes=[Bsc])
    return sc, Bsc


class NormCtx:
    def __init__(self, k, consts, width=T):
        self.k = k
        self.c = consts
        self.sq = k.sb("n_sq", [128, NCH, width], BF16)
        self.Bsq = k.buf("n_sq")
        self.st = k.bank("n_st")
        self.Bst = k.buf("n_st")
        self.sd = k.sb("n_sd", [128, width], F32)
        self.Bsd = k.buf("n_sd")
        self.rstd = self.sd
        self.Brstd = self.Bsd
        self.tmp = [k.sb("n_tmp%d" % i, [128, width], F32) for i in range(2)]
        self.Btmp = k.bufs("n_tmp", 2)


def emit_norm(n, xt, Bx, w, scale, Bscale, shift_ap_fn, Bshift, hT, BhT):
    k = n.k
    P = k.P
    c = n.c
    P.op("act", lambda e: e.activation(out=n.sq[:, :, 0:w], in_=xt[:, :, 0:w], func=AF.Square), reads=list(Bx), writes=[n.Bsq])
    for ch in range(NCH):
        P.op("pe", lambda e, ch=ch: e.matmul(n.st[:, 0:w], lhsT=c["ones"][:, :], rhs=n.sq[:, ch, 0:w],
                                              start=(ch == 0), stop=(ch == NCH - 1)),
             reads=[n.Bsq, c["Bones"]], writes=[n.Bst])
    P.op("act", lambda e: e.activation(out=n.sd[:, 0:w], in_=n.st[:, 0:w], func=AF.Sqrt, bias=c["eps"][:, 0:1], scale=1.0 / D),
         reads=[n.Bst, c["Beps"]], writes=[n.Bsd])
    P.op("dve", lambda e: e.reciprocal(out=n.rstd[:, 0:w], in_=n.sd[:, 0:w]), reads=[n.Bsd], writes=[n.Brstd])
    for ch in range(NCH):
        s = ch % 2
        P.op("dve", lambda e, ch=ch, s=s: e.scalar_tensor_tensor(out=n.tmp[s][:, 0:w], in0=xt[:, ch, 0:w],
                                                                  scalar=scale[:, ch:ch + 1], in1=n.rstd[:, 0:w],
                                                                  op0=ALU.mult, op1=ALU.mult),
             reads=[Bx[ch], Bscale, n.Brstd], writes=[n.Btmp[s]])
        P.op("act", lambda e, ch=ch, s=s: e.activation(out=hT[:, ch, 0:w], in_=n.tmp[s][:, 0:w], func=AF.Identity,
                                                        bias=shift_ap_fn(ch), scale=1.0),
             reads=[n.Btmp[s], Bshift], writes=[BhT[ch]])


def build_mlp():
    k = KB()
    nc, P = k.nc, k.P
    xT = k.din("xT", [D, TOK])
    xo = k.dout("xo", [D, TOK])
    cb = k.din("cb", [128, D])
    adaT = k.din("adaT", [24, 128, D])
    adab = k.din("adab", [128, 24])
    ng = k.din("ng", [128, NCH])
    w1 = k.din("w1", [8, 128, 4096])
    w2 = k.din("w2", [8, 128, 4096])
    xv = xT.rearrange("(c p) t -> p c t", p=128)
    xov = xo.rearrange("(c p) t -> p c t", p=128)

    C = emit_consts(k)
    W1 = k.sb("W1", [128, 8, 4096], BF16)
    W2 = k.sb("W2", [128, 8, 4096], BF16)
    BW1 = k.bufs("W1_", 8)
    BW2 = k.bufs("W2_", 8)
    xt = [k.sb("xt%d" % i, [128, NCH, T], F32) for i in range(2)]
    Bxt = [k.bufs("xt%d_" % i, NCH) for i in range(2)]
    mod, Bmod = emit_mod(k, cb, adaT, adab, 3, xt[1], Bxt[1])
    ngt = k.sb("ngt", [128, NCH], F32)
    Bng = k.buf("ngt")
    P.dma("sp", lambda e: e.dma_start(out=ngt[:, :], in_=ng[:, :]), writes=[Bng], key=Bng)
    scale2, Bsc2 = emit_scale(k, "scale2", mod, Bmod, 8, ngt[:, :], Bng)
    for t in range(8):
        P.dma("pool", lambda e, t=t: e.dma_start(out=W1[:, t, :], in_=w1[t, :, :]), writes=[BW1[t]], key=BW1[t])
    for t in range(8):
        P.dma("pool", lambda e, t=t: e.dma_start(out=W2[:, t, :], in_=w2[t, :, :]), writes=[BW2[t]], key=BW2[t])

    N = NormCtx(k, C)
    hT = k.sb("hT", [128, NCH, T], BF16)
    BhT = k.bufs("hT_", NCH)
    hid = k.sb("hid", [128, 16, T], BF16)
    Bhid = k.bufs("hid_", 16)
    r = [k.sb("r%d" % i, [128, T], F32) for i in range(2)]
    Br = k.bufs("r_", 2)
    ph = [k.bank("ph%d" % i) for i in range(3)]
    Bph = k.bufs("ph_", 3)
    po = [k.bank("po%d" % i) for i in range(2)]
    Bpo = k.bufs("po_", 2)

    hcount = 0
    ocount = 0
    for i in range(NT):
        s = i % 2
        x = xt[s]
        Bx = Bxt[s]
        P.dma("sp", lambda e, x=x, i=i: e.dma_start(out=x[:, :, :], in_=xv[:, :, i * T:(i + 1) * T]), writes=Bx, key=Bx[0])
        emit_norm(N, x, Bx, T, scale2, Bsc2, lambda ch: mod[:, ch:ch + 1], Bmod, hT, BhT)
        for half in range(2):
            for jc in range(16):
                j = half * 16 + jc
                wt, m = j // 4, j % 4
                pb = hcount % 3
                rb = hcount % 2
                hcount += 1
                for kc in range(NCH):
                    P.op("pe", lambda e, wt=wt, m=m, kc=kc, pb=pb: e.matmul(
                        ph[pb][:, :], lhsT=W1[:, wt, kc * 512 + m * 128: kc * 512 + (m + 1) * 128], rhs=hT[:, kc, :],
                        start=(kc == 0), stop=(kc == NCH - 1)),
                         reads=[BW1[wt], BhT[kc]], writes=[Bph[pb]])
                P.op("act", lambda e, pb=pb, rb=rb: e.activation(out=r[rb][:, :], in_=ph[pb][:, :], func=AF.Relu),
                     reads=[Bph[pb]], writes=[Br[rb]])
                P.op("dve", lambda e, rb=rb, jc=jc: e.tensor_tensor(out=hid[:, jc, :], in0=r[rb][:, :], in1=r[rb][:, :], op=ALU.mult),
                     reads=[Br[rb]], writes=[Bhid[jc]])
            for c in range(NCH):
                ob = ocount % 2
                ocount += 1
                for kc in range(16):
                    kg = half * 16 + kc
                    P.op("pe", lambda e, c=c, kc=kc, kg=kg, ob=ob: e.matmul(
                        po[ob][:, :], lhsT=W2[:, c, kg * 128:(kg + 1) * 128], rhs=hid[:, kc, :],
                        start=(kc == 0), stop=(kc == 15)),
                         reads=[BW2[c], Bhid[kc]], writes=[Bpo[ob]])
                P.op("dve", lambda e, c=c, ob=ob, x=x: e.scalar_tensor_tensor(
                    out=x[:, c, :], in0=po[ob][:, :], scalar=mod[:, 16 + c:17 + c], in1=x[:, c, :],
                    op0=ALU.mult, op1=ALU.add),
                     reads=[Bpo[ob], Bmod, Bx[c]], writes=[Bx[c]])
        P.dma("sp", lambda e, x=x, i=i: e.dma_start(out=xov[:, :, i * T:(i + 1) * T], in_=x[:, :, :]), reads=Bx, key=Bx[1])
    n = P.finalize()
    return nc, n


def tileA(W):
    K, N = W.shape
    assert K == 1024
    return np.ascontiguousarray(W.reshape(8, 128, N // 512, 512).transpose(2, 1, 0, 3).reshape(N // 512, 128, 4096))


def tileB(W2):
    return np.ascontiguousarray(W2.reshape(32, 128, 8, 128).transpose(2, 1, 0, 3).reshape(8, 128, 4096))


def pvec(v):
    return np.ascontiguousarray(v.reshape(-1, 128).T)


def ada_rows(ada_w_i, vec_ids):
    WT = ada_w_i.T
    rows = np.concatenate([WT[v * D:(v + 1) * D] for v in vec_ids])
    return np.ascontiguousarray(rows.reshape(-1, 128, D))


def ada_cols(ada_b_i, vec_ids):
    return np.ascontiguousarray(np.concatenate([pvec(ada_b_i[v * D:(v + 1) * D]) for v in vec_ids], axis=1))


def core_tokens(core):
    b, q = core // 4, core % 4
    return b, q * TOK


_cache = {}


def get_prog(name, builder):
    if name not in _cache:
        _cache[name] = builder()[0]
    return _cache[name]


def run_mlp(xTs, c, ada_w_i, ada_b_i, ng_i, w1_i, w2_i):
    nc = get_prog("mlp", build_mlp)
    aT = ada_rows(ada_w_i, [3, 4, 5])
    ab = ada_cols(ada_b_i, [3, 4, 5])
    w1t = tileA(w1_i)
    w2t = tileB(w2_i)
    ngp = pvec(ng_i)
    in_maps = []
    for core in range(8):
        b, _ = core_tokens(core)
        in_maps.append({"xT": xTs[core], "cb": np.ascontiguousarray(np.broadcast_to(c[b], (128, D))),
                        "adaT": aT, "adab": ab, "ng": ngp, "w1": w1t, "w2": w2t})
    res = run_bass_kernel_spmd(nc, in_maps, core_ids=list(range(8)))
    return [r["xo"] for r in res.results]


NVC = 56 + NCH * CW


def build_conv():
    k = KB()
    nc, P = k.nc, k.P
    xT = k.din("xT", [D, TOK])
    xh = k.din("xh", [D, HALO])
    hv = k.din("hv", [128, 1])
    xo = k.dout("xo", [D, TOK])
    cb = k.din("cb", [128, D])
    adaT = k.din("adaT", [24, 128, D])
    adab = k.din("adab", [128, 24])
    vec = k.din("vec", [128, NVC])
    identd = k.din("ident", [128, 128])
    win = k.din("win", [4, 128, 4096])
    wout = k.din("wout", [2, 128, 4096])
    xv = xT.rearrange("(c p) t -> p c t", p=128)
    xhv = xh.rearrange("(c p) t -> p c t", p=128)
    xov = xo.rearrange("(c p) t -> p c t", p=128)

    C = emit_consts(k)
    ident = k.sb("ident_sb", [128, 128], BF16)
    Bid = k.buf("ident")
    P.dma("pool", lambda e: e.dma_start(out=ident[:, :], in_=identd[:, :]), writes=[Bid], key=Bid)
    vt = k.sb("vt", [128, NVC], F32)
    Bvt = k.buf("vt")
    P.dma("sp", lambda e: e.dma_start(out=vt[:, :], in_=vec[:, :]), writes=[Bvt], key=Bvt)
    hvt = k.sb("hvt", [128, 1], F32)
    Bhv = k.buf("hvt")
    P.dma("sp", lambda e: e.dma_start(out=hvt[:, :], in_=hv[:, :]), writes=[Bhv], key=Bhv)
    xt = [k.sb("xt%d" % i, [128, NCH, T], F32) for i in range(2)]
    Bxt = [k.bufs("xt%d_" % i, NCH) for i in range(2)]
    mod, Bmod = emit_mod(k, cb, adaT, adab, 3, xt[1], Bxt[1])
    scale1, Bsc1 = emit_scale(k, "scale1", mod, Bmod, 8, vt[:, 0:8], Bvt)
    bg = k.sb("bg", [128, NCH], F32)
    Bbg = k.buf("bg")
    P.op("dve", lambda e: e.tensor_tensor(out=bg[:, :], in0=vt[:, 48:56], in1=mod[:, 16:24], op=ALU.mult),
         reads=[Bvt, Bmod], writes=[Bbg])
    Win = k.sb("Win", [128, 4, 4096], BF16)
    Wout = k.sb("Wout", [128, 2, 4096], BF16)
    BWin = k.bufs("Win_", 4)
    BWout = k.bufs("Wout_", 2)
    for t in range(4):
        P.dma("pool", lambda e, t=t: e.dma_start(out=Win[:, t, :], in_=win[t, :, :]), writes=[BWin[t]], key=BWin[t])
    for t in range(2):
        P.dma("pool", lambda e, t=t: e.dma_start(out=Wout[:, t, :], in_=wout[t, :, :]), writes=[BWout[t]], key=BWout[t])

    N = NormCtx(k, C)
    hT = k.sb("hT", [128, NCH, T], BF16)
    BhT = k.bufs("hT_", NCH)
    sg = N.sq
    Bsg = N.Bsq
    ub = [k.sb("ub%d" % i, [128, NCH, T + HALO], BF16) for i in range(2)]
    Bu = [k.bufs("ub%d_" % i, NCH) for i in range(2)]
    xht = k.sb("xht", [128, NCH, HALO], F32)
    Bxh = k.bufs("xht_", NCH)
    dg = [k.sb("dg%d" % i, [128, CW, 128], BF16) for i in range(2)]
    Bdg = k.bufs("dg_", 2)
    v = k.sb("v", [128, NCH, T], F32)
    Bv = k.bufs("v_", NCH)
    vb = [k.sb("vb%d" % i, [128, T], BF16) for i in range(2)]
    Bvb = k.bufs("vb_", 2)
    vsq = [k.sb("vsq%d" % i, [128, T], BF16) for i in range(2)]
    Bvsq = k.bufs("vsq_", 2)
    mean = k.sb("mean", [128, T], F32)
    Bmean = k.buf("mean")
    var = k.sb("var", [128, T], F32)
    Bvar = k.buf("var")
    t1 = [k.sb("t1_%d" % i, [128, T], F32) for i in range(2)]
    Bt1 = k.bufs("t1_", 2)
    pw = [k.bank("pw%d" % i) for i in range(2)]
    Bpw = k.bufs("pw_", 2)
    pc = [k.bank("pc%d" % i) for i in range(2)]
    Bpc = k.bufs("pc_", 2)
    pm = k.bank("pm")
    Bpm = k.buf("pm")
    pq = k.bank("pq")
    Bpq = k.buf("pq")
    cnt = {"w": 0, "c": 0}

    def front(x, Bx, w, slot, off):
        emit_norm(N, x, Bx, w, scale1, Bsc1, lambda ch: mod[:, ch:ch + 1], Bmod, hT, BhT)
        for c in range(NCH):
            for part in (1, 0):
                j = part * 8 + c
                wt, m = j // 4, j % 4
                pb = cnt["w"] % 2
                cnt["w"] += 1
                for kc in range(NCH):
                    P.op("pe", lambda e, wt=wt, m=m, kc=kc, pb=pb: e.matmul(
                        pw[pb][:, 0:w], lhsT=Win[:, wt, kc * 512 + m * 128: kc * 512 + (m + 1) * 128], rhs=hT[:, kc, 0:w],
                        start=(kc == 0), stop=(kc == NCH - 1)),
                         reads=[BWin[wt], BhT[kc]], writes=[Bpw[pb]])
                if part == 1:
                    P.op("act", lambda e, c=c, pb=pb: e.activation(out=sg[:, c, 0:w], in_=pw[pb][:, 0:w], func=AF.Sigmoid,
                                                                   bias=vt[:, 16 + c:17 + c], scale=1.0),
                         reads=[Bpw[pb], Bvt], writes=[Bsg])
                else:
                    P.op("dve", lambda e, c=c, pb=pb: e.scalar_tensor_tensor(
                        out=ub[slot][:, c, off:off + w], in0=pw[pb][:, 0:w], scalar=vt[:, 8 + c:9 + c], in1=sg[:, c, 0:w],
                        op0=ALU.add, op1=ALU.mult),
                         reads=[Bpw[pb], Bvt, Bsg], writes=[Bu[slot][c]])

    P.dma("sp", lambda e: e.dma_start(out=xht[:, :, :], in_=xhv[:, :, :]), writes=Bxh, key=Bxh[0])
    front(xht, Bxh, HALO, 0, 0)
    P.op("dve", lambda e: e.tensor_scalar(out=ub[0][:, :, 0:HALO], in0=ub[0][:, :, 0:HALO], scalar1=hvt[:, 0:1], scalar2=None,
                                          op0=ALU.mult), reads=Bu[0] + [Bhv], writes=Bu[0])

    for i in range(NT):
        s = i % 2
        x = xt[s]
        Bx = Bxt[s]
        P.dma("sp", lambda e, x=x, i=i: e.dma_start(out=x[:, :, :], in_=xv[:, :, i * T:(i + 1) * T]), writes=Bx, key=Bx[0])
        front(x, Bx, T, s, HALO)
        P.op("dve", lambda e, s=s: e.tensor_copy(out=ub[1 - s][:, :, 0:HALO], in_=ub[s][:, :, T:T + HALO]),
             reads=Bu[s], writes=Bu[1 - s])
        for c in range(NCH):
            ds = cnt["c"] % 2
            cb_ = cnt["c"] % 2
            cnt["c"] += 1
            P.op("pool", lambda e, c=c, ds=ds: e.tensor_tensor(
                out=dg[ds][:, :, :], in0=ident[:, :].unsqueeze(1).broadcast_to([128, CW, 128]),
                in1=vt[:, 56 + c * CW:56 + (c + 1) * CW].unsqueeze(2).broadcast_to([128, CW, 128]), op=ALU.mult),
                 reads=[Bid, Bvt], writes=[Bdg[ds]])
            for kk in range(CW):
                P.op("pe", lambda e, c=c, kk=kk, ds=ds, cb_=cb_, s=s: e.matmul(
                    pc[cb_][:, :], lhsT=dg[ds][:, kk, :], rhs=ub[s][:, c, kk:kk + T], start=(kk == 0), stop=(kk == CW - 1)),
                     reads=[Bdg[ds], Bu[s][c]], writes=[Bpc[cb_]])
            P.op("act", lambda e, c=c, cb_=cb_: e.activation(out=v[:, c, :], in_=pc[cb_][:, :], func=AF.Identity,
                                                             bias=vt[:, 24 + c:25 + c], scale=1.0),
                 reads=[Bpc[cb_], Bvt], writes=[Bv[c]])
            P.op("act", lambda e, c=c, cb_=cb_, ds=ds: e.activation(out=vsq[ds][:, :], in_=pc[cb_][:, :], func=AF.Square,
                                                                    bias=vt[:, 24 + c:25 + c], scale=1.0),
                 reads=[Bpc[cb_], Bvt], writes=[Bvsq[ds]])
            P.op("dve", lambda e, c=c, ds=ds: e.tensor_copy(out=vb[ds][:, :], in_=v[:, c, :]), reads=[Bv[c]], writes=[Bvb[ds]])
            P.op("pe", lambda e, c=c, ds=ds: e.matmul(pm[:, :], lhsT=C["ones"][:, :], rhs=vb[ds][:, :], start=(c == 0), stop=(c == NCH - 1)),
                 reads=[Bvb[ds], C["Bones"]], writes=[Bpm])
            P.op("pe", lambda e, c=c, ds=ds: e.matmul(pq[:, :], lhsT=C["ones"][:, :], rhs=vsq[ds][:, :], start=(c == 0), stop=(c == NCH - 1)),
                 reads=[Bvsq[ds], C["Bones"]], writes=[Bpq])
        P.op("act", lambda e: e.activation(out=mean[:, :], in_=pm[:, :], func=AF.Identity, scale=1.0 / D), reads=[Bpm], writes=[Bmean])
        P.op("dve", lambda e: e.tensor_tensor(out=var[:, :], in0=mean[:, :], in1=mean[:, :], op=ALU.mult), reads=[Bmean], writes=[Bvar])
        P.op("dve", lambda e: e.scalar_tensor_tensor(out=var[:, :], in0=pq[:, :], scalar=1.0 / D, in1=var[:, :],
                                                     op0=ALU.mult, op1=ALU.subtract), reads=[Bpq, Bvar], writes=[Bvar])
        P.op("act", lambda e: e.activation(out=var[:, :], in_=var[:, :], func=AF.Sqrt, bias=C["eps"][:, 0:1], scale=1.0),
             reads=[Bvar, C["Beps"]], writes=[Bvar])
        P.op("dve", lambda e: e.reciprocal(out=var[:, :], in_=var[:, :]), reads=[Bvar], writes=[Bvar])
        for c in range(NCH):
            ts_ = c % 2
            P.op("dve", lambda e, c=c, ts_=ts_: e.tensor_tensor(out=t1[ts_][:, :], in0=v[:, c, :], in1=mean[:, :], op=ALU.subtract),
                 reads=[Bv[c], Bmean], writes=[Bt1[ts_]])
            P.op("dve", lambda e, ts_=ts_: e.tensor_tensor(out=t1[ts_][:, :], in0=t1[ts_][:, :], in1=var[:, :], op=ALU.mult),
                 reads=[Bt1[ts_], Bvar], writes=[Bt1[ts_]])
            P.op("act", lambda e, c=c, ts_=ts_: e.activation(out=hT[:, c, :], in_=t1[ts_][:, :], func=AF.Silu,
                                                             bias=vt[:, 40 + c:41 + c], scale=vt[:, 32 + c:33 + c]),
                 reads=[Bt1[ts_], Bvt], writes=[BhT[c]])
        for c in range(NCH):
            wt, m = c // 4, c % 4
            pb = cnt["w"] % 2
            cnt["w"] += 1
            for kc in range(NCH):
                P.op("pe", lambda e, wt=wt, m=m, kc=kc, pb=pb: e.matmul(
                    pw[pb][:, :], lhsT=Wout[:, wt, kc * 512 + m * 128: kc * 512 + (m + 1) * 128], rhs=hT[:, kc, :],
                    start=(kc == 0), stop=(kc == NCH - 1)),
                     reads=[BWout[wt], BhT[kc]], writes=[Bpw[pb]])
            P.op("dve", lambda e, c=c, pb=pb, x=x: e.scalar_tensor_tensor(
                out=x[:, c, :], in0=pw[pb][:, :], scalar=mod[:, 16 + c:17 + c], in1=x[:, c, :], op0=ALU.mult, op1=ALU.add),
                 reads=[Bpw[pb], Bmod, Bx[c]], writes=[Bx[c]])
            P.op("dve", lambda e, c=c, x=x: e.tensor_scalar(out=x[:, c, :], in0=x[:, c, :], scalar1=bg[:, c:c + 1], scalar2=None,
                                                            op0=ALU.add), reads=[Bx[c], Bbg], writes=[Bx[c]])
        P.dma("sp", lambda e, x=x, i=i: e.dma_start(out=xov[:, :, i * T:(i + 1) * T], in_=x[:, :, :]), reads=Bx, key=Bx[1])
    n = P.finalize()
    return nc, n


def conv_vec(z, j):
    cols = [pvec(z["mix_norm_g_i"]), pvec(z["conv_b_in"][j][:D]), pvec(z["conv_b_in"][j][D:]), pvec(z["conv_dw_b"][j]),
            pvec(z["conv_ln_g"][j]), pvec(z["conv_ln_b"][j]), pvec(z["conv_b_out"][j])]
    dw = z["conv_dw"][j]
    dwp = dw.reshape(CW, NCH, 128).transpose(2, 1, 0).reshape(128, NCH * CW)
    return np.ascontiguousarray(np.concatenate(cols + [dwp], axis=1).astype(np.float32))


def run_conv(xTs, halos, c, ada_w_i, ada_b_i, mix_g_i, z, j):
    nc = get_prog("conv", build_conv)
    aT = ada_rows(ada_w_i, [0, 1, 2])
    ab = ada_cols(ada_b_i, [0, 1, 2])
    zz = dict(z)
    zz["mix_norm_g_i"] = mix_g_i
    vec = conv_vec(zz, j)
    wint = tileA(z["conv_w_in"][j])
    woutt = tileA(z["conv_w_out"][j])
    ident = np.eye(128, dtype=np.float32)
    in_maps = []
    for core in range(8):
        b, t0 = core_tokens(core)
        in_maps.append({"xT": xTs[core], "xh": halos[core],
                        "hv": np.full((128, 1), 0.0 if t0 == 0 else 1.0, np.float32),
                        "cb": np.ascontiguousarray(np.broadcast_to(c[b], (128, D))),
                        "adaT": aT, "adab": ab, "vec": vec, "ident": ident, "win": wint, "wout": woutt})
    res = run_bass_kernel_spmd(nc, in_maps, core_ids=list(range(8)))
    return [r["xo"] for r in res.results]


def build_a1():
    k = KB()
    nc, P = k.nc, k.P
    xT = k.din("xT", [D, TOK])
    ho = k.dout("ho", [D, TOK], BF16)
    cb = k.din("cb", [128, D])
    adaT = k.din("adaT", [16, 128, D])
    adab = k.din("adab", [128, 16])
    ng = k.din("ng", [128, NCH])
    xv = xT.rearrange("(c p) t -> p c t", p=128)
    hov = ho.rearrange("(c p) t -> p c t", p=128)
    C = emit_consts(k)
    xt = [k.sb("xt%d" % i, [128, NCH, T], F32) for i in range(2)]
    Bxt = [k.bufs("xt%d_" % i, NCH) for i in range(2)]
    mod, Bmod = emit_mod(k, cb, adaT, adab, 2, xt[1], Bxt[1])
    ngt = k.sb("ngt", [128, NCH], F32)
    Bng = k.buf("ngt")
    P.dma("sp", lambda e: e.dma_start(out=ngt[:, :], in_=ng[:, :]), writes=[Bng], key=Bng)
    scale1, Bsc1 = emit_scale(k, "scale1", mod, Bmod, 8, ngt[:, :], Bng)
    N = NormCtx(k, C)
    hT = [k.sb("hT%d" % i, [128, NCH, T], BF16) for i in range(2)]
    BhT = [k.bufs("hT%d_" % i, NCH) for i in range(2)]
    for i in range(NT):
        s = i % 2
        x, Bx = xt[s], Bxt[s]
        P.dma("sp", lambda e, x=x, i=i: e.dma_start(out=x[:, :, :], in_=xv[:, :, i * T:(i + 1) * T]), writes=Bx, key=Bx[0])
        emit_norm(N, x, Bx, T, scale1, Bsc1, lambda ch: mod[:, ch:ch + 1], Bmod, hT[s], BhT[s])
        P.dma("sp", lambda e, s=s, i=i: e.dma_start(out=hov[:, :, i * T:(i + 1) * T], in_=hT[s][:, :, :]), reads=BhT[s], key=BhT[s][0])
    n = P.finalize()
    return nc, n


def build_a3():
    k = KB()
    nc, P = k.nc, k.P
    xT = k.din("xT", [D, TOK])
    aT = k.din("aT", [D, TOK], BF16)
    xo = k.dout("xo", [D, TOK])
    cb = k.din("cb", [128, D])
    adaT = k.din("adaT", [8, 128, D])
    adab = k.din("adab", [128, 8])
    wo = k.din("wo", [2, 128, 4096])
    xv = xT.rearrange("(c p) t -> p c t", p=128)
    av = aT.rearrange("(c p) t -> p c t", p=128)
    xov = xo.rearrange("(c p) t -> p c t", p=128)
    xt = [k.sb("xt%d" % i, [128, NCH, T], F32) for i in range(2)]
    Bxt = [k.bufs("xt%d_" % i, NCH) for i in range(2)]
    mod, Bmod = emit_mod(k, cb, adaT, adab, 1, xt[1], Bxt[1])
    Wo = k.sb("Wo", [128, 2, 4096], BF16)
    BWo = k.bufs("Wo_", 2)
    for t in range(2):
        P.dma("pool", lambda e, t=t: e.dma_start(out=Wo[:, t, :], in_=wo[t, :, :]), writes=[BWo[t]], key=BWo[t])
    at = [k.sb("at%d" % i, [128, NCH, T], BF16) for i in range(2)]
    Bat = [k.bufs("at%d_" % i, NCH) for i in range(2)]
    pw = [k.bank("pw%d" % i) for i in range(2)]
    Bpw = k.bufs("pw_", 2)
    cnt = 0
    for i in range(NT):
        s = i % 2
        x, Bx = xt[s], Bxt[s]
        P.dma("sp", lambda e, x=x, i=i: e.dma_start(out=x[:, :, :], in_=xv[:, :, i * T:(i + 1) * T]), writes=Bx, key=Bx[0])
        P.dma("sp", lambda e, s=s, i=i: e.dma_start(out=at[s][:, :, :], in_=av[:, :, i * T:(i + 1) * T]), writes=Bat[s], key=Bat[s][0])
        for c in range(NCH):
            wt, m = c // 4, c % 4
            pb = cnt % 2
            cnt += 1
            for kc in range(NCH):
                P.op("pe", lambda e, wt=wt, m=m, kc=kc, pb=pb, s=s: e.matmul(
                    pw[pb][:, :], lhsT=Wo[:, wt, kc * 512 + m * 128: kc * 512 + (m + 1) * 128], rhs=at[s][:, kc, :],
                    start=(kc == 0), stop=(kc == NCH - 1)),
                     reads=[BWo[wt], Bat[s][kc]], writes=[Bpw[pb]])
            P.op("dve", lambda e, c=c, pb=pb, x=x: e.scalar_tensor_tensor(
                out=x[:, c, :], in0=pw[pb][:, :], scalar=mod[:, c:c + 1], in1=x[:, c, :], op0=ALU.mult, op1=ALU.add),
                 reads=[Bpw[pb], Bmod, Bx[c]], writes=[Bx[c]])
        P.dma("sp", lambda e, x=x, i=i: e.dma_start(out=xov[:, :, i * T:(i + 1) * T], in_=x[:, :, :]), reads=Bx, key=Bx[1])
    n = P.finalize()
    return nc, n


NQS = S // T
NKT = S // 128
HG = 4


def build_a2():
    k = KB()
    nc, P = k.nc, k.P
    hTa = k.din("hTa", [D, S], BF16)
    wqk = k.din("wqk", [128, 4096])
    wv = k.din("wv", [128, NCH * 256])
    gqk = k.din("gqk", [128, 2])
    ABd = k.din("AB", [128, HG * 128])
    shcd = k.din("shc", [128, 16])
    kconst = k.din("kconst", [64, S], BF16)
    cmaskd = k.din("cmask", [4, 128, T])
    identd = k.din("ident", [128, 128])
    bdd = k.din("bdones", [128, 128])
    ao = k.dout("ao", [HG * 64, S], BF16)
    QA = nc.dram_tensor("QA", [HG, 128, S], BF16, kind="Internal").ap()
    KA = nc.dram_tensor("KA", [HG, 64, S], BF16, kind="Internal").ap()
    BQA = k.bufs("QAd", HG)
    BKA = k.bufs("KAd", HG)
    hv_ = hTa.rearrange("(c p) t -> p c t", p=128)

    C = emit_consts(k)
    ident = k.sb("ident_sb", [128, 128], BF16)
    Bid = k.buf("ident")
    bdo = k.sb("bdo", [128, 128], BF16)
    Bbd = k.buf("bdo")
    cmask = k.sb("cmask_sb", [128, 4, T], BF16)
    Bcm = k.buf("cmask")
    AB = k.sb("AB_sb", [128, HG * 128], F32)
    BAB = k.buf("AB")
    shc = k.sb("shc_sb", [128, 16], F32)
    Bshc = k.buf("shc")
    gq = k.sb("gqk_sb", [128, 2], F32)
    Bgq = k.buf("gqk")
    onesf = k.sb("onesf", [128, 64], F32)
    Bof = k.buf("onesf")
    Wqk = k.sb("Wqk", [128, 4096], BF16)
    BWqk = k.buf("Wqk")
    Wv = k.sb("Wv", [128, NCH * 256], BF16)
    BWv = k.buf("Wv")
    Kaug = k.sb("Kaug", [128, S], BF16)
    BKlo = k.buf("Kaug_lo")
    BKhi = k.buf("Kaug_hi")
    Vall = k.sb("Vall", [128, NKT, HG, 65], BF16)
    BV = k.bufs("Vall_", NKT)
    Bvones = k.buf("Vones")
    P.dma("pool", lambda e: e.dma_start(out=ident[:, :], in_=identd[:, :]), writes=[Bid], key=Bid)
    P.dma("pool", lambda e: e.dma_start(out=bdo[:, :], in_=bdd[:, :]), writes=[Bbd], key=Bbd)
    for d_ in range(4):
        P.dma("pool", lambda e, d_=d_: e.dma_start(out=cmask[:, d_, :], in_=cmaskd[d_, :, :]), writes=[Bcm], key=Bcm)
    P.dma("pool", lambda e: e.dma_start(out=Wqk[:, :], in_=wqk[:, :]), writes=[BWqk], key=BWqk)
    P.dma("pool", lambda e: e.dma_start(out=Wv[:, :], in_=wv[:, :]), writes=[BWv], key=BWv)
    P.dma("sp", lambda e: e.dma_start(out=AB[:, :], in_=ABd[:, :]), writes=[BAB], key=BAB)
    P.dma("sp", lambda e: e.dma_start(out=shc[:, :], in_=shcd[:, :]), writes=[Bshc], key=Bshc)
    P.dma("sp", lambda e: e.dma_start(out=gq[:, :], in_=gqk[:, :]), writes=[Bgq], key=Bgq)
    P.dma("sp", lambda e: e.dma_start(out=Kaug[64:128, :], in_=kconst[:, :]), writes=[BKhi], key=BKhi)
    P.op("pool", lambda e: e.memset(onesf[:, :], 1.0), writes=[Bof])
    P.op("pool", lambda e: e.memset(Vall[:, :, :, 64:65], 1.0), writes=[Bvones])

    banks = [k.bank("bk%d" % i) for i in range(8)]
    Bb = k.bufs("bk_", 8)

    ht = [k.sb("ht%d" % i, [128, NCH, T], BF16) for i in range(2)]
    Bht = k.bufs("ht_", 2)
    sqb = k.sb("sqb", [128, T], BF16)
    Bsqb = k.buf("sqb")
    sd = k.sb("sd", [128, T], F32)
    Bsd = k.buf("sd")
    knf = k.sb("knf", [128, T], F32)
    Bknf = k.buf("knf")
    kb = [k.sb("kb%d" % i, [128, T], BF16) for i in range(2)]
    Bkb = k.bufs("kb_", 2)
    qnf = [k.sb("qnf%d" % i, [128, T], F32) for i in range(2)]
    Bqnf = k.bufs("qnf_", 2)
    kmT = [k.sb("kmT%d" % i, [128, 64], F32) for i in range(2)]
    Bkm = k.bufs("kmT_", 2)
    QAt = [k.sb("QAt%d" % i, [128, T], BF16) for i in range(2)]
    BQAt = k.bufs("QAt_", 2)
    mb = [k.sb("mb%d" % i, [128, 64], F32) for i in range(2)]
    Bmb = k.bufs("mb_", 2)
    gp = [k.sb("gp%d" % i, [128, 64], F32) for i in range(2)]
    Bgp = k.bufs("gp_", 2)
    top8 = [k.sb("top8_%d" % i, [128, 8], F32) for i in range(2)]
    Btop = k.bufs("top8_", 2)
    Mbb = [k.sb("Mbb%d" % i, [128, 128], BF16) for i in range(2)]
    BMbb = k.bufs("Mbb_", 2)
    for r in range(2):
        P.op("pool", lambda e, r=r: e.memset(Mbb[r][:, :], 0.0), writes=[BMbb[r]])
        P.op("pool", lambda e, r=r: e.memset(kmT[r][:, :], 0.0), writes=[Bkm[r]])
    pkc = 0
    rr = 0
    qac = 0
    for i in range(NQS):
        s = i % 2
        P.dma("sp", lambda e, s=s, i=i: e.dma_start(out=ht[s][:, :, :], in_=hv_[:, :, i * T:(i + 1) * T]), writes=[Bht[s]], key=Bht[s])
        for isk in (1, 0):
            for hp in range(2):
                m = isk * 2 + hp
                pb = pkc % 2
                pkc += 1
                for kc in range(NCH):
                    P.op("pe", lambda e, m=m, kc=kc, pb=pb, s=s: e.matmul(
                        banks[pb][:, :], lhsT=Wqk[:, kc * 512 + m * 128: kc * 512 + (m + 1) * 128], rhs=ht[s][:, kc, :],
                        start=(kc == 0), stop=(kc == NCH - 1)), reads=[BWqk, Bht[s]], writes=[Bb[pb]])
                P.op("act", lambda e, pb=pb: e.activation(out=sqb[:, :], in_=banks[pb][:, :], func=AF.Square), reads=[Bb[pb]], writes=[Bsqb])
                P.op("pe", lambda e: e.matmul(banks[2][:, :], lhsT=bdo[:, :], rhs=sqb[:, :], start=True, stop=True),
                     reads=[Bbd, Bsqb], writes=[Bb[2]])
                P.op("act", lambda e: e.activation(out=sd[:, :], in_=banks[2][:, :], func=AF.Sqrt, bias=C["eps"][:, 0:1], scale=1.0 / 64),
                     reads=[Bb[2], C["Beps"]], writes=[Bsd])
                P.op("dve", lambda e: e.reciprocal(out=sd[:, :], in_=sd[:, :]), reads=[Bsd], writes=[Bsd])
                if isk:
                    P.op("dve", lambda e, pb=pb: e.scalar_tensor_tensor(out=knf[:, :], in0=banks[pb][:, :], scalar=gq[:, 1:2], in1=sd[:, :],
                                                                         op0=ALU.mult, op1=ALU.mult),
                         reads=[Bb[pb], Bgq, Bsd], writes=[Bknf])
                    kbs = pkc % 2
                    P.op("act", lambda e, kbs=kbs: e.activation(out=kb[kbs][:, :], in_=knf[:, :], func=AF.Identity),
                         reads=[Bknf], writes=[Bkb[kbs]])
                    for hb in range(2):
                        h = 2 * hp + hb
                        P.dma("pool", lambda e, h=h, hb=hb, kbs=kbs, i=i: e.dma_start(
                            out=KA[h, :, i * T:(i + 1) * T], in_=kb[kbs][hb * 64:(hb + 1) * 64, :]),
                              reads=[Bkb[kbs]], writes=[BKA[h]], key=Bkb[kbs])
                    P.op("dve", lambda e, hp=hp, i=i: e.tensor_reduce(out=kmT[hp][:, 2 * i:2 * i + 2],
                                                                       in_=knf[:, :].rearrange("p (b t) -> p b t", b=2),
                                                                       axis=AX.X, op=ALU.add),
                         reads=[Bknf], writes=[Bkm[hp]])
                else:
                    P.op("dve", lambda e, pb=pb, hp=hp: e.scalar_tensor_tensor(out=qnf[hp][:, :], in0=banks[pb][:, :], scalar=gq[:, 0:1],
                                                                                in1=sd[:, :], op0=ALU.mult, op1=ALU.mult),
                         reads=[Bb[pb], Bgq, Bsd], writes=[Bqnf[hp]])
        for h in range(HG):
            hp, hb = h // 2, h % 2
            p0 = hb * 64
            qs_ = qac % 2
            qac += 1
            P.op("dve", lambda e, hp=hp, p0=p0, qs_=qs_: e.tensor_copy(out=QAt[qs_][0:64, :], in_=qnf[hp][p0:p0 + 64, :]),
                 reads=[Bqnf[hp]], writes=[BQAt[qs_]])
            for js in range(4):
                m = 2 * i + js // 2
                r = rr % 2
                rr += 1
                P.op("pool", lambda e, r=r: e.memset(mb[r][:, :], NEG), writes=[Bmb[r]])
                if m >= 3:
                    P.op("pe", lambda e, hp=hp, p0=p0, js=js: e.matmul(
                        banks[3][:, 0:64], lhsT=qnf[hp][p0:p0 + 64, js * 128:(js + 1) * 128], rhs=kmT[hp][p0:p0 + 64, 0:64],
                        start=True, stop=True), reads=[Bqnf[hp], Bkm[hp]], writes=[Bb[3]])
                    P.op("pool", lambda e, r=r: e.memset(gp[r][:, :], -1e30), writes=[Bgp[r]])
                    P.op("dve", lambda e, r=r, m=m: e.tensor_copy(out=gp[r][:, 0:m], in_=banks[3][:, 0:m]), reads=[Bb[3]], writes=[Bgp[r]])
                    P.op("dve", lambda e, r=r, m=m: e.max(out=top8[r][:, :], in_=gp[r][:, 0:max(m, 8)]), reads=[Bgp[r]], writes=[Btop[r]])
                    P.op("dve", lambda e, r=r, m=m: e.tensor_scalar(out=mb[r][:, 0:m], in0=gp[r][:, 0:m], scalar1=top8[r][:, 2:3], scalar2=1.0,
                                                                    op0=ALU.is_ge, op1=ALU.subtract),
                         reads=[Bgp[r], Btop[r]], writes=[Bmb[r]])
                    P.op("dve", lambda e, r=r, m=m: e.tensor_scalar(out=mb[r][:, 0:m], in0=mb[r][:, 0:m], scalar1=-NEG, scalar2=None,
                                                                    op0=ALU.mult), reads=[Bmb[r]], writes=[Bmb[r]])
                lo = 0 if m < 3 else m
                P.op("dve", lambda e, r=r, lo=lo, m=m: e.memset(mb[r][:, lo:m + 1], 0.0), reads=[Bmb[r]], writes=[Bmb[r]])
                P.op("dve", lambda e, r=r, js=js, h=h: e.tensor_copy(out=mb[r][:, 63:64], in_=shc[:, js * 4 + h:js * 4 + h + 1]),
                     reads=[Bmb[r], Bshc], writes=[Bmb[r]])
                P.op("dve", lambda e, r=r: e.tensor_copy(out=Mbb[r][:, 64:128], in_=mb[r][:, :]), reads=[Bmb[r]], writes=[BMbb[r]])
                P.op("pe", lambda e, r=r: e.matmul(banks[4][:, 0:128], lhsT=Mbb[r][:, :], rhs=ident[:, :], start=True, stop=True),
                     reads=[BMbb[r], Bid], writes=[Bb[4]])
                P.op("act", lambda e, qs_=qs_, js=js: e.activation(out=QAt[qs_][64:128, js * 128:(js + 1) * 128], in_=banks[4][64:128, 0:128],
                                                                    func=AF.Identity), reads=[Bb[4]], writes=[BQAt[qs_]])
            P.dma("pool", lambda e, h=h, qs_=qs_, i=i: e.dma_start(out=QA[h, :, i * T:(i + 1) * T], in_=QAt[qs_][:, :]),
                  reads=[BQAt[qs_]], writes=[BQA[h]], key=BQAt[qs_])
        for js in range(4):
            kt = 4 * i + js
            for kc in range(NCH):
                P.op("pe", lambda e, kc=kc, js=js, s=s: e.matmul(
                    banks[5][:, 0:256], lhsT=ht[s][:, kc, js * 128:(js + 1) * 128], rhs=Wv[:, kc * 256:(kc + 1) * 256],
                    start=(kc == 0), stop=(kc == NCH - 1)), reads=[Bht[s], BWv], writes=[Bb[5]])
            P.op("act", lambda e, kt=kt: e.activation(out=Vall[:, kt, :, 0:64], in_=banks[5][:, 0:256].rearrange("p (h d) -> p h d", h=HG),
                                                       func=AF.Identity), reads=[Bb[5], Bvones], writes=[BV[kt]])

    qa = [k.sb("qa%d" % i, [128, T], BF16) for i in range(2)]
    Bqa = k.bufs("qa_", 2)
    pT = [k.sb("pT%d" % i, [128, T], BF16) for i in range(3)]
    BpT = k.bufs("pT_", 3)
    rc = k.sb("rc", [128, T], F32)
    Brc = k.buf("rc")
    bc = k.sb("bc", [64, T], F32)
    Bbc = k.buf("bc")
    aot = [k.sb("aot%d" % i, [64, T], BF16) for i in range(2)]
    Baot = k.bufs("aot_", 2)
    uc = 0
    qc = 0
    for h in range(HG):
        P.dma("sp", lambda e, h=h: e.dma_start(out=Kaug[0:64, :], in_=KA[h, :, :]), reads=[BKA[h]], writes=[BKlo], key=BKlo)
        for qs in range(NQS):
            sl = qc % 2
            ob = 6 + qc % 2
            qc += 1
            P.dma("sp", lambda e, h=h, qs=qs, sl=sl: e.dma_start(out=qa[sl][:, :], in_=QA[h, :, qs * T:(qs + 1) * T]),
                  reads=[BQA[h]], writes=[Bqa[sl]], key=Bqa[sl])
            nk = 4 * qs + 4
            for kt in range(nk):
                sb_ = uc % 2
                pt_ = uc % 3
                uc += 1
                dk = kt - 4 * qs
                P.op("pe", lambda e, kt=kt, sl=sl, sb_=sb_, dk=dk: e.matmul(
                    banks[sb_][:, :], lhsT=Kaug[:, kt * 128:(kt + 1) * 128], rhs=qa[sl][:, :], start=True, stop=(dk < 0)),
                     reads=[BKlo, BKhi, Bqa[sl]], writes=[Bb[sb_]])
                if dk >= 0:
                    P.op("pe", lambda e, sb_=sb_, dk=dk: e.matmul(banks[sb_][:, :], lhsT=ident[:, :], rhs=cmask[:, dk, :], start=False, stop=True),
                         reads=[Bid, Bcm], writes=[Bb[sb_]])
                ri = dk + 124
                P.op("act", lambda e, sb_=sb_, pt_=pt_, h=h, ri=ri: e.activation(
                    out=pT[pt_][:, :], in_=banks[sb_][:, :], func=AF.Exp, bias=AB[:, h * 128 + ri:h * 128 + ri + 1], scale=0.125),
                     reads=[Bb[sb_], BAB], writes=[BpT[pt_]])
                P.op("pe", lambda e, kt=kt, h=h, pt_=pt_, ob=ob, nk=nk: e.matmul(
                    banks[ob][0:65, :], lhsT=Vall[:, kt, h, :], rhs=pT[pt_][:, :], start=(kt == 0), stop=(kt == nk - 1)),
                     reads=[BV[kt], Bvones, BpT[pt_]], writes=[Bb[ob]])
            P.op("dve", lambda e, ob=ob: e.reciprocal(out=rc[64:65, :], in_=banks[ob][64:65, :]), reads=[Bb[ob]], writes=[Brc])
            P.op("pe", lambda e: e.matmul(banks[2][0:64, :], lhsT=onesf[64:65, 0:64], rhs=rc[64:65, :], start=True, stop=True),
                 reads=[Bof, Brc], writes=[Bb[2]])
            P.op("act", lambda e: e.activation(out=bc[:, :], in_=banks[2][0:64, :], func=AF.Identity), reads=[Bb[2]], writes=[Bbc])
            ar = qc % 2
            P.op("dve", lambda e, ob=ob, ar=ar: e.tensor_tensor(out=aot[ar][:, :], in0=banks[ob][0:64, :], in1=bc[:, :], op=ALU.mult),
                 reads=[Bb[ob], Bbc], writes=[Baot[ar]])
            P.dma("pool", lambda e, h=h, qs=qs, ar=ar: e.dma_start(out=ao[h * 64:(h + 1) * 64, qs * T:(qs + 1) * T], in_=aot[ar][:, :]),
                  reads=[Baot[ar]], key=Baot[ar])
    n = P.finalize()
    return nc, n


def _bf16():
    import ml_dtypes
    return ml_dtypes.bfloat16


def a2_consts(g):
    slopes = (2.0 ** (-8.0 * (np.arange(16) + 1) / 16)).astype(np.float32)
    p = np.arange(128, dtype=np.float32)[:, None]
    ri = np.arange(128, dtype=np.float32)[None, :]
    AB = np.zeros((128, HG * 128), np.float32)
    shc = np.zeros((128, 16), np.float32)
    for hl in range(HG):
        sl = slopes[4 * g + hl]
        AB[:, hl * 128:(hl + 1) * 128] = sl * (p + 128.0 * (ri - 124.0))
        for js in range(4):
            shc[:, js * 4 + hl] = -8.0 * sl * (js * 128.0 + p[:, 0])
    return AB, shc


def a2_static():
    kc = np.zeros((64, S), np.float32)
    for n in range(63):
        kc[n, n * 256:(n + 1) * 256] = 1.0
    kc[63, :] = 1.0
    cm = np.zeros((4, 128, T), np.float32)
    col = np.arange(T)[None, :]
    for dk in range(4):
        kp = dk * 128 + np.arange(128)[:, None]
        kb_, qb_ = kp // 256, col // 256
        cm[dk] = np.where(((kb_ == qb_) & (col < kp)) | (kb_ > qb_), NEG, 0.0)
    bd = np.zeros((128, 128), np.float32)
    bd[:64, :64] = 1.0
    bd[64:, 64:] = 1.0
    return kc.astype(_bf16()), cm, bd


def run_a1(xTs, c, ada_w_i, ada_b_i, ng_i):
    nc = get_prog("a1", build_a1)
    aT = ada_rows(ada_w_i, [0, 1])
    ab = ada_cols(ada_b_i, [0, 1])
    ngp = pvec(ng_i)
    in_maps = []
    for core in range(8):
        b, _ = core_tokens(core)
        in_maps.append({"xT": xTs[core], "cb": np.ascontiguousarray(np.broadcast_to(c[b], (128, D))),
                        "adaT": aT, "adab": ab, "ng": ngp})
    res = run_bass_kernel_spmd(nc, in_maps, core_ids=list(range(8)))
    return [r["ho"] for r in res.results]


def run_a2(hTas, wqkv, gqn, gkn):
    nc = get_prog("a2", build_a2)
    kc, cm, bd = a2_static()
    ident = np.eye(128, dtype=np.float32)
    in_maps = []
    for core in range(8):
        b, g = core // 4, core % 4
        Wq = wqkv[:, g * 256:(g + 1) * 256]
        Wk = wqkv[:, D + g * 256:D + (g + 1) * 256]
        Wv = wqkv[:, 2 * D + g * 256:2 * D + (g + 1) * 256]
        wqk = tileA(np.concatenate([Wq, Wk], axis=1))[0]
        wv = np.ascontiguousarray(Wv.reshape(8, 128, 256).transpose(1, 0, 2).reshape(128, 2048))
        gqk = np.ascontiguousarray(np.stack([np.tile(gqn, 2), np.tile(gkn, 2)], axis=1).astype(np.float32))
        AB, shc = a2_consts(g)
        in_maps.append({"hTa": hTas[b], "wqk": wqk, "wv": wv, "gqk": gqk, "AB": AB, "shc": shc, "kconst": kc,
                        "cmask": cm, "ident": ident, "bdones": bd})
    res = run_bass_kernel_spmd(nc, in_maps, core_ids=list(range(8)))
    return [r["ao"] for r in res.results]


def run_a3(xTs, aTs, c, ada_w_i, ada_b_i, wo_i):
    nc = get_prog("a3", build_a3)
    aT = ada_rows(ada_w_i, [2])
    ab = ada_cols(ada_b_i, [2])
    wot = tileA(wo_i)
    in_maps = []
    for core in range(8):
        b, _ = core_tokens(core)
        in_maps.append({"xT": xTs[core], "aT": aTs[core], "cb": np.ascontiguousarray(np.broadcast_to(c[b], (128, D))),
                        "adaT": aT, "adab": ab, "wo": wot})
    res = run_bass_kernel_spmd(nc, in_maps, core_ids=list(range(8)))
    return [r["xo"] for r in res.results]


def kernel(x, c, ada_w, ada_b, mix_norm_g, mlp_norm_g,
           conv_w_in, conv_b_in, conv_dw, conv_dw_b, conv_ln_g, conv_ln_b, conv_w_out, conv_b_out,
           attn_w_qkv, attn_q_norm_g, attn_k_norm_g, attn_w_o, mlp_w1, mlp_w2):
    f = lambda a: np.asarray(a, dtype=np.float32)
    x, c, ada_w, ada_b = f(x), f(c), f(ada_w), f(ada_b)
    z = {"conv_w_in": f(conv_w_in), "conv_b_in": f(conv_b_in), "conv_dw": f(conv_dw), "conv_dw_b": f(conv_dw_b),
         "conv_ln_g": f(conv_ln_g), "conv_ln_b": f(conv_ln_b), "conv_w_out": f(conv_w_out), "conv_b_out": f(conv_b_out)}
    mix_norm_g, mlp_norm_g = f(mix_norm_g), f(mlp_norm_g)
    attn_w_qkv, attn_w_o = f(attn_w_qkv), f(attn_w_o)
    gqn, gkn = f(attn_q_norm_g), f(attn_k_norm_g)
    mlp_w1, mlp_w2 = f(mlp_w1), f(mlp_w2)
    xTs = [np.ascontiguousarray(x[core // 4, (core % 4) * TOK:(core % 4 + 1) * TOK].T) for core in range(8)]
    for i in range(4):
        j = i // 2
        if i % 2 == 0:
            halos = []
            for core in range(8):
                if core % 4 == 0:
                    halos.append(np.zeros((D, HALO), np.float32))
                else:
                    halos.append(np.ascontiguousarray(xTs[core - 1][:, TOK - HALO:]))
            xTs = run_conv(xTs, halos, c, ada_w[i], ada_b[i], mix_norm_g[i], z, j)
        else:
            hos = run_a1(xTs, c, ada_w[i], ada_b[i], mix_norm_g[i])
            hTas = [np.ascontiguousarray(np.concatenate(hos[4 * b:4 * b + 4], axis=1)) for b in range(NB)]
            aos = run_a2(hTas, attn_w_qkv[j], gqn[j], gkn[j])
            aTs = []
            for core in range(8):
                b, q = core // 4, core % 4
                aTs.append(np.ascontiguousarray(np.concatenate([aos[4 * b + g][:, q * TOK:(q + 1) * TOK] for g in range(4)], axis=0)))
            xTs = run_a3(xTs, aTs, c, ada_w[i], ada_b[i], attn_w_o[j])
        xTs = run_mlp(xTs, c, ada_w[i], ada_b[i], mlp_norm_g[i], mlp_w1[i], mlp_w2[i])
    out = np.empty((NB, S, D), np.float32)
    for core in range(8):
        b, q = core // 4, core % 4
        out[b, q * TOK:(q + 1) * TOK] = xTs[core].T
    return out
```

```python
import numpy as np
import concourse.bass as bass
import concourse.mybir as mybir
from concourse.bass_utils import run_bass_kernel_spmd

F32 = mybir.dt.float32
BF16 = mybir.dt.bfloat16
ALU = mybir.AluOpType
AF = mybir.ActivationFunctionType
AX = mybir.AxisListType

EPOCH = 20000
ENGS = ("pe", "act", "dve", "pool", "sp")

D = 1024
NCH = 8
S = 16384
NB = 2
TOK = 4096
T = 512
NT = TOK // T
CW = 31
HALO = CW - 1
EPS = 1e-6
NEG = -30000.0


class Buf:
    __slots__ = ("name", "w", "r", "dcount")

    def __init__(self, name):
        self.name = name
        self.w = None
        self.r = []
        self.dcount = 0


class Op:
    __slots__ = ("eng", "fn", "reads", "writes", "idx", "waits", "sig", "key", "ev", "inc")

    def __init__(self, eng, fn, reads, writes, key=None, inc=16):
        self.inc = inc
        self.eng = eng
        self.fn = fn
        self.reads = reads
        self.writes = writes
        self.key = key
        self.waits = []
        self.sig = False
        self.ev = None


class Prog:
    def __init__(self, nc):
        self.nc = nc
        self.ops = []
        self.sems = {}

    def op(self, eng, fn, reads=(), writes=()):
        o = Op(eng, fn, tuple(reads), tuple(writes))
        self.ops.append(o)
        return o

    def dma(self, q, fn, reads=(), writes=(), key=None, inc=16):
        o = Op(q, fn, tuple(reads), tuple(writes), key=key, inc=inc)
        self.ops.append(o)
        return o

    def barrier(self):
        o = Op(None, None, (), ())
        self.ops.append(o)
        return o

    def _sem(self, name):
        if name not in self.sems:
            self.sems[name] = self.nc.alloc_semaphore(name=name)
        return self.sems[name]

    def finalize(self, final_eng="sp"):
        nc = self.nc
        for i, o in enumerate(self.ops):
            o.idx = i
        deps_of = []
        last_op = {}
        bar_ops = {}
        for o in self.ops:
            if o.eng is None:
                bar_ops[o.idx] = dict(last_op)
                for d in last_op.values():
                    if d.key is None:
                        d.sig = True
                deps_of.append(([], []))
                continue
            deps = set()
            for b in o.reads:
                if b.w is not None:
                    deps.add(b.w)
            for b in o.writes:
                if b.w is not None:
                    deps.add(b.w)
                for r in b.r:
                    deps.add(r)
            deps.discard(o)
            for b in o.reads:
                b.r.append(o)
            for b in o.writes:
                b.w = o
                b.r = []
            best = {}
            dma_deps = []
            for d in deps:
                if d.key is not None:
                    dma_deps.append(d)
                else:
                    if d.eng == "pe" and o.eng == "pe":
                        continue
                    if d.eng not in best or best[d.eng].idx < d.idx:
                        best[d.eng] = d
            deps_of.append((list(best.values()), dma_deps))
            for d in best.values():
                d.sig = True
            if o.key is None:
                last_op[o.eng] = o
        cnt = {e: 0 for e in ENGS}
        slot_total = []
        free_slots = []
        used_slots = []
        buf_slot = {}
        for o in self.ops:
            if o.eng is None:
                free_slots.extend(used_slots)
                used_slots = []
                continue
            if o.key is not None:
                b = o.key
                if id(b) not in buf_slot:
                    if free_slots:
                        sl = free_slots.pop()
                    else:
                        sl = len(slot_total)
                        slot_total.append(0)
                    buf_slot[id(b)] = sl
                    used_slots.append(sl)
                sl = buf_slot[id(b)]
                slot_total[sl] += o.inc
                o.ev = ("dma_%d" % sl, slot_total[sl])
            elif o.sig:
                c = cnt[o.eng]
                cnt[o.eng] = c + 1
                o.ev = ("%s_%d" % (o.eng, c // EPOCH), c % EPOCH + 1)
        running = {}
        known = {e: {} for e in ENGS}
        pending = {e: None for e in ENGS}
        bar_snap = {}
        for o, (cdeps, ddeps) in zip(self.ops, deps_of):
            if o.eng is None:
                bar_snap[o.idx] = dict(running)
                for e in ENGS:
                    pending[e] = o.idx
                continue
            w = {}
            if pending[o.eng] is not None:
                bi = pending[o.eng]
                pending[o.eng] = None
                for d in bar_ops[bi].values():
                    if d.ev is not None and d.key is None:
                        s_, v_ = d.ev
                        if w.get(s_, 0) < v_:
                            w[s_] = v_
                for s_, v_ in bar_snap[bi].items():
                    if w.get(s_, 0) < v_:
                        w[s_] = v_
            for d in cdeps:
                s_, v_ = d.ev
                if w.get(s_, 0) < v_:
                    w[s_] = v_
            for d in ddeps:
                s_ = d.ev[0]
                v_ = running[s_]
                if w.get(s_, 0) < v_:
                    w[s_] = v_
            kn = known[o.eng]
            for s_, v_ in w.items():
                if kn.get(s_, 0) < v_:
                    kn[s_] = v_
                    o.waits.append((s_, v_))
            if o.key is not None:
                running[o.ev[0]] = o.ev[1]
        for o in self.ops:
            for s_, v_ in o.waits:
                self._sem(s_)
            if o.ev is not None:
                self._sem(o.ev[0])
        per_eng = {e: [o for o in self.ops if o.eng == e] for e in ENGS}
        sems = self.sems
        finals = sorted(running.items())
        self.n_sems = len(sems)

        def emit(engobj, ename):
            for o in per_eng[ename]:
                for s_, v_ in o.waits:
                    engobj.wait_ge(sems[s_], v_)
                ins = o.fn(engobj)
                if o.ev is not None:
                    ins.then_inc(sems[o.ev[0]], o.inc if o.key is not None else 1)
            if ename == final_eng:
                for s_, v_ in finals:
                    engobj.wait_ge(sems[s_], v_)

        with nc.Block() as block:
            @block.tensor
            def _(e):
                emit(e, "pe")

            @block.scalar
            def _(e):
                emit(e, "act")

            @block.vector
            def _(e):
                emit(e, "dve")

            @block.gpsimd
            def _(e):
                emit(e, "pool")

            @block.sync
            def _(e):
                emit(e, "sp")
        return len(self.ops)


ARENA_WORDS = 52600
UPTO = 4


class KB:
    def __init__(self, fused=False):
        self.nc = bass.Bass("TRN2", target_bir_lowering=False)
        self.P = Prog(self.nc)
        self.fused = fused
        self.tag = ""
        if fused:
            self.arena = self.nc.alloc_sbuf_tensor("arena", [128, ARENA_WORDS], F32)
            self.banks = [self.nc.alloc_psum_tensor("bank%d" % i, [128, 512], F32) for i in range(8)]
            self.off = 0
            self.nb = 0

    def begin_pass(self, tag):
        self.tag = tag
        self.off = 0
        self.nb = 0

    def din(self, name, shape, dt=F32):
        return self.nc.dram_tensor(name, list(shape), dt, kind="ExternalInput").ap()

    def dout(self, name, shape, dt=F32):
        return self.nc.dram_tensor(name, list(shape), dt, kind="ExternalOutput").ap()

    def dint(self, name, shape, dt=F32):
        return self.nc.dram_tensor(name, list(shape), dt, kind="Internal").ap()

    def sb(self, name, shape, dt=F32):
        if not self.fused:
            return self.nc.alloc_sbuf_tensor(name, list(shape), dt)
        esz = 4 if dt == F32 else 2
        n = 1
        for d_ in shape[1:]:
            n *= d_
        words = (n * esz + 3) // 4
        words = (words + 7) // 8 * 8
        assert self.off + words <= ARENA_WORDS, (self.tag, name, self.off, words)
        v = self.arena[0:shape[0], self.off:self.off + words]
        self.off += words
        if dt != F32:
            v = v.bitcast(dt)
        v = v[:, 0:n]
        if len(shape) == 3:
            v = v.rearrange("p (a b) -> p a b", a=shape[1])
        elif len(shape) == 4:
            v = v.rearrange("p (a b c) -> p a b c", a=shape[1], b=shape[2])
        return v

    def bank(self, name):
        if not self.fused:
            return self.nc.alloc_psum_tensor(name, [128, 512], F32)
        b = self.banks[self.nb]
        self.nb += 1
        return b

    def bufs(self, name, n):
        return [Buf("%s%s%d" % (self.tag, name, i)) for i in range(n)]

    def buf(self, name):
        return Buf(self.tag + name)


def emit_consts(k):
    P = k.P
    c = {}
    c["ones"] = k.sb("c_ones", [128, 128], BF16)
    c["eps"] = k.sb("c_eps", [128, 1], F32)
    c["Bones"] = k.buf("c_ones")
    c["Beps"] = k.buf("c_eps")
    P.op("pool", lambda e: e.memset(c["ones"][:, :], 1.0), writes=[c["Bones"]])
    P.op("pool", lambda e: e.memset(c["eps"][:, :], EPS), writes=[c["Beps"]])
    return c


def emit_mod(k, cb, adaT, adab, nv, R, BR):
    P = k.P
    n = nv * 8
    Rf = R[:, :, :].rearrange("p c t -> p (c t)")
    wch = [Rf[:, 0:D], Rf[:, D:2 * D]]
    Bw = [[BR[0], BR[1]], [BR[2], BR[3]]]
    junk = Rf[:, 2 * D:3 * D]
    Bj = [BR[4], BR[5]]
    scb = Rf[:, 3 * D:4 * D]
    Bscb = [BR[6], BR[7]]
    mod = k.sb("m_mod", [128, n], F32)
    Bmod = k.buf("m_mod")
    ab = k.sb("m_ab", [128, n], F32)
    Bab = k.buf("m_ab")
    P.dma("sp", lambda e: e.dma_start(out=scb, in_=cb[:, :]), writes=Bscb, key=BR[6])
    P.dma("sp", lambda e: e.dma_start(out=ab[:, :], in_=adab[:, :]), writes=[Bab], key=Bab)
    P.op("act", lambda e: e.activation(out=junk, in_=scb, func=AF.Sigmoid), reads=Bscb, writes=Bj)
    P.op("dve", lambda e: e.tensor_tensor(out=scb, in0=scb, in1=junk, op=ALU.mult),
         reads=Bscb + Bj, writes=Bscb)
    for j in range(n):
        s = j % 2
        P.dma("sp", lambda e, s=s, j=j: e.dma_start(out=wch[s], in_=adaT[j, :, :]), writes=Bw[s], key=Bw[s][0])
        P.op("dve", lambda e, s=s: e.tensor_tensor(out=junk, in0=wch[s], in1=scb, op=ALU.mult),
             reads=Bw[s] + Bscb, writes=Bj)
        P.op("dve", lambda e, j=j: e.tensor_reduce(out=mod[:, j:j + 1], in_=junk, axis=AX.X, op=ALU.add),
             reads=Bj, writes=[Bmod])
    P.op("dve", lambda e: e.tensor_tensor(out=mod[:, :], in0=mod[:, :], in1=ab[:, :], op=ALU.add),
         reads=[Bmod, Bab], writes=[Bmod])
    return mod, Bmod


def emit_scale(k, name, mod, Bmod, sc_lo, g_ap, Bg):
    P = k.P
    sc = k.sb(name, [128, NCH], F32)
    Bsc = k.buf(name)
    P.op("dve", lambda e: e.tensor_scalar(out=sc[:, :], in0=mod[:, sc_lo:sc_lo + NCH], scalar1=1.0, scalar2=None,
                                          op0=ALU.add), reads=[Bmod], writes=[Bsc])
    P.op("dve", lambda e: e.tensor_tensor(out=sc[:, :], in0=sc[:, :], in1=g_ap, op=ALU.mult),
         reads=[Bsc, Bg], writes=[Bsc])
    return sc, Bsc


class NormCtx:
    def __init__(self, k, consts, width=T):
        self.k = k
        self.c = consts
        self.sq = k.sb("n_sq", [128, NCH, width], BF16)
        self.Bsq = k.buf("n_sq")
        self.st = k.bank("n_st")
        self.Bst = k.buf("n_st")
        self.sd = k.sb("n_sd", [128, width], F32)
        self.Bsd = k.buf("n_sd")
        self.rstd = self.sd
        self.Brstd = self.Bsd
        self.tmp = [k.sb("n_tmp%d" % i, [128, width], F32) for i in range(2)]
        self.Btmp = k.bufs("n_tmp", 2)


def emit_norm(n, xt, Bx, w, scale, Bscale, shift_ap_fn, Bshift, hT, BhT):
    k = n.k
    P = k.P
    c = n.c
    P.op("act", lambda e: e.activation(out=n.sq[:, :, 0:w], in_=xt[:, :, 0:w], func=AF.Square), reads=list(Bx), writes=[n.Bsq])
    for ch in range(NCH):
        P.op("pe", lambda e, ch=ch: e.matmul(n.st[:, 0:w], lhsT=c["ones"][:, :], rhs=n.sq[:, ch, 0:w],
                                              start=(ch == 0), stop=(ch == NCH - 1)),
             reads=[n.Bsq, c["Bones"]], writes=[n.Bst])
    P.op("act", lambda e: e.activation(out=n.sd[:, 0:w], in_=n.st[:, 0:w], func=AF.Sqrt, bias=c["eps"][:, 0:1], scale=1.0 / D),
         reads=[n.Bst, c["Beps"]], writes=[n.Bsd])
    P.op("dve", lambda e: e.reciprocal(out=n.rstd[:, 0:w], in_=n.sd[:, 0:w]), reads=[n.Bsd], writes=[n.Brstd])
    for ch in range(NCH):
        s = ch % 2
        P.op("dve", lambda e, ch=ch, s=s: e.scalar_tensor_tensor(out=n.tmp[s][:, 0:w], in0=xt[:, ch, 0:w],
                                                                  scalar=scale[:, ch:ch + 1], in1=n.rstd[:, 0:w],
                                                                  op0=ALU.mult, op1=ALU.mult),
             reads=[Bx[ch], Bscale, n.Brstd], writes=[n.Btmp[s]])
        P.op("act", lambda e, ch=ch, s=s: e.activation(out=hT[:, ch, 0:w], in_=n.tmp[s][:, 0:w], func=AF.Identity,
                                                        bias=shift_ap_fn(ch), scale=1.0),
             reads=[n.Btmp[s], Bshift], writes=[BhT[ch]])


def build_mlp():
    k = KB()
    io = {"xT": k.din("xT", [D, TOK]), "xo": k.dout("xo", [D, TOK]), "cb": k.din("cb", [128, D]),
          "adaT": k.din("adaT", [24, 128, D]), "adab": k.din("adab", [128, 24]), "ng": k.din("ng", [128, NCH]),
          "w1": k.din("w1", [8, 128, 4096]), "w2": k.din("w2", [8, 128, 4096])}
    emit_mlp(k, io)
    n = k.P.finalize()
    return k.nc, n


def emit_mlp(k, io):
    nc, P = k.nc, k.P
    xT, xo, cb, adaT, adab, ng, w1, w2 = [io[n_] for n_ in ("xT", "xo", "cb", "adaT", "adab", "ng", "w1", "w2")]
    xv = xT.rearrange("(c p) t -> p c t", p=128)
    xov = xo.rearrange("(c p) t -> p c t", p=128)

    C = emit_consts(k)
    W1 = k.sb("W1", [128, 8, 4096], BF16)
    W2 = k.sb("W2", [128, 8, 4096], BF16)
    BW1 = k.bufs("W1_", 8)
    BW2 = k.bufs("W2_", 8)
    xt = [k.sb("xt%d" % i, [128, NCH, T], F32) for i in range(2)]
    Bxt = [k.bufs("xt%d_" % i, NCH) for i in range(2)]
    mod, Bmod = emit_mod(k, cb, adaT, adab, 3, xt[1], Bxt[1])
    ngt = k.sb("ngt", [128, NCH], F32)
    Bng = k.buf("ngt")
    P.dma("sp", lambda e: e.dma_start(out=ngt[:, :], in_=ng[:, :]), writes=[Bng], key=Bng)
    scale2, Bsc2 = emit_scale(k, "scale2", mod, Bmod, 8, ngt[:, :], Bng)
    for t in range(8):
        P.dma("pool", lambda e, t=t: e.dma_start(out=W1[:, t, :], in_=w1[t, :, :]), writes=[BW1[t]], key=BW1[t])
    for t in range(8):
        P.dma("pool", lambda e, t=t: e.dma_start(out=W2[:, t, :], in_=w2[t, :, :]), writes=[BW2[t]], key=BW2[t])

    N = NormCtx(k, C)
    hT = k.sb("hT", [128, NCH, T], BF16)
    BhT = k.bufs("hT_", NCH)
    hid = k.sb("hid", [128, 16, T], BF16)
    Bhid = k.bufs("hid_", 16)
    r = [k.sb("r%d" % i, [128, T], F32) for i in range(2)]
    Br = k.bufs("r_", 2)
    ph = [k.bank("ph%d" % i) for i in range(3)]
    Bph = k.bufs("ph_", 3)
    po = [k.bank("po%d" % i) for i in range(2)]
    Bpo = k.bufs("po_", 2)

    hcount = 0
    ocount = 0
    for i in range(NT):
        s = i % 2
        x = xt[s]
        Bx = Bxt[s]
        P.dma("sp", lambda e, x=x, i=i: e.dma_start(out=x[:, :, :], in_=xv[:, :, i * T:(i + 1) * T]), writes=Bx, key=Bx[0])
        emit_norm(N, x, Bx, T, scale2, Bsc2, lambda ch: mod[:, ch:ch + 1], Bmod, hT, BhT)
        for half in range(2):
            for jc in range(16):
                j = half * 16 + jc
                wt, m = j // 4, j % 4
                pb = hcount % 3
                rb = hcount % 2
                hcount += 1
                for kc in range(NCH):
                    P.op("pe", lambda e, wt=wt, m=m, kc=kc, pb=pb: e.matmul(
                        ph[pb][:, :], lhsT=W1[:, wt, kc * 512 + m * 128: kc * 512 + (m + 1) * 128], rhs=hT[:, kc, :],
                        start=(kc == 0), stop=(kc == NCH - 1)),
                         reads=[BW1[wt], BhT[kc]], writes=[Bph[pb]])
                P.op("act", lambda e, pb=pb, rb=rb: e.activation(out=r[rb][:, :], in_=ph[pb][:, :], func=AF.Relu),
                     reads=[Bph[pb]], writes=[Br[rb]])
                P.op("dve", lambda e, rb=rb, jc=jc: e.tensor_tensor(out=hid[:, jc, :], in0=r[rb][:, :], in1=r[rb][:, :], op=ALU.mult),
                     reads=[Br[rb]], writes=[Bhid[jc]])
            for c in range(NCH):
                ob = ocount % 2
                ocount += 1
                for kc in range(16):
                    kg = half * 16 + kc
                    P.op("pe", lambda e, c=c, kc=kc, kg=kg, ob=ob: e.matmul(
                        po[ob][:, :], lhsT=W2[:, c, kg * 128:(kg + 1) * 128], rhs=hid[:, kc, :],
                        start=(kc == 0), stop=(kc == 15)),
                         reads=[BW2[c], Bhid[kc]], writes=[Bpo[ob]])
                P.op("dve", lambda e, c=c, ob=ob, x=x: e.scalar_tensor_tensor(
                    out=x[:, c, :], in0=po[ob][:, :], scalar=mod[:, 16 + c:17 + c], in1=x[:, c, :],
                    op0=ALU.mult, op1=ALU.add),
                     reads=[Bpo[ob], Bmod, Bx[c]], writes=[Bx[c]])
        P.dma("sp", lambda e, x=x, i=i: e.dma_start(out=xov[:, :, i * T:(i + 1) * T], in_=x[:, :, :]), reads=Bx, key=Bx[1])


def tileA(W):
    K, N = W.shape
    assert K == 1024
    return np.ascontiguousarray(W.reshape(8, 128, N // 512, 512).transpose(2, 1, 0, 3).reshape(N // 512, 128, 4096))


def tileB(W2):
    return np.ascontiguousarray(W2.reshape(32, 128, 8, 128).transpose(2, 1, 0, 3).reshape(8, 128, 4096))


def pvec(v):
    return np.ascontiguousarray(v.reshape(-1, 128).T)


def ada_rows(ada_w_i, vec_ids):
    WT = ada_w_i.T
    rows = np.concatenate([WT[v * D:(v + 1) * D] for v in vec_ids])
    return np.ascontiguousarray(rows.reshape(-1, 128, D))


def ada_cols(ada_b_i, vec_ids):
    return np.ascontiguousarray(np.concatenate([pvec(ada_b_i[v * D:(v + 1) * D]) for v in vec_ids], axis=1))


def core_tokens(core):
    b, q = core // 4, core % 4
    return b, q * TOK


_cache = {}


def get_prog(name, builder):
    if name not in _cache:
        _cache[name] = builder()[0]
    return _cache[name]


def run_mlp(xTs, c, ada_w_i, ada_b_i, ng_i, w1_i, w2_i):
    nc = get_prog("mlp", build_mlp)
    aT = ada_rows(ada_w_i, [3, 4, 5])
    ab = ada_cols(ada_b_i, [3, 4, 5])
    w1t = tileA(w1_i)
    w2t = tileB(w2_i)
    ngp = pvec(ng_i)
    in_maps = []
    for core in range(8):
        b, _ = core_tokens(core)
        in_maps.append({"xT": xTs[core], "cb": np.ascontiguousarray(np.broadcast_to(c[b], (128, D))),
                        "adaT": aT, "adab": ab, "ng": ngp, "w1": w1t, "w2": w2t})
    res = run_bass_kernel_spmd(nc, in_maps, core_ids=list(range(8)))
    return [r["xo"] for r in res.results]


NVC = 56 + NCH * CW


def build_conv():
    k = KB()
    xh = k.din("xh", [D, HALO])
    io = {"xT": k.din("xT", [D, TOK]), "xh_fn": (lambda e: xh.rearrange("(c p) t -> p c t", p=128)), "hv": k.din("hv", [128, 1]),
          "xo": k.dout("xo", [D, TOK]), "cb": k.din("cb", [128, D]), "adaT": k.din("adaT", [24, 128, D]),
          "adab": k.din("adab", [128, 24]), "vec": k.din("vec", [128, NVC]), "ident": k.din("ident", [128, 128]),
          "win": k.din("win", [4, 128, 4096]), "wout": k.din("wout", [2, 128, 4096])}
    emit_conv(k, io)
    n = k.P.finalize()
    return k.nc, n


def emit_conv(k, io):
    nc, P = k.nc, k.P
    xT, hv, xo, cb, adaT, adab, vec, identd, win, wout = [io[n_] for n_ in (
        "xT", "hv", "xo", "cb", "adaT", "adab", "vec", "ident", "win", "wout")]
    xh_fn = io["xh_fn"]
    xv = xT.rearrange("(c p) t -> p c t", p=128)
    xov = xo.rearrange("(c p) t -> p c t", p=128)

    C = emit_consts(k)
    ident = k.sb("ident_sb", [128, 128], BF16)
    Bid = k.buf("ident")
    P.dma("pool", lambda e: e.dma_start(out=ident[:, :], in_=identd[:, :]), writes=[Bid], key=Bid)
    vt = k.sb("vt", [128, NVC], F32)
    Bvt = k.buf("vt")
    P.dma("sp", lambda e: e.dma_start(out=vt[:, :], in_=vec[:, :]), writes=[Bvt], key=Bvt)
    hvt = k.sb("hvt", [128, 1], F32)
    Bhv = k.buf("hvt")
    P.dma("sp", lambda e: e.dma_start(out=hvt[:, :], in_=hv[:, :]), writes=[Bhv], key=Bhv)
    xt = [k.sb("xt%d" % i, [128, NCH, T], F32) for i in range(2)]
    Bxt = [k.bufs("xt%d_" % i, NCH) for i in range(2)]
    mod, Bmod = emit_mod(k, cb, adaT, adab, 3, xt[1], Bxt[1])
    scale1, Bsc1 = emit_scale(k, "scale1", mod, Bmod, 8, vt[:, 0:8], Bvt)
    bg = k.sb("bg", [128, NCH], F32)
    Bbg = k.buf("bg")
    P.op("dve", lambda e: e.tensor_tensor(out=bg[:, :], in0=vt[:, 48:56], in1=mod[:, 16:24], op=ALU.mult),
         reads=[Bvt, Bmod], writes=[Bbg])
    Win = k.sb("Win", [128, 4, 4096], BF16)
    Wout = k.sb("Wout", [128, 2, 4096], BF16)
    BWin = k.bufs("Win_", 4)
    BWout = k.bufs("Wout_", 2)
    for t in range(4):
        P.dma("pool", lambda e, t=t: e.dma_start(out=Win[:, t, :], in_=win[t, :, :]), writes=[BWin[t]], key=BWin[t])
    for t in range(2):
        P.dma("pool", lambda e, t=t: e.dma_start(out=Wout[:, t, :], in_=wout[t, :, :]), writes=[BWout[t]], key=BWout[t])

    N = NormCtx(k, C)
    hT = k.sb("hT", [128, NCH, T], BF16)
    BhT = k.bufs("hT_", NCH)
    sg = N.sq
    Bsg = N.Bsq
    ub = [k.sb("ub%d" % i, [128, NCH, T + HALO], BF16) for i in range(2)]
    Bu = [k.bufs("ub%d_" % i, NCH) for i in range(2)]
    xht = k.sb("xht", [128, NCH, HALO], F32)
    Bxh = k.bufs("xht_", NCH)
    dg = [k.sb("dg%d" % i, [128, CW, 128], BF16) for i in range(2)]
    Bdg = k.bufs("dg_", 2)
    v = k.sb("v", [128, NCH, T], F32)
    Bv = k.bufs("v_", NCH)
    vb = [k.sb("vb%d" % i, [128, T], BF16) for i in range(2)]
    Bvb = k.bufs("vb_", 2)
    vsq = [k.sb("vsq%d" % i, [128, T], BF16) for i in range(2)]
    Bvsq = k.bufs("vsq_", 2)
    mean = k.sb("mean", [128, T], F32)
    Bmean = k.buf("mean")
    var = k.sb("var", [128, T], F32)
    Bvar = k.buf("var")
    t1 = [k.sb("t1_%d" % i, [128, T], F32) for i in range(2)]
    Bt1 = k.bufs("t1_", 2)
    pw = [k.bank("pw%d" % i) for i in range(2)]
    Bpw = k.bufs("pw_", 2)
    pc = [k.bank("pc%d" % i) for i in range(2)]
    Bpc = k.bufs("pc_", 2)
    pm = k.bank("pm")
    Bpm = k.buf("pm")
    pq = k.bank("pq")
    Bpq = k.buf("pq")
    cnt = {"w": 0, "c": 0}

    def front(x, Bx, w, slot, off):
        emit_norm(N, x, Bx, w, scale1, Bsc1, lambda ch: mod[:, ch:ch + 1], Bmod, hT, BhT)
        for c in range(NCH):
            for part in (1, 0):
                j = part * 8 + c
                wt, m = j // 4, j % 4
                pb = cnt["w"] % 2
                cnt["w"] += 1
                for kc in range(NCH):
                    P.op("pe", lambda e, wt=wt, m=m, kc=kc, pb=pb: e.matmul(
                        pw[pb][:, 0:w], lhsT=Win[:, wt, kc * 512 + m * 128: kc * 512 + (m + 1) * 128], rhs=hT[:, kc, 0:w],
                        start=(kc == 0), stop=(kc == NCH - 1)),
                         reads=[BWin[wt], BhT[kc]], writes=[Bpw[pb]])
                if part == 1:
                    P.op("act", lambda e, c=c, pb=pb: e.activation(out=sg[:, c, 0:w], in_=pw[pb][:, 0:w], func=AF.Sigmoid,
                                                                   bias=vt[:, 16 + c:17 + c], scale=1.0),
                         reads=[Bpw[pb], Bvt], writes=[Bsg])
                else:
                    P.op("dve", lambda e, c=c, pb=pb: e.scalar_tensor_tensor(
                        out=ub[slot][:, c, off:off + w], in0=pw[pb][:, 0:w], scalar=vt[:, 8 + c:9 + c], in1=sg[:, c, 0:w],
                        op0=ALU.add, op1=ALU.mult),
                         reads=[Bpw[pb], Bvt, Bsg], writes=[Bu[slot][c]])

    P.dma("sp", lambda e: e.dma_start(out=xht[:, :, :], in_=xh_fn(e)), writes=Bxh, key=Bxh[0])
    front(xht, Bxh, HALO, 0, 0)
    P.op("dve", lambda e: e.tensor_scalar(out=ub[0][:, :, 0:HALO], in0=ub[0][:, :, 0:HALO], scalar1=hvt[:, 0:1], scalar2=None,
                                          op0=ALU.mult), reads=Bu[0] + [Bhv], writes=Bu[0])

    for i in range(NT):
        s = i % 2
        x = xt[s]
        Bx = Bxt[s]
        P.dma("sp", lambda e, x=x, i=i: e.dma_start(out=x[:, :, :], in_=xv[:, :, i * T:(i + 1) * T]), writes=Bx, key=Bx[0])
        front(x, Bx, T, s, HALO)
        P.op("dve", lambda e, s=s: e.tensor_copy(out=ub[1 - s][:, :, 0:HALO], in_=ub[s][:, :, T:T + HALO]),
             reads=Bu[s], writes=Bu[1 - s])
        for c in range(NCH):
            ds = cnt["c"] % 2
            cb_ = cnt["c"] % 2
            cnt["c"] += 1
            P.op("pool", lambda e, c=c, ds=ds: e.tensor_tensor(
                out=dg[ds][:, :, :], in0=ident[:, :].unsqueeze(1).broadcast_to([128, CW, 128]),
                in1=vt[:, 56 + c * CW:56 + (c + 1) * CW].unsqueeze(2).broadcast_to([128, CW, 128]), op=ALU.mult),
                 reads=[Bid, Bvt], writes=[Bdg[ds]])
            for kk in range(CW):
                P.op("pe", lambda e, c=c, kk=kk, ds=ds, cb_=cb_, s=s: e.matmul(
                    pc[cb_][:, :], lhsT=dg[ds][:, kk, :], rhs=ub[s][:, c, kk:kk + T], start=(kk == 0), stop=(kk == CW - 1)),
                     reads=[Bdg[ds], Bu[s][c]], writes=[Bpc[cb_]])
            P.op("act", lambda e, c=c, cb_=cb_: e.activation(out=v[:, c, :], in_=pc[cb_][:, :], func=AF.Identity,
                                                             bias=vt[:, 24 + c:25 + c], scale=1.0),
                 reads=[Bpc[cb_], Bvt], writes=[Bv[c]])
            P.op("act", lambda e, c=c, cb_=cb_, ds=ds: e.activation(out=vsq[ds][:, :], in_=pc[cb_][:, :], func=AF.Square,
                                                                    bias=vt[:, 24 + c:25 + c], scale=1.0),
                 reads=[Bpc[cb_], Bvt], writes=[Bvsq[ds]])
            P.op("dve", lambda e, c=c, ds=ds: e.tensor_copy(out=vb[ds][:, :], in_=v[:, c, :]), reads=[Bv[c]], writes=[Bvb[ds]])
            P.op("pe", lambda e, c=c, ds=ds: e.matmul(pm[:, :], lhsT=C["ones"][:, :], rhs=vb[ds][:, :], start=(c == 0), stop=(c == NCH - 1)),
                 reads=[Bvb[ds], C["Bones"]], writes=[Bpm])
            P.op("pe", lambda e, c=c, ds=ds: e.matmul(pq[:, :], lhsT=C["ones"][:, :], rhs=vsq[ds][:, :], start=(c == 0), stop=(c == NCH - 1)),
                 reads=[Bvsq[ds], C["Bones"]], writes=[Bpq])
        P.op("act", lambda e: e.activation(out=mean[:, :], in_=pm[:, :], func=AF.Identity, scale=1.0 / D), reads=[Bpm], writes=[Bmean])
        P.op("dve", lambda e: e.tensor_tensor(out=var[:, :], in0=mean[:, :], in1=mean[:, :], op=ALU.mult), reads=[Bmean], writes=[Bvar])
        P.op("dve", lambda e: e.scalar_tensor_tensor(out=var[:, :], in0=pq[:, :], scalar=1.0 / D, in1=var[:, :],
                                                     op0=ALU.mult, op1=ALU.subtract), reads=[Bpq, Bvar], writes=[Bvar])
        P.op("act", lambda e: e.activation(out=var[:, :], in_=var[:, :], func=AF.Sqrt, bias=C["eps"][:, 0:1], scale=1.0),
             reads=[Bvar, C["Beps"]], writes=[Bvar])
        P.op("dve", lambda e: e.reciprocal(out=var[:, :], in_=var[:, :]), reads=[Bvar], writes=[Bvar])
        for c in range(NCH):
            ts_ = c % 2
            P.op("dve", lambda e, c=c, ts_=ts_: e.tensor_tensor(out=t1[ts_][:, :], in0=v[:, c, :], in1=mean[:, :], op=ALU.subtract),
                 reads=[Bv[c], Bmean], writes=[Bt1[ts_]])
            P.op("dve", lambda e, ts_=ts_: e.tensor_tensor(out=t1[ts_][:, :], in0=t1[ts_][:, :], in1=var[:, :], op=ALU.mult),
                 reads=[Bt1[ts_], Bvar], writes=[Bt1[ts_]])
            P.op("act", lambda e, c=c, ts_=ts_: e.activation(out=hT[:, c, :], in_=t1[ts_][:, :], func=AF.Silu,
                                                             bias=vt[:, 40 + c:41 + c], scale=vt[:, 32 + c:33 + c]),
                 reads=[Bt1[ts_], Bvt], writes=[BhT[c]])
        for c in range(NCH):
            wt, m = c // 4, c % 4
            pb = cnt["w"] % 2
            cnt["w"] += 1
            for kc in range(NCH):
                P.op("pe", lambda e, wt=wt, m=m, kc=kc, pb=pb: e.matmul(
                    pw[pb][:, :], lhsT=Wout[:, wt, kc * 512 + m * 128: kc * 512 + (m + 1) * 128], rhs=hT[:, kc, :],
                    start=(kc == 0), stop=(kc == NCH - 1)),
                     reads=[BWout[wt], BhT[kc]], writes=[Bpw[pb]])
            P.op("dve", lambda e, c=c, pb=pb, x=x: e.scalar_tensor_tensor(
                out=x[:, c, :], in0=pw[pb][:, :], scalar=mod[:, 16 + c:17 + c], in1=x[:, c, :], op0=ALU.mult, op1=ALU.add),
                 reads=[Bpw[pb], Bmod, Bx[c]], writes=[Bx[c]])
            P.op("dve", lambda e, c=c, x=x: e.tensor_scalar(out=x[:, c, :], in0=x[:, c, :], scalar1=bg[:, c:c + 1], scalar2=None,
                                                            op0=ALU.add), reads=[Bx[c], Bbg], writes=[Bx[c]])
        P.dma("sp", lambda e, x=x, i=i: e.dma_start(out=xov[:, :, i * T:(i + 1) * T], in_=x[:, :, :]), reads=Bx, key=Bx[1])


def conv_vec(z, j):
    cols = [pvec(z["mix_norm_g_i"]), pvec(z["conv_b_in"][j][:D]), pvec(z["conv_b_in"][j][D:]), pvec(z["conv_dw_b"][j]),
            pvec(z["conv_ln_g"][j]), pvec(z["conv_ln_b"][j]), pvec(z["conv_b_out"][j])]
    dw = z["conv_dw"][j]
    dwp = dw.reshape(CW, NCH, 128).transpose(2, 1, 0).reshape(128, NCH * CW)
    return np.ascontiguousarray(np.concatenate(cols + [dwp], axis=1).astype(np.float32))


def run_conv(xTs, halos, c, ada_w_i, ada_b_i, mix_g_i, z, j):
    nc = get_prog("conv", build_conv)
    aT = ada_rows(ada_w_i, [0, 1, 2])
    ab = ada_cols(ada_b_i, [0, 1, 2])
    zz = dict(z)
    zz["mix_norm_g_i"] = mix_g_i
    vec = conv_vec(zz, j)
    wint = tileA(z["conv_w_in"][j])
    woutt = tileA(z["conv_w_out"][j])
    ident = np.eye(128, dtype=np.float32)
    in_maps = []
    for core in range(8):
        b, t0 = core_tokens(core)
        in_maps.append({"xT": xTs[core], "xh": halos[core],
                        "hv": np.full((128, 1), 0.0 if t0 == 0 else 1.0, np.float32),
                        "cb": np.ascontiguousarray(np.broadcast_to(c[b], (128, D))),
                        "adaT": aT, "adab": ab, "vec": vec, "ident": ident, "win": wint, "wout": woutt})
    res = run_bass_kernel_spmd(nc, in_maps, core_ids=list(range(8)))
    return [r["xo"] for r in res.results]


def build_a1():
    k = KB()
    io = {"xT": k.din("xT", [D, TOK]), "ho": k.dout("ho", [D, TOK], BF16), "cb": k.din("cb", [128, D]),
          "adaT": k.din("adaT", [16, 128, D]), "adab": k.din("adab", [128, 16]), "ng": k.din("ng", [128, NCH])}
    emit_a1(k, io)
    n = k.P.finalize()
    return k.nc, n


def emit_a1(k, io):
    nc, P = k.nc, k.P
    xT, ho, cb, adaT, adab, ng = [io[n_] for n_ in ("xT", "ho", "cb", "adaT", "adab", "ng")]
    xv = xT.rearrange("(c p) t -> p c t", p=128)
    hov = ho.rearrange("(c p) t -> p c t", p=128)
    C = emit_consts(k)
    xt = [k.sb("xt%d" % i, [128, NCH, T], F32) for i in range(2)]
    Bxt = [k.bufs("xt%d_" % i, NCH) for i in range(2)]
    mod, Bmod = emit_mod(k, cb, adaT, adab, 2, xt[1], Bxt[1])
    ngt = k.sb("ngt", [128, NCH], F32)
    Bng = k.buf("ngt")
    P.dma("sp", lambda e: e.dma_start(out=ngt[:, :], in_=ng[:, :]), writes=[Bng], key=Bng)
    scale1, Bsc1 = emit_scale(k, "scale1", mod, Bmod, 8, ngt[:, :], Bng)
    N = NormCtx(k, C)
    hT = [k.sb("hT%d" % i, [128, NCH, T], BF16) for i in range(2)]
    BhT = [k.bufs("hT%d_" % i, NCH) for i in range(2)]
    for i in range(NT):
        s = i % 2
        x, Bx = xt[s], Bxt[s]
        P.dma("sp", lambda e, x=x, i=i: e.dma_start(out=x[:, :, :], in_=xv[:, :, i * T:(i + 1) * T]), writes=Bx, key=Bx[0])
        emit_norm(N, x, Bx, T, scale1, Bsc1, lambda ch: mod[:, ch:ch + 1], Bmod, hT[s], BhT[s])
        P.dma("sp", lambda e, s=s, i=i: e.dma_start(out=hov[:, :, i * T:(i + 1) * T], in_=hT[s][:, :, :]), reads=BhT[s], key=BhT[s][0])


def build_a3():
    k = KB()
    aT = k.din("aT", [D, TOK], BF16)
    av = aT.rearrange("(c p) t -> p c t", p=128)

    def a_load(P, dst, Bd, i):
        P.dma("sp", lambda e: e.dma_start(out=dst[:, :, :], in_=av[:, :, i * T:(i + 1) * T]), writes=Bd, key=Bd[0])

    io = {"xT": k.din("xT", [D, TOK]), "a_load": a_load, "xo": k.dout("xo", [D, TOK]), "cb": k.din("cb", [128, D]),
          "adaT": k.din("adaT", [8, 128, D]), "adab": k.din("adab", [128, 8]), "wo": k.din("wo", [2, 128, 4096])}
    emit_a3(k, io)
    n = k.P.finalize()
    return k.nc, n


def emit_a3(k, io):
    nc, P = k.nc, k.P
    xT, xo, cb, adaT, adab, wo = [io[n_] for n_ in ("xT", "xo", "cb", "adaT", "adab", "wo")]
    a_load = io["a_load"]
    xv = xT.rearrange("(c p) t -> p c t", p=128)
    xov = xo.rearrange("(c p) t -> p c t", p=128)
    xt = [k.sb("xt%d" % i, [128, NCH, T], F32) for i in range(2)]
    Bxt = [k.bufs("xt%d_" % i, NCH) for i in range(2)]
    mod, Bmod = emit_mod(k, cb, adaT, adab, 1, xt[1], Bxt[1])
    Wo = k.sb("Wo", [128, 2, 4096], BF16)
    BWo = k.bufs("Wo_", 2)
    for t in range(2):
        P.dma("pool", lambda e, t=t: e.dma_start(out=Wo[:, t, :], in_=wo[t, :, :]), writes=[BWo[t]], key=BWo[t])
    at = [k.sb("at%d" % i, [128, NCH, T], BF16) for i in range(2)]
    Bat = [k.bufs("at%d_" % i, NCH) for i in range(2)]
    pw = [k.bank("pw%d" % i) for i in range(2)]
    Bpw = k.bufs("pw_", 2)
    cnt = 0
    for i in range(NT):
        s = i % 2
        x, Bx = xt[s], Bxt[s]
        P.dma("sp", lambda e, x=x, i=i: e.dma_start(out=x[:, :, :], in_=xv[:, :, i * T:(i + 1) * T]), writes=Bx, key=Bx[0])
        a_load(P, at[s], Bat[s], i)
        for c in range(NCH):
            wt, m = c // 4, c % 4
            pb = cnt % 2
            cnt += 1
            for kc in range(NCH):
                P.op("pe", lambda e, wt=wt, m=m, kc=kc, pb=pb, s=s: e.matmul(
                    pw[pb][:, :], lhsT=Wo[:, wt, kc * 512 + m * 128: kc * 512 + (m + 1) * 128], rhs=at[s][:, kc, :],
                    start=(kc == 0), stop=(kc == NCH - 1)),
                     reads=[BWo[wt], Bat[s][kc]], writes=[Bpw[pb]])
            P.op("dve", lambda e, c=c, pb=pb, x=x: e.scalar_tensor_tensor(
                out=x[:, c, :], in0=pw[pb][:, :], scalar=mod[:, c:c + 1], in1=x[:, c, :], op0=ALU.mult, op1=ALU.add),
                 reads=[Bpw[pb], Bmod, Bx[c]], writes=[Bx[c]])
        P.dma("sp", lambda e, x=x, i=i: e.dma_start(out=xov[:, :, i * T:(i + 1) * T], in_=x[:, :, :]), reads=Bx, key=Bx[1])


NQS = S // T
NKT = S // 128
HG = 4


def build_a2():
    k = KB()
    hTa = k.din("hTa", [D, S], BF16)
    hv_ = hTa.rearrange("(c p) t -> p c t", p=128)
    ao = k.dout("ao", [HG * 64, S], BF16)
    io = {"h_tile": (lambda i: hv_[:, :, i * T:(i + 1) * T]),
          "ao_dst": (lambda h, qs: ao[h * 64:(h + 1) * 64, qs * T:(qs + 1) * T]),
          "wqk": k.din("wqk", [128, 4096]), "wv": k.din("wv", [128, NCH * 256]), "gqk": k.din("gqk", [128, 2]),
          "AB": k.din("AB", [128, HG * 128]), "shc": k.din("shc", [128, 16]), "kconst": k.din("kconst", [64, S], BF16),
          "cmask": k.din("cmask", [4, 128, T]), "ident": k.din("ident", [128, 128]), "bdones": k.din("bdones", [128, 128]),
          "QA": k.dint("QA", [HG, 128, S], BF16), "KA": k.dint("KA", [HG, 64, S], BF16)}
    emit_a2(k, io)
    n = k.P.finalize()
    return k.nc, n


def emit_a2(k, io):
    nc, P = k.nc, k.P
    wqk, wv, gqk, ABd, shcd, kconst, cmaskd, identd, bdd, QA, KA = [io[n_] for n_ in (
        "wqk", "wv", "gqk", "AB", "shc", "kconst", "cmask", "ident", "bdones", "QA", "KA")]
    h_tile, ao_dst = io["h_tile"], io["ao_dst"]
    BQA = k.bufs("QAd", HG)
    BKA = k.bufs("KAd", HG)

    C = emit_consts(k)
    ident = k.sb("ident_sb", [128, 128], BF16)
    Bid = k.buf("ident")
    bdo = k.sb("bdo", [128, 128], BF16)
    Bbd = k.buf("bdo")
    cmask = k.sb("cmask_sb", [128, 4, T], BF16)
    Bcm = k.buf("cmask")
    AB = k.sb("AB_sb", [128, HG * 128], F32)
    BAB = k.buf("AB")
    shc = k.sb("shc_sb", [128, 16], F32)
    Bshc = k.buf("shc")
    gq = k.sb("gqk_sb", [128, 2], F32)
    Bgq = k.buf("gqk")
    onesf = k.sb("onesf", [128, 64], F32)
    Bof = k.buf("onesf")
    Wqk = k.sb("Wqk", [128, 4096], BF16)
    BWqk = k.buf("Wqk")
    Wv = k.sb("Wv", [128, NCH * 256], BF16)
    BWv = k.buf("Wv")
    Kaug = k.sb("Kaug", [128, S], BF16)
    BKlo = k.buf("Kaug_lo")
    BKhi = k.buf("Kaug_hi")
    Vall = k.sb("Vall", [128, NKT, HG, 65], BF16)
    BV = k.bufs("Vall_", NKT)
    Bvones = k.buf("Vones")
    P.dma("pool", lambda e: e.dma_start(out=ident[:, :], in_=identd[:, :]), writes=[Bid], key=Bid)
    P.dma("pool", lambda e: e.dma_start(out=bdo[:, :], in_=bdd[:, :]), writes=[Bbd], key=Bbd)
    for d_ in range(4):
        P.dma("pool", lambda e, d_=d_: e.dma_start(out=cmask[:, d_, :], in_=cmaskd[d_, :, :]), writes=[Bcm], key=Bcm)
    P.dma("pool", lambda e: e.dma_start(out=Wqk[:, :], in_=wqk[:, :]), writes=[BWqk], key=BWqk)
    P.dma("pool", lambda e: e.dma_start(out=Wv[:, :], in_=wv[:, :]), writes=[BWv], key=BWv)
    P.dma("sp", lambda e: e.dma_start(out=AB[:, :], in_=ABd[:, :]), writes=[BAB], key=BAB)
    P.dma("sp", lambda e: e.dma_start(out=shc[:, :], in_=shcd[:, :]), writes=[Bshc], key=Bshc)
    P.dma("sp", lambda e: e.dma_start(out=gq[:, :], in_=gqk[:, :]), writes=[Bgq], key=Bgq)
    P.dma("sp", lambda e: e.dma_start(out=Kaug[64:128, :], in_=kconst[:, :]), writes=[BKhi], key=BKhi)
    P.op("pool", lambda e: e.memset(onesf[:, :], 1.0), writes=[Bof])
    P.op("pool", lambda e: e.memset(Vall[:, :, :, 64:65], 1.0), writes=[Bvones])

    banks = [k.bank("bk%d" % i) for i in range(8)]
    Bb = k.bufs("bk_", 8)

    ht = [k.sb("ht%d" % i, [128, NCH, T], BF16) for i in range(2)]
    Bht = k.bufs("ht_", 2)
    sqb = k.sb("sqb", [128, T], BF16)
    Bsqb = k.buf("sqb")
    sd = k.sb("sd", [128, T], F32)
    Bsd = k.buf("sd")
    knf = k.sb("knf", [128, T], F32)
    Bknf = k.buf("knf")
    kb = [k.sb("kb%d" % i, [128, T], BF16) for i in range(2)]
    Bkb = k.bufs("kb_", 2)
    qnf = [k.sb("qnf%d" % i, [128, T], F32) for i in range(2)]
    Bqnf = k.bufs("qnf_", 2)
    kmT = [k.sb("kmT%d" % i, [128, 64], F32) for i in range(2)]
    Bkm = k.bufs("kmT_", 2)
    QAt = [k.sb("QAt%d" % i, [128, T], BF16) for i in range(2)]
    BQAt = k.bufs("QAt_", 2)
    mb = [k.sb("mb%d" % i, [128, 64], F32) for i in range(2)]
    Bmb = k.bufs("mb_", 2)
    gp = [k.sb("gp%d" % i, [128, 64], F32) for i in range(2)]
    Bgp = k.bufs("gp_", 2)
    top8 = [k.sb("top8_%d" % i, [128, 8], F32) for i in range(2)]
    Btop = k.bufs("top8_", 2)
    Mbb = [k.sb("Mbb%d" % i, [128, 128], BF16) for i in range(2)]
    BMbb = k.bufs("Mbb_", 2)
    for r in range(2):
        P.op("pool", lambda e, r=r: e.memset(Mbb[r][:, :], 0.0), writes=[BMbb[r]])
        P.op("pool", lambda e, r=r: e.memset(kmT[r][:, :], 0.0), writes=[Bkm[r]])
    pkc = 0
    rr = 0
    qac = 0
    for i in range(NQS):
        s = i % 2
        P.dma("sp", lambda e, s=s, i=i: e.dma_start(out=ht[s][:, :, :], in_=h_tile(i)), writes=[Bht[s]], key=Bht[s])
        for isk in (1, 0):
            for hp in range(2):
                m = isk * 2 + hp
                pb = pkc % 2
                pkc += 1
                for kc in range(NCH):
                    P.op("pe", lambda e, m=m, kc=kc, pb=pb, s=s: e.matmul(
                        banks[pb][:, :], lhsT=Wqk[:, kc * 512 + m * 128: kc * 512 + (m + 1) * 128], rhs=ht[s][:, kc, :],
                        start=(kc == 0), stop=(kc == NCH - 1)), reads=[BWqk, Bht[s]], writes=[Bb[pb]])
                P.op("act", lambda e, pb=pb: e.activation(out=sqb[:, :], in_=banks[pb][:, :], func=AF.Square), reads=[Bb[pb]], writes=[Bsqb])
                P.op("pe", lambda e: e.matmul(banks[2][:, :], lhsT=bdo[:, :], rhs=sqb[:, :], start=True, stop=True),
                     reads=[Bbd, Bsqb], writes=[Bb[2]])
                P.op("act", lambda e: e.activation(out=sd[:, :], in_=banks[2][:, :], func=AF.Sqrt, bias=C["eps"][:, 0:1], scale=1.0 / 64),
                     reads=[Bb[2], C["Beps"]], writes=[Bsd])
                P.op("dve", lambda e: e.reciprocal(out=sd[:, :], in_=sd[:, :]), reads=[Bsd], writes=[Bsd])
                if isk:
                    P.op("dve", lambda e, pb=pb: e.scalar_tensor_tensor(out=knf[:, :], in0=banks[pb][:, :], scalar=gq[:, 1:2], in1=sd[:, :],
                                                                         op0=ALU.mult, op1=ALU.mult),
                         reads=[Bb[pb], Bgq, Bsd], writes=[Bknf])
                    kbs = pkc % 2
                    P.op("act", lambda e, kbs=kbs: e.activation(out=kb[kbs][:, :], in_=knf[:, :], func=AF.Identity),
                         reads=[Bknf], writes=[Bkb[kbs]])
                    for hb in range(2):
                        h = 2 * hp + hb
                        P.dma("pool", lambda e, h=h, hb=hb, kbs=kbs, i=i: e.dma_start(
                            out=KA[h, :, i * T:(i + 1) * T], in_=kb[kbs][hb * 64:(hb + 1) * 64, :]),
                              reads=[Bkb[kbs]], writes=[BKA[h]], key=Bkb[kbs])
                    P.op("dve", lambda e, hp=hp, i=i: e.tensor_reduce(out=kmT[hp][:, 2 * i:2 * i + 2],
                                                                       in_=knf[:, :].rearrange("p (b t) -> p b t", b=2),
                                                                       axis=AX.X, op=ALU.add),
                         reads=[Bknf], writes=[Bkm[hp]])
                else:
                    P.op("dve", lambda e, pb=pb, hp=hp: e.scalar_tensor_tensor(out=qnf[hp][:, :], in0=banks[pb][:, :], scalar=gq[:, 0:1],
                                                                                in1=sd[:, :], op0=ALU.mult, op1=ALU.mult),
                         reads=[Bb[pb], Bgq, Bsd], writes=[Bqnf[hp]])
        for h in range(HG):
            hp, hb = h // 2, h % 2
            p0 = hb * 64
            qs_ = qac % 2
            qac += 1
            P.op("dve", lambda e, hp=hp, p0=p0, qs_=qs_: e.tensor_copy(out=QAt[qs_][0:64, :], in_=qnf[hp][p0:p0 + 64, :]),
                 reads=[Bqnf[hp]], writes=[BQAt[qs_]])
            for js in range(4):
                m = 2 * i + js // 2
                r = rr % 2
                rr += 1
                P.op("pool", lambda e, r=r: e.memset(mb[r][:, :], NEG), writes=[Bmb[r]])
                if m >= 3:
                    P.op("pe", lambda e, hp=hp, p0=p0, js=js: e.matmul(
                        banks[3][:, 0:64], lhsT=qnf[hp][p0:p0 + 64, js * 128:(js + 1) * 128], rhs=kmT[hp][p0:p0 + 64, 0:64],
                        start=True, stop=True), reads=[Bqnf[hp], Bkm[hp]], writes=[Bb[3]])
                    P.op("pool", lambda e, r=r: e.memset(gp[r][:, :], -1e30), writes=[Bgp[r]])
                    P.op("dve", lambda e, r=r, m=m: e.tensor_copy(out=gp[r][:, 0:m], in_=banks[3][:, 0:m]), reads=[Bb[3]], writes=[Bgp[r]])
                    P.op("dve", lambda e, r=r, m=m: e.max(out=top8[r][:, :], in_=gp[r][:, 0:max(m, 8)]), reads=[Bgp[r]], writes=[Btop[r]])
                    P.op("dve", lambda e, r=r, m=m: e.tensor_scalar(out=mb[r][:, 0:m], in0=gp[r][:, 0:m], scalar1=top8[r][:, 2:3], scalar2=1.0,
                                                                    op0=ALU.is_ge, op1=ALU.subtract),
                         reads=[Bgp[r], Btop[r]], writes=[Bmb[r]])
                    P.op("dve", lambda e, r=r, m=m: e.tensor_scalar(out=mb[r][:, 0:m], in0=mb[r][:, 0:m], scalar1=-NEG, scalar2=None,
                                                                    op0=ALU.mult), reads=[Bmb[r]], writes=[Bmb[r]])
                lo = 0 if m < 3 else m
                P.op("dve", lambda e, r=r, lo=lo, m=m: e.memset(mb[r][:, lo:m + 1], 0.0), reads=[Bmb[r]], writes=[Bmb[r]])
                P.op("dve", lambda e, r=r, js=js, h=h: e.tensor_copy(out=mb[r][:, 63:64], in_=shc[:, js * 4 + h:js * 4 + h + 1]),
                     reads=[Bmb[r], Bshc], writes=[Bmb[r]])
                P.op("dve", lambda e, r=r: e.tensor_copy(out=Mbb[r][:, 64:128], in_=mb[r][:, :]), reads=[Bmb[r]], writes=[BMbb[r]])
                P.op("pe", lambda e, r=r: e.matmul(banks[4][:, 0:128], lhsT=Mbb[r][:, :], rhs=ident[:, :], start=True, stop=True),
                     reads=[BMbb[r], Bid], writes=[Bb[4]])
                P.op("act", lambda e, qs_=qs_, js=js: e.activation(out=QAt[qs_][64:128, js * 128:(js + 1) * 128], in_=banks[4][64:128, 0:128],
                                                                    func=AF.Identity), reads=[Bb[4]], writes=[BQAt[qs_]])
            P.dma("pool", lambda e, h=h, qs_=qs_, i=i: e.dma_start(out=QA[h, :, i * T:(i + 1) * T], in_=QAt[qs_][:, :]),
                  reads=[BQAt[qs_]], writes=[BQA[h]], key=BQAt[qs_])
        for js in range(4):
            kt = 4 * i + js
            for kc in range(NCH):
                P.op("pe", lambda e, kc=kc, js=js, s=s: e.matmul(
                    banks[5][:, 0:256], lhsT=ht[s][:, kc, js * 128:(js + 1) * 128], rhs=Wv[:, kc * 256:(kc + 1) * 256],
                    start=(kc == 0), stop=(kc == NCH - 1)), reads=[Bht[s], BWv], writes=[Bb[5]])
            P.op("act", lambda e, kt=kt: e.activation(out=Vall[:, kt, :, 0:64], in_=banks[5][:, 0:256].rearrange("p (h d) -> p h d", h=HG),
                                                       func=AF.Identity), reads=[Bb[5], Bvones], writes=[BV[kt]])

    LA = 3
    NPT = 6
    SBK = [0, 1, 3, 4]
    qa = [k.sb("qa%d" % i, [128, T], BF16) for i in range(2)]
    Bqa = k.bufs("qa_", 2)
    pT = [k.sb("pT%d" % i, [128, T], BF16) for i in range(NPT)]
    BpT = k.bufs("pT_", NPT)
    rc = k.sb("rc", [128, T], F32)
    Brc = k.buf("rc")
    bc = k.sb("bc", [64, T], F32)
    Bbc = k.buf("bc")
    aot = [k.sb("aot%d" % i, [64, T], BF16) for i in range(2)]
    Baot = k.bufs("aot_", 2)
    st = {"fin": 0}
    for h in range(HG):
        P.dma("sp", lambda e, h=h: e.dma_start(out=Kaug[0:64, :], in_=KA[h, :, :]), reads=[BKA[h]], writes=[BKlo], key=BKlo)
        units = [(qs, kt) for qs in range(NQS) for kt in range(4 * qs + 4)]
        NU = len(units)
        pend = []

        def emit_qk(u, h=h):
            qs, kt = units[u]
            sl = qs % 2
            if kt == 0:
                P.dma("sp", lambda e: e.dma_start(out=qa[sl][:, :], in_=QA[h, :, qs * T:(qs + 1) * T]),
                      reads=[BQA[h]], writes=[Bqa[sl]], key=Bqa[sl])
            sb_ = SBK[u % 4]
            pt_ = u % NPT
            dk = kt - 4 * qs
            P.op("pe", lambda e: e.matmul(banks[sb_][:, :], lhsT=Kaug[:, kt * 128:(kt + 1) * 128], rhs=qa[sl][:, :],
                                          start=True, stop=(dk < 0)),
                 reads=[BKlo, BKhi, Bqa[sl]], writes=[Bb[sb_]])
            if dk >= 0:
                P.op("pe", lambda e: e.matmul(banks[sb_][:, :], lhsT=ident[:, :], rhs=cmask[:, dk, :], start=False, stop=True),
                     reads=[Bid, Bcm], writes=[Bb[sb_]])
            ri = dk + 124
            P.op("act", lambda e: e.activation(out=pT[pt_][:, :], in_=banks[sb_][:, :], func=AF.Exp,
                                               bias=AB[:, h * 128 + ri:h * 128 + ri + 1], scale=0.125),
                 reads=[Bb[sb_], BAB], writes=[BpT[pt_]])

        def emit_fin(qs, h=h):
            ob = 6 + qs % 2
            ar = st["fin"] % 2
            st["fin"] += 1
            P.op("dve", lambda e: e.reciprocal(out=rc[64:65, :], in_=banks[ob][64:65, :]), reads=[Bb[ob]], writes=[Brc])
            P.op("pe", lambda e: e.matmul(banks[2][0:64, :], lhsT=onesf[64:65, 0:64], rhs=rc[64:65, :], start=True, stop=True),
                 reads=[Bof, Brc], writes=[Bb[2]])
            P.op("act", lambda e: e.activation(out=bc[:, :], in_=banks[2][0:64, :], func=AF.Identity), reads=[Bb[2]], writes=[Bbc])
            P.op("dve", lambda e: e.tensor_tensor(out=aot[ar][:, :], in0=banks[ob][0:64, :], in1=bc[:, :], op=ALU.mult),
                 reads=[Bb[ob], Bbc], writes=[Baot[ar]])
            P.dma("pool", lambda e: e.dma_start(out=ao_dst(h, qs), in_=aot[ar][:, :]), reads=[Baot[ar]], key=Baot[ar])

        def emit_pv(u, step, h=h):
            qs, kt = units[u]
            ob = 6 + qs % 2
            pt_ = u % NPT
            nk = 4 * qs + 4
            P.op("pe", lambda e: e.matmul(banks[ob][0:65, :], lhsT=Vall[:, kt, h, :], rhs=pT[pt_][:, :],
                                          start=(kt == 0), stop=(kt == nk - 1)),
                 reads=[BV[kt], Bvones, BpT[pt_]], writes=[Bb[ob]])
            if kt == nk - 1:
                pend.append((step + 2, qs))

        for step in range(NU + LA):
            if step < NU:
                emit_qk(step)
            if step >= LA:
                emit_pv(step - LA, step)
            while pend and pend[0][0] <= step:
                emit_fin(pend.pop(0)[1])
        while pend:
            emit_fin(pend.pop(0)[1])


def _bf16():
    import ml_dtypes
    return ml_dtypes.bfloat16


def a2_consts(g):
    slopes = (2.0 ** (-8.0 * (np.arange(16) + 1) / 16)).astype(np.float32)
    p = np.arange(128, dtype=np.float32)[:, None]
    ri = np.arange(128, dtype=np.float32)[None, :]
    AB = np.zeros((128, HG * 128), np.float32)
    shc = np.zeros((128, 16), np.float32)
    for hl in range(HG):
        sl = slopes[4 * g + hl]
        AB[:, hl * 128:(hl + 1) * 128] = sl * (p + 128.0 * (ri - 124.0))
        for js in range(4):
            shc[:, js * 4 + hl] = -8.0 * sl * (js * 128.0 + p[:, 0])
    return AB, shc


def a2_static():
    kc = np.zeros((64, S), np.float32)
    for n in range(63):
        kc[n, n * 256:(n + 1) * 256] = 1.0
    kc[63, :] = 1.0
    cm = np.zeros((4, 128, T), np.float32)
    col = np.arange(T)[None, :]
    for dk in range(4):
        kp = dk * 128 + np.arange(128)[:, None]
        kb_, qb_ = kp // 256, col // 256
        cm[dk] = np.where(((kb_ == qb_) & (col < kp)) | (kb_ > qb_), NEG, 0.0)
    bd = np.zeros((128, 128), np.float32)
    bd[:64, :64] = 1.0
    bd[64:, 64:] = 1.0
    return kc.astype(_bf16()), cm, bd


def run_a1(xTs, c, ada_w_i, ada_b_i, ng_i):
    nc = get_prog("a1", build_a1)
    aT = ada_rows(ada_w_i, [0, 1])
    ab = ada_cols(ada_b_i, [0, 1])
    ngp = pvec(ng_i)
    in_maps = []
    for core in range(8):
        b, _ = core_tokens(core)
        in_maps.append({"xT": xTs[core], "cb": np.ascontiguousarray(np.broadcast_to(c[b], (128, D))),
                        "adaT": aT, "adab": ab, "ng": ngp})
    res = run_bass_kernel_spmd(nc, in_maps, core_ids=list(range(8)))
    return [r["ho"] for r in res.results]


def run_a2(hTas, wqkv, gqn, gkn):
    nc = get_prog("a2", build_a2)
    kc, cm, bd = a2_static()
    ident = np.eye(128, dtype=np.float32)
    in_maps = []
    for core in range(8):
        b, g = core // 4, core % 4
        Wq = wqkv[:, g * 256:(g + 1) * 256]
        Wk = wqkv[:, D + g * 256:D + (g + 1) * 256]
        Wv = wqkv[:, 2 * D + g * 256:2 * D + (g + 1) * 256]
        wqk = tileA(np.concatenate([Wq, Wk], axis=1))[0]
        wv = np.ascontiguousarray(Wv.reshape(8, 128, 256).transpose(1, 0, 2).reshape(128, 2048))
        gqk = np.ascontiguousarray(np.stack([np.tile(gqn, 2), np.tile(gkn, 2)], axis=1).astype(np.float32))
        AB, shc = a2_consts(g)
        in_maps.append({"hTa": hTas[b], "wqk": wqk, "wv": wv, "gqk": gqk, "AB": AB, "shc": shc, "kconst": kc,
                        "cmask": cm, "ident": ident, "bdones": bd})
    res = run_bass_kernel_spmd(nc, in_maps, core_ids=list(range(8)))
    return [r["ao"] for r in res.results]


def run_a3(xTs, aTs, c, ada_w_i, ada_b_i, wo_i):
    nc = get_prog("a3", build_a3)
    aT = ada_rows(ada_w_i, [2])
    ab = ada_cols(ada_b_i, [2])
    wot = tileA(wo_i)
    in_maps = []
    for core in range(8):
        b, _ = core_tokens(core)
        in_maps.append({"xT": xTs[core], "aT": aTs[core], "cb": np.ascontiguousarray(np.broadcast_to(c[b], (128, D))),
                        "adaT": aT, "adab": ab, "wo": wot})
    res = run_bass_kernel_spmd(nc, in_maps, core_ids=list(range(8)))
    return [r["xo"] for r in res.results]


def build_fused(upto=4):
    k = KB(fused=True)
    nc, P = k.nc, k.P
    x0 = k.din("xT", [D, TOK])
    xh0 = k.din("xh", [D, HALO])
    hv = k.din("hv", [128, 1])
    cb = k.din("cb", [128, D])
    identd = k.din("ident", [128, 128])
    ABd = k.din("AB", [128, HG * 128])
    shcd = k.din("shc", [128, 16])
    kconst = k.din("kconst", [64, S], BF16)
    cmaskd = k.din("cmask", [4, 128, T])
    bdd = k.din("bdones", [128, 128])
    adaT = [k.din("adaT%d" % i, [48, 128, D]) for i in range(4)]
    adab = [k.din("adab%d" % i, [128, 48]) for i in range(4)]
    ngm = [k.din("ngm%d" % i, [128, NCH]) for i in range(4)]
    w1 = [k.din("w1_%d" % i, [8, 128, 4096]) for i in range(4)]
    w2 = [k.din("w2_%d" % i, [8, 128, 4096]) for i in range(4)]
    vec = [k.din("vec%d" % j, [128, NVC]) for j in range(2)]
    win = [k.din("win%d" % j, [4, 128, 4096]) for j in range(2)]
    wout = [k.din("wout%d" % j, [2, 128, 4096]) for j in range(2)]
    nga = [k.din("nga%d" % j, [128, NCH]) for j in range(2)]
    wqk = [k.din("wqk%d" % j, [128, 4096]) for j in range(2)]
    wv = [k.din("wv%d" % j, [128, NCH * 256]) for j in range(2)]
    gqk = [k.din("gqk%d" % j, [128, 2]) for j in range(2)]
    wo = [k.din("wo%d" % j, [2, 128, 4096]) for j in range(2)]
    xo = k.dout("xo", [D, TOK])
    xa = k.dint("xa", [D, TOK])
    xb = k.dint("xb", [D, TOK])
    hown = k.dint("hown", [D, TOK], BF16)
    hG = k.dint("hG", [4 * D, TOK], BF16)
    aown = k.dint("aown", [4 * 256, TOK], BF16)
    aoG = k.dint("aoG", [4 * D, TOK], BF16)
    amine = k.dint("amine", [D, TOK], BF16)
    hl = k.dint("hl", [D, HALO])
    hlG = k.dint("hlG", [8 * D, HALO])
    QA = k.dint("QA", [HG, 128, S], BF16)
    KA = k.dint("KA", [HG, 64, S], BF16)
    npass = [0]
    dyn = {}

    def newpass(name):
        P.barrier()
        k.begin_pass("p%d%s_" % (npass[0], name))
        npass[0] += 1

    def mlp(i, xin, xout):
        newpass("mlp")
        emit_mlp(k, {"xT": xin, "xo": xout, "cb": cb, "adaT": adaT[i][24:48], "adab": adab[i][:, 24:48], "ng": ngm[i],
                     "w1": w1[i], "w2": w2[i]})

    def conv(i, j, xin, xout, xh_fn):
        newpass("conv")
        emit_conv(k, {"xT": xin, "xh_fn": xh_fn, "hv": hv, "xo": xout, "cb": cb, "adaT": adaT[i][0:24], "adab": adab[i][:, 0:24],
                      "vec": vec[j], "ident": identd, "win": win[j], "wout": wout[j]})

    def attn(i, j, xin, xout):
        newpass("a1")
        emit_a1(k, {"xT": xin, "ho": hown, "cb": cb, "adaT": adaT[i][0:16], "adab": adab[i][:, 0:16], "ng": nga[j]})
        newpass("ag1")
        for c_ in range(NCH):
            Bg = k.buf("hG%d" % c_)
            P.dma("pool", lambda e, c_=c_: e.collective_compute(
                "AllGather", ALU.bypass, replica_groups=[[0, 1, 2, 3], [4, 5, 6, 7]],
                ins=[hown[c_ * 128:(c_ + 1) * 128, :]], outs=[hG[c_ * 512:(c_ + 1) * 512, :]]), writes=[Bg], key=Bg, inc=1)
        newpass("a2")

        hGv = hG.rearrange("(c r p) t -> r p c t", c=NCH, r=4, p=128)

        def h_tile(ti):
            r, cc = ti // NT, ti % NT
            return hGv[r][:, :, cc * T:(cc + 1) * T]

        def ao_dst(h, qs):
            q, cc = qs // NT, qs % NT
            return aown[q * 256 + h * 64:q * 256 + (h + 1) * 64, cc * T:(cc + 1) * T]

        emit_a2(k, {"h_tile": h_tile, "ao_dst": ao_dst, "wqk": wqk[j], "wv": wv[j], "gqk": gqk[j], "AB": ABd, "shc": shcd,
                    "kconst": kconst, "cmask": cmaskd, "ident": identd, "bdones": bdd, "QA": QA, "KA": KA})
        newpass("ag2")
        for m_ in range(8):
            Bg2 = k.buf("aoG%d" % m_)
            P.dma("pool", lambda e, m_=m_: e.collective_compute(
                "AllGather", ALU.bypass, replica_groups=[[0, 1, 2, 3], [4, 5, 6, 7]],
                ins=[aown[m_ * 128:(m_ + 1) * 128, :]], outs=[aoG[m_ * 512:(m_ + 1) * 512, :]]), writes=[Bg2], key=Bg2, inc=1)
        newpass("a3")

        Bam = k.buf("amine")
        for g_ in range(4):
            for fh in range(2):
                def fn(e, g_=g_, fh=fh):
                    if "q1024" not in dyn:
                        dyn["q1024"] = e.snap((e.partition_id() % 4) * 1024)
                    base = fh * 512 + g_ * 128
                    return e.dma_start(out=amine[(g_ * 2 + fh) * 128:(g_ * 2 + fh + 1) * 128, :],
                                       in_=aoG[base:base + 3200, :][bass.ds(dyn["q1024"], 128), :])
                P.dma("sp", fn, writes=[Bam], key=Bam)
        amv = amine.rearrange("(c p) t -> p c t", p=128)

        def a_load(P_, dst, Bd, ti):
            P_.dma("sp", lambda e: e.dma_start(out=dst[:, :, :], in_=amv[:, :, ti * T:(ti + 1) * T]), reads=[Bam], writes=Bd, key=Bd[0])

        emit_a3(k, {"xT": xin, "a_load": a_load, "xo": xout, "cb": cb, "adaT": adaT[i][16:24], "adab": adab[i][:, 16:24], "wo": wo[j]})

    conv(0, 0, x0, xa, lambda e: xh0.rearrange("(c p) t -> p c t", p=128))
    mlp(0, xa, xb if upto > 1 else xo)
    if upto == 1:
        n = P.finalize()
        return nc, n
    attn(1, 0, xb, xa)
    mlp(1, xa, xb if upto > 2 else xo)
    if upto == 2:
        n = P.finalize()
        return nc, n
    newpass("halo")
    Bhl = k.buf("hl")
    BhlG = k.buf("hlG")
    P.dma("sp", lambda e: e.dma_start(out=hl[:, :], in_=xb[:, TOK - HALO:TOK]), writes=[Bhl], key=Bhl)
    P.dma("pool", lambda e: e.collective_compute("AllGather", ALU.bypass, replica_groups=[list(range(8))],
                                                  ins=[hl[:, :]], outs=[hlG[:, :]]), reads=[Bhl], writes=[BhlG], key=BhlG, inc=1)
    def xh2(e):
        if "prev" not in dyn:
            dyn["prev"] = e.snap(((e.partition_id() + 7) % 8) * D)
        return hlG[bass.ds(dyn["prev"], D), :].rearrange("(c p) t -> p c t", p=128)

    conv(2, 1, xb, xa, xh2)
    mlp(2, xa, xb)
    attn(3, 1, xb, xa)
    mlp(3, xa, xo)
    n = P.finalize()
    return nc, n


def kernel(x, c, ada_w, ada_b, mix_norm_g, mlp_norm_g,
           conv_w_in, conv_b_in, conv_dw, conv_dw_b, conv_ln_g, conv_ln_b, conv_w_out, conv_b_out,
           attn_w_qkv, attn_q_norm_g, attn_k_norm_g, attn_w_o, mlp_w1, mlp_w2):
    f = lambda a: np.asarray(a, dtype=np.float32)
    x, c, ada_w, ada_b = f(x), f(c), f(ada_w), f(ada_b)
    z = {"conv_w_in": f(conv_w_in), "conv_b_in": f(conv_b_in), "conv_dw": f(conv_dw), "conv_dw_b": f(conv_dw_b),
         "conv_ln_g": f(conv_ln_g), "conv_ln_b": f(conv_ln_b), "conv_w_out": f(conv_w_out), "conv_b_out": f(conv_b_out)}
    mix_norm_g, mlp_norm_g = f(mix_norm_g), f(mlp_norm_g)
    attn_w_qkv, attn_w_o = f(attn_w_qkv), f(attn_w_o)
    gqn, gkn = f(attn_q_norm_g), f(attn_k_norm_g)
    mlp_w1, mlp_w2 = f(mlp_w1), f(mlp_w2)
    nc = get_prog("fused%d" % UPTO, lambda: build_fused(UPTO))
    kc, cm, bd = a2_static()
    ident = np.eye(128, dtype=np.float32)
    shared = {"ident": ident, "kconst": kc, "cmask": cm, "bdones": bd}
    for i in range(4):
        shared["adaT%d" % i] = ada_rows(ada_w[i], [0, 1, 2, 3, 4, 5])
        shared["adab%d" % i] = ada_cols(ada_b[i], [0, 1, 2, 3, 4, 5])
        shared["ngm%d" % i] = pvec(mlp_norm_g[i])
        shared["w1_%d" % i] = tileA(mlp_w1[i])
        shared["w2_%d" % i] = tileB(mlp_w2[i])
    for j in range(2):
        zz = dict(z)
        zz["mix_norm_g_i"] = mix_norm_g[2 * j]
        shared["vec%d" % j] = conv_vec(zz, j)
        shared["win%d" % j] = tileA(z["conv_w_in"][j])
        shared["wout%d" % j] = tileA(z["conv_w_out"][j])
        shared["nga%d" % j] = pvec(mix_norm_g[2 * j + 1])
        shared["gqk%d" % j] = np.ascontiguousarray(np.stack([np.tile(gqn[j], 2), np.tile(gkn[j], 2)], axis=1).astype(np.float32))
        shared["wo%d" % j] = tileA(attn_w_o[j])
    per_g = []
    for g in range(4):
        d_ = {}
        AB, shc = a2_consts(g)
        d_["AB"], d_["shc"] = AB, shc
        for j in range(2):
            W = attn_w_qkv[j]
            Wq = W[:, g * 256:(g + 1) * 256]
            Wk = W[:, D + g * 256:D + (g + 1) * 256]
            Wv = W[:, 2 * D + g * 256:2 * D + (g + 1) * 256]
            d_["wqk%d" % j] = tileA(np.concatenate([Wq, Wk], axis=1))[0]
            d_["wv%d" % j] = np.ascontiguousarray(Wv.reshape(8, 128, 256).transpose(1, 0, 2).reshape(128, 2048))
        per_g.append(d_)
    in_maps = []
    for core in range(8):
        b, q = core // 4, core % 4
        m = dict(shared)
        m.update(per_g[q])
        m["xT"] = np.ascontiguousarray(x[b, q * TOK:(q + 1) * TOK].T)
        m["xh"] = np.ascontiguousarray(x[b, q * TOK - HALO:q * TOK].T) if q > 0 else np.zeros((D, HALO), np.float32)
        m["hv"] = np.full((128, 1), 0.0 if q == 0 else 1.0, np.float32)
        m["cb"] = np.ascontiguousarray(np.broadcast_to(c[b], (128, D)))
        in_maps.append(m)
    res = run_bass_kernel_spmd(nc, in_maps, core_ids=list(range(8)))
    out = np.empty((NB, S, D), np.float32)
    for core in range(8):
        b, q = core // 4, core % 4
        out[b, q * TOK:(q + 1) * TOK] = res.results[core]["xo"].T
    return out


def kernel_unfused(x, c, ada_w, ada_b, mix_norm_g, mlp_norm_g,
           conv_w_in, conv_b_in, conv_dw, conv_dw_b, conv_ln_g, conv_ln_b, conv_w_out, conv_b_out,
           attn_w_qkv, attn_q_norm_g, attn_k_norm_g, attn_w_o, mlp_w1, mlp_w2):
    f = lambda a: np.asarray(a, dtype=np.float32)
    x, c, ada_w, ada_b = f(x), f(c), f(ada_w), f(ada_b)
    z = {"conv_w_in": f(conv_w_in), "conv_b_in": f(conv_b_in), "conv_dw": f(conv_dw), "conv_dw_b": f(conv_dw_b),
         "conv_ln_g": f(conv_ln_g), "conv_ln_b": f(conv_ln_b), "conv_w_out": f(conv_w_out), "conv_b_out": f(conv_b_out)}
    mix_norm_g, mlp_norm_g = f(mix_norm_g), f(mlp_norm_g)
    attn_w_qkv, attn_w_o = f(attn_w_qkv), f(attn_w_o)
    gqn, gkn = f(attn_q_norm_g), f(attn_k_norm_g)
    mlp_w1, mlp_w2 = f(mlp_w1), f(mlp_w2)
    xTs = [np.ascontiguousarray(x[core // 4, (core % 4) * TOK:(core % 4 + 1) * TOK].T) for core in range(8)]
    for i in range(4):
        j = i // 2
        if i % 2 == 0:
            halos = []
            for core in range(8):
                if core % 4 == 0:
                    halos.append(np.zeros((D, HALO), np.float32))
                else:
                    halos.append(np.ascontiguousarray(xTs[core - 1][:, TOK - HALO:]))
            xTs = run_conv(xTs, halos, c, ada_w[i], ada_b[i], mix_norm_g[i], z, j)
        else:
            hos = run_a1(xTs, c, ada_w[i], ada_b[i], mix_norm_g[i])
            hTas = [np.ascontiguousarray(np.concatenate(hos[4 * b:4 * b + 4], axis=1)) for b in range(NB)]
            aos = run_a2(hTas, attn_w_qkv[j], gqn[j], gkn[j])
            aTs = []
            for core in range(8):
                b, q = core // 4, core % 4
                aTs.append(np.ascontiguousarray(np.concatenate([aos[4 * b + g][:, q * TOK:(q + 1) * TOK] for g in range(4)], axis=0)))
            xTs = run_a3(xTs, aTs, c, ada_w[i], ada_b[i], attn_w_o[j])
        xTs = run_mlp(xTs, c, ada_w[i], ada_b[i], mlp_norm_g[i], mlp_w1[i], mlp_w2[i])
    out = np.empty((NB, S, D), np.float32)
    for core in range(8):
        b, q = core // 4, core % 4
        out[b, q * TOK:(q + 1) * TOK] = xTs[core].T
    return out
```

```python
import numpy as np
import concourse.bass as bass
import concourse.mybir as mybir
from concourse.bass_utils import run_bass_kernel_spmd

F32 = mybir.dt.float32
BF16 = mybir.dt.bfloat16
ALU = mybir.AluOpType
AF = mybir.ActivationFunctionType
AX = mybir.AxisListType

EPOCH = 20000
ENGS = ("pe", "act", "dve", "pool", "sp")

D = 1024
NCH = 8
S = 16384
NB = 2
TOK = 4096
T = 512
NT = TOK // T
CW = 31
HALO = CW - 1
EPS = 1e-6
NEG = -30000.0


class Buf:
    __slots__ = ("name", "w", "r", "dcount")

    def __init__(self, name):
        self.name = name
        self.w = None
        self.r = []
        self.dcount = 0


class Op:
    __slots__ = ("eng", "fn", "reads", "writes", "idx", "waits", "sig", "key", "ev", "inc")

    def __init__(self, eng, fn, reads, writes, key=None, inc=16):
        self.inc = inc
        self.eng = eng
        self.fn = fn
        self.reads = reads
        self.writes = writes
        self.key = key
        self.waits = []
        self.sig = False
        self.ev = None


class Prog:
    def __init__(self, nc):
        self.nc = nc
        self.ops = []
        self.sems = {}

    def op(self, eng, fn, reads=(), writes=()):
        o = Op(eng, fn, tuple(reads), tuple(writes))
        self.ops.append(o)
        return o

    def dma(self, q, fn, reads=(), writes=(), key=None, inc=16):
        o = Op(q, fn, tuple(reads), tuple(writes), key=key, inc=inc)
        self.ops.append(o)
        return o

    def barrier(self):
        o = Op(None, None, (), ())
        self.ops.append(o)
        return o

    def _sem(self, name):
        if name not in self.sems:
            self.sems[name] = self.nc.alloc_semaphore(name=name)
        return self.sems[name]

    def finalize(self, final_eng="sp"):
        nc = self.nc
        for i, o in enumerate(self.ops):
            o.idx = i
        deps_of = []
        last_op = {}
        bar_ops = {}
        for o in self.ops:
            if o.eng is None:
                bar_ops[o.idx] = dict(last_op)
                for d in last_op.values():
                    if d.key is None:
                        d.sig = True
                deps_of.append(([], []))
                continue
            deps = set()
            for b in o.reads:
                if b.w is not None:
                    deps.add(b.w)
            for b in o.writes:
                if b.w is not None:
                    deps.add(b.w)
                for r in b.r:
                    deps.add(r)
            deps.discard(o)
            for b in o.reads:
                b.r.append(o)
            for b in o.writes:
                b.w = o
                b.r = []
            best = {}
            dma_deps = []
            for d in deps:
                if d.key is not None:
                    dma_deps.append(d)
                else:
                    if d.eng == "pe" and o.eng == "pe":
                        continue
                    if d.eng not in best or best[d.eng].idx < d.idx:
                        best[d.eng] = d
            deps_of.append((list(best.values()), dma_deps))
            for d in best.values():
                d.sig = True
            if o.key is None:
                last_op[o.eng] = o
        cnt = {e: 0 for e in ENGS}
        slot_total = []
        free_slots = []
        used_slots = []
        buf_slot = {}
        for o in self.ops:
            if o.eng is None:
                free_slots.extend(used_slots)
                used_slots = []
                continue
            if o.key is not None:
                b = o.key
                if id(b) not in buf_slot:
                    if free_slots:
                        sl = free_slots.pop()
                    else:
                        sl = len(slot_total)
                        slot_total.append(0)
                    buf_slot[id(b)] = sl
                    used_slots.append(sl)
                sl = buf_slot[id(b)]
                slot_total[sl] += o.inc
                o.ev = ("dma_%d" % sl, slot_total[sl])
            elif o.sig:
                c = cnt[o.eng]
                cnt[o.eng] = c + 1
                o.ev = ("%s_%d" % (o.eng, c // EPOCH), c % EPOCH + 1)
        running = {}
        known = {e: {} for e in ENGS}
        pending = {e: None for e in ENGS}
        bar_snap = {}
        for o, (cdeps, ddeps) in zip(self.ops, deps_of):
            if o.eng is None:
                bar_snap[o.idx] = dict(running)
                for e in ENGS:
                    pending[e] = o.idx
                continue
            w = {}
            if pending[o.eng] is not None:
                bi = pending[o.eng]
                pending[o.eng] = None
                for d in bar_ops[bi].values():
                    if d.ev is not None and d.key is None:
                        s_, v_ = d.ev
                        if w.get(s_, 0) < v_:
                            w[s_] = v_
                for s_, v_ in bar_snap[bi].items():
                    if w.get(s_, 0) < v_:
                        w[s_] = v_
            for d in cdeps:
                s_, v_ = d.ev
                if w.get(s_, 0) < v_:
                    w[s_] = v_
            for d in ddeps:
                s_ = d.ev[0]
                v_ = running[s_]
                if w.get(s_, 0) < v_:
                    w[s_] = v_
            kn = known[o.eng]
            for s_, v_ in w.items():
                if kn.get(s_, 0) < v_:
                    kn[s_] = v_
                    o.waits.append((s_, v_))
            if o.key is not None:
                running[o.ev[0]] = o.ev[1]
        for o in self.ops:
            for s_, v_ in o.waits:
                self._sem(s_)
            if o.ev is not None:
                self._sem(o.ev[0])
        per_eng = {e: [o for o in self.ops if o.eng == e] for e in ENGS}
        sems = self.sems
        finals = sorted(running.items())
        self.n_sems = len(sems)

        def emit(engobj, ename):
            for o in per_eng[ename]:
                for s_, v_ in o.waits:
                    engobj.wait_ge(sems[s_], v_)
                ins = o.fn(engobj)
                if o.ev is not None:
                    ins.then_inc(sems[o.ev[0]], o.inc if o.key is not None else 1)
            if ename == final_eng:
                for s_, v_ in finals:
                    engobj.wait_ge(sems[s_], v_)

        with nc.Block() as block:
            @block.tensor
            def _(e):
                emit(e, "pe")

            @block.scalar
            def _(e):
                emit(e, "act")

            @block.vector
            def _(e):
                emit(e, "dve")

            @block.gpsimd
            def _(e):
                emit(e, "pool")

            @block.sync
            def _(e):
                emit(e, "sp")
        return len(self.ops)


ARENA_WORDS = 52600
UPTO = 4


class KB:
    def __init__(self, fused=False):
        self.nc = bass.Bass("TRN2", target_bir_lowering=False)
        self.P = Prog(self.nc)
        self.fused = fused
        self.tag = ""
        if fused:
            self.arena = self.nc.alloc_sbuf_tensor("arena", [128, ARENA_WORDS], F32)
            self.banks = [self.nc.alloc_psum_tensor("bank%d" % i, [128, 512], F32) for i in range(8)]
            self.off = 0
            self.nb = 0

    def begin_pass(self, tag):
        self.tag = tag
        self.off = 0
        self.nb = 0

    def din(self, name, shape, dt=F32):
        return self.nc.dram_tensor(name, list(shape), dt, kind="ExternalInput").ap()

    def dout(self, name, shape, dt=F32):
        return self.nc.dram_tensor(name, list(shape), dt, kind="ExternalOutput").ap()

    def dint(self, name, shape, dt=F32):
        return self.nc.dram_tensor(name, list(shape), dt, kind="Internal").ap()

    def sb(self, name, shape, dt=F32):
        if not self.fused:
            return self.nc.alloc_sbuf_tensor(name, list(shape), dt)
        esz = 4 if dt == F32 else 2
        n = 1
        for d_ in shape[1:]:
            n *= d_
        words = (n * esz + 3) // 4
        words = (words + 7) // 8 * 8
        assert self.off + words <= ARENA_WORDS, (self.tag, name, self.off, words)
        v = self.arena[0:shape[0], self.off:self.off + words]
        self.off += words
        if dt != F32:
            v = v.bitcast(dt)
        v = v[:, 0:n]
        if len(shape) == 3:
            v = v.rearrange("p (a b) -> p a b", a=shape[1])
        elif len(shape) == 4:
            v = v.rearrange("p (a b c) -> p a b c", a=shape[1], b=shape[2])
        return v

    def bank(self, name):
        if not self.fused:
            return self.nc.alloc_psum_tensor(name, [128, 512], F32)
        b = self.banks[self.nb]
        self.nb += 1
        return b

    def bufs(self, name, n):
        return [Buf("%s%s%d" % (self.tag, name, i)) for i in range(n)]

    def buf(self, name):
        return Buf(self.tag + name)


def emit_consts(k):
    P = k.P
    c = {}
    c["ones"] = k.sb("c_ones", [128, 128], BF16)
    c["eps"] = k.sb("c_eps", [128, 1], F32)
    c["Bones"] = k.buf("c_ones")
    c["Beps"] = k.buf("c_eps")
    P.op("pool", lambda e: e.memset(c["ones"][:, :], 1.0), writes=[c["Bones"]])
    P.op("pool", lambda e: e.memset(c["eps"][:, :], EPS), writes=[c["Beps"]])
    return c


def emit_mod(k, cb, adaT, adab, nv, R, BR):
    P = k.P
    n = nv * 8
    Rf = R[:, :, :].rearrange("p c t -> p (c t)")
    wch = [Rf[:, 0:D], Rf[:, D:2 * D]]
    Bw = [[BR[0], BR[1]], [BR[2], BR[3]]]
    junk = Rf[:, 2 * D:3 * D]
    Bj = [BR[4], BR[5]]
    scb = Rf[:, 3 * D:4 * D]
    Bscb = [BR[6], BR[7]]
    mod = k.sb("m_mod", [128, n], F32)
    Bmod = k.buf("m_mod")
    ab = k.sb("m_ab", [128, n], F32)
    Bab = k.buf("m_ab")
    P.dma("sp", lambda e: e.dma_start(out=scb, in_=cb[:, :]), writes=Bscb, key=BR[6])
    P.dma("sp", lambda e: e.dma_start(out=ab[:, :], in_=adab[:, :]), writes=[Bab], key=Bab)
    P.op("act", lambda e: e.activation(out=junk, in_=scb, func=AF.Sigmoid), reads=Bscb, writes=Bj)
    P.op("dve", lambda e: e.tensor_tensor(out=scb, in0=scb, in1=junk, op=ALU.mult),
         reads=Bscb + Bj, writes=Bscb)
    for j in range(n):
        s = j % 2
        P.dma("sp", lambda e, s=s, j=j: e.dma_start(out=wch[s], in_=adaT[j, :, :]), writes=Bw[s], key=Bw[s][0])
        P.op("dve", lambda e, s=s: e.tensor_tensor(out=junk, in0=wch[s], in1=scb, op=ALU.mult),
             reads=Bw[s] + Bscb, writes=Bj)
        P.op("dve", lambda e, j=j: e.tensor_reduce(out=mod[:, j:j + 1], in_=junk, axis=AX.X, op=ALU.add),
             reads=Bj, writes=[Bmod])
    P.op("dve", lambda e: e.tensor_tensor(out=mod[:, :], in0=mod[:, :], in1=ab[:, :], op=ALU.add),
         reads=[Bmod, Bab], writes=[Bmod])
    return mod, Bmod


def emit_scale(k, name, mod, Bmod, sc_lo, g_ap, Bg):
    P = k.P
    sc = k.sb(name, [128, NCH], F32)
    Bsc = k.buf(name)
    P.op("dve", lambda e: e.tensor_scalar(out=sc[:, :], in0=mod[:, sc_lo:sc_lo + NCH], scalar1=1.0, scalar2=None,
                                          op0=ALU.add), reads=[Bmod], writes=[Bsc])
    P.op("dve", lambda e: e.tensor_tensor(out=sc[:, :], in0=sc[:, :], in1=g_ap, op=ALU.mult),
         reads=[Bsc, Bg], writes=[Bsc])
    return sc, Bsc


class NormCtx:
    def __init__(self, k, consts, width=T):
        self.k = k
        self.c = consts
        self.sq = k.sb("n_sq", [128, NCH, width], BF16)
        self.Bsq = k.buf("n_sq")
        self.st = k.bank("n_st")
        self.Bst = k.buf("n_st")
        self.sd = k.sb("n_sd", [128, width], F32)
        self.Bsd = k.buf("n_sd")
        self.rstd = self.sd
        self.Brstd = self.Bsd
        self.tmp = [k.sb("n_tmp%d" % i, [128, width], F32) for i in range(2)]
        self.Btmp = k.bufs("n_tmp", 2)


def emit_norm(n, xt, Bx, w, scale, Bscale, shift_ap_fn, Bshift, hT, BhT):
    k = n.k
    P = k.P
    c = n.c
    P.op("act", lambda e: e.activation(out=n.sq[:, :, 0:w], in_=xt[:, :, 0:w], func=AF.Square), reads=list(Bx), writes=[n.Bsq])
    for ch in range(NCH):
        P.op("pe", lambda e, ch=ch: e.matmul(n.st[:, 0:w], lhsT=c["ones"][:, :], rhs=n.sq[:, ch, 0:w],
                                              start=(ch == 0), stop=(ch == NCH - 1)),
             reads=[n.Bsq, c["Bones"]], writes=[n.Bst])
    P.op("act", lambda e: e.activation(out=n.sd[:, 0:w], in_=n.st[:, 0:w], func=AF.Sqrt, bias=c["eps"][:, 0:1], scale=1.0 / D),
         reads=[n.Bst, c["Beps"]], writes=[n.Bsd])
    P.op("dve", lambda e: e.reciprocal(out=n.rstd[:, 0:w], in_=n.sd[:, 0:w]), reads=[n.Bsd], writes=[n.Brstd])
    for ch in range(NCH):
        s = ch % 2
        P.op("dve", lambda e, ch=ch, s=s: e.scalar_tensor_tensor(out=n.tmp[s][:, 0:w], in0=xt[:, ch, 0:w],
                                                                  scalar=scale[:, ch:ch + 1], in1=n.rstd[:, 0:w],
                                                                  op0=ALU.mult, op1=ALU.mult),
             reads=[Bx[ch], Bscale, n.Brstd], writes=[n.Btmp[s]])
        P.op("act", lambda e, ch=ch, s=s: e.activation(out=hT[:, ch, 0:w], in_=n.tmp[s][:, 0:w], func=AF.Identity,
                                                        bias=shift_ap_fn(ch), scale=1.0),
             reads=[n.Btmp[s], Bshift], writes=[BhT[ch]])


def build_mlp():
    k = KB()
    io = {"xT": k.din("xT", [D, TOK]), "xo": k.dout("xo", [D, TOK]), "cb": k.din("cb", [128, D]),
          "adaT": k.din("adaT", [24, 128, D]), "adab": k.din("adab", [128, 24]), "ng": k.din("ng", [128, NCH]),
          "w1": k.din("w1", [8, 128, 4096]), "w2": k.din("w2", [8, 128, 4096])}
    emit_mlp(k, io)
    n = k.P.finalize()
    return k.nc, n


def emit_mlp(k, io):
    nc, P = k.nc, k.P
    xT, xo, cb, adaT, adab, ng, w1, w2 = [io[n_] for n_ in ("xT", "xo", "cb", "adaT", "adab", "ng", "w1", "w2")]
    xv = xT.rearrange("(c p) t -> p c t", p=128)
    xov = xo.rearrange("(c p) t -> p c t", p=128)

    C = emit_consts(k)
    W1 = k.sb("W1", [128, 8, 4096], BF16)
    W2 = k.sb("W2", [128, 8, 4096], BF16)
    BW1 = k.bufs("W1_", 8)
    BW2 = k.bufs("W2_", 8)
    xt = [k.sb("xt%d" % i, [128, NCH, T], F32) for i in range(2)]
    Bxt = [k.bufs("xt%d_" % i, NCH) for i in range(2)]
    mod, Bmod = emit_mod(k, cb, adaT, adab, 3, xt[1], Bxt[1])
    ngt = k.sb("ngt", [128, NCH], F32)
    Bng = k.buf("ngt")
    P.dma("sp", lambda e: e.dma_start(out=ngt[:, :], in_=ng[:, :]), writes=[Bng], key=Bng)
    scale2, Bsc2 = emit_scale(k, "scale2", mod, Bmod, 8, ngt[:, :], Bng)
    for t in range(8):
        P.dma("pool", lambda e, t=t: e.dma_start(out=W1[:, t, :], in_=w1[t, :, :]), writes=[BW1[t]], key=BW1[t])
    for t in range(8):
        P.dma("pool", lambda e, t=t: e.dma_start(out=W2[:, t, :], in_=w2[t, :, :]), writes=[BW2[t]], key=BW2[t])

    N = NormCtx(k, C)
    hT = k.sb("hT", [128, NCH, T], BF16)
    BhT = k.bufs("hT_", NCH)
    hid = k.sb("hid", [128, 16, T], BF16)
    Bhid = k.bufs("hid_", 16)
    r = [k.sb("r%d" % i, [128, T], F32) for i in range(2)]
    Br = k.bufs("r_", 2)
    ph = [k.bank("ph%d" % i) for i in range(4)]
    Bph = k.bufs("ph_", 4)
    po = [k.bank("po%d" % i) for i in range(3)]
    Bpo = k.bufs("po_", 3)

    hcount = 0
    ocount = 0
    for i in range(NT):
        s = i % 2
        x = xt[s]
        Bx = Bxt[s]
        P.dma("sp", lambda e, x=x, i=i: e.dma_start(out=x[:, :, :], in_=xv[:, :, i * T:(i + 1) * T]), writes=Bx, key=Bx[0])
        emit_norm(N, x, Bx, T, scale2, Bsc2, lambda ch: mod[:, ch:ch + 1], Bmod, hT, BhT)
        for half in range(2):
            for jc in range(16):
                j = half * 16 + jc
                wt, m = j // 4, j % 4
                pb = hcount % 4
                rb = hcount % 2
                hcount += 1
                for kc in range(NCH):
                    P.op("pe", lambda e, wt=wt, m=m, kc=kc, pb=pb: e.matmul(
                        ph[pb][:, :], lhsT=W1[:, wt, kc * 512 + m * 128: kc * 512 + (m + 1) * 128], rhs=hT[:, kc, :],
                        start=(kc == 0), stop=(kc == NCH - 1)),
                         reads=[BW1[wt], BhT[kc]], writes=[Bph[pb]])
                P.op("act", lambda e, pb=pb, rb=rb: e.activation(out=r[rb][:, :], in_=ph[pb][:, :], func=AF.Relu),
                     reads=[Bph[pb]], writes=[Br[rb]])
                P.op("dve", lambda e, rb=rb, jc=jc: e.tensor_tensor(out=hid[:, jc, :], in0=r[rb][:, :], in1=r[rb][:, :], op=ALU.mult),
                     reads=[Br[rb]], writes=[Bhid[jc]])
            for c in range(NCH):
                ob = ocount % 3
                ocount += 1
                for kc in range(16):
                    kg = half * 16 + kc
                    P.op("pe", lambda e, c=c, kc=kc, kg=kg, ob=ob: e.matmul(
                        po[ob][:, :], lhsT=W2[:, c, kg * 128:(kg + 1) * 128], rhs=hid[:, kc, :],
                        start=(kc == 0), stop=(kc == 15)),
                         reads=[BW2[c], Bhid[kc]], writes=[Bpo[ob]])
                P.op("dve", lambda e, c=c, ob=ob, x=x: e.scalar_tensor_tensor(
                    out=x[:, c, :], in0=po[ob][:, :], scalar=mod[:, 16 + c:17 + c], in1=x[:, c, :],
                    op0=ALU.mult, op1=ALU.add),
                     reads=[Bpo[ob], Bmod, Bx[c]], writes=[Bx[c]])
        P.dma("sp", lambda e, x=x, i=i: e.dma_start(out=xov[:, :, i * T:(i + 1) * T], in_=x[:, :, :]), reads=Bx, key=Bx[1])


def tileA(W):
    K, N = W.shape
    assert K == 1024
    return np.ascontiguousarray(W.reshape(8, 128, N // 512, 512).transpose(2, 1, 0, 3).reshape(N // 512, 128, 4096))


def tileB(W2):
    return np.ascontiguousarray(W2.reshape(32, 128, 8, 128).transpose(2, 1, 0, 3).reshape(8, 128, 4096))


def pvec(v):
    return np.ascontiguousarray(v.reshape(-1, 128).T)


def ada_rows(ada_w_i, vec_ids):
    WT = ada_w_i.T
    rows = np.concatenate([WT[v * D:(v + 1) * D] for v in vec_ids])
    return np.ascontiguousarray(rows.reshape(-1, 128, D))


def ada_cols(ada_b_i, vec_ids):
    return np.ascontiguousarray(np.concatenate([pvec(ada_b_i[v * D:(v + 1) * D]) for v in vec_ids], axis=1))


def core_tokens(core):
    b, q = core // 4, core % 4
    return b, q * TOK


_cache = {}


def get_prog(name, builder):
    if name not in _cache:
        _cache[name] = builder()[0]
    return _cache[name]


def run_mlp(xTs, c, ada_w_i, ada_b_i, ng_i, w1_i, w2_i):
    nc = get_prog("mlp", build_mlp)
    aT = ada_rows(ada_w_i, [3, 4, 5])
    ab = ada_cols(ada_b_i, [3, 4, 5])
    w1t = tileA(w1_i)
    w2t = tileB(w2_i)
    ngp = pvec(ng_i)
    in_maps = []
    for core in range(8):
        b, _ = core_tokens(core)
        in_maps.append({"xT": xTs[core], "cb": np.ascontiguousarray(np.broadcast_to(c[b], (128, D))),
                        "adaT": aT, "adab": ab, "ng": ngp, "w1": w1t, "w2": w2t})
    res = run_bass_kernel_spmd(nc, in_maps, core_ids=list(range(8)))
    return [r["xo"] for r in res.results]


NVC = 56 + NCH * CW


def build_conv():
    k = KB()
    xh = k.din("xh", [D, HALO])
    io = {"xT": k.din("xT", [D, TOK]), "xh_fn": (lambda e: xh.rearrange("(c p) t -> p c t", p=128)), "hv": k.din("hv", [128, 1]),
          "xo": k.dout("xo", [D, TOK]), "cb": k.din("cb", [128, D]), "adaT": k.din("adaT", [24, 128, D]),
          "adab": k.din("adab", [128, 24]), "vec": k.din("vec", [128, NVC]), "ident": k.din("ident", [128, 128]),
          "win": k.din("win", [4, 128, 4096]), "wout": k.din("wout", [2, 128, 4096])}
    emit_conv(k, io)
    n = k.P.finalize()
    return k.nc, n


def emit_conv(k, io):
    nc, P = k.nc, k.P
    xT, hv, xo, cb, adaT, adab, vec, identd, win, wout = [io[n_] for n_ in (
        "xT", "hv", "xo", "cb", "adaT", "adab", "vec", "ident", "win", "wout")]
    xh_fn = io["xh_fn"]
    xv = xT.rearrange("(c p) t -> p c t", p=128)
    xov = xo.rearrange("(c p) t -> p c t", p=128)

    C = emit_consts(k)
    ident = k.sb("ident_sb", [128, 128], BF16)
    Bid = k.buf("ident")
    P.dma("pool", lambda e: e.dma_start(out=ident[:, :], in_=identd[:, :]), writes=[Bid], key=Bid)
    vt = k.sb("vt", [128, NVC], F32)
    Bvt = k.buf("vt")
    P.dma("sp", lambda e: e.dma_start(out=vt[:, :], in_=vec[:, :]), writes=[Bvt], key=Bvt)
    hvt = k.sb("hvt", [128, 1], F32)
    Bhv = k.buf("hvt")
    P.dma("sp", lambda e: e.dma_start(out=hvt[:, :], in_=hv[:, :]), writes=[Bhv], key=Bhv)
    xt = [k.sb("xt%d" % i, [128, NCH, T], F32) for i in range(2)]
    Bxt = [k.bufs("xt%d_" % i, NCH) for i in range(2)]
    mod, Bmod = emit_mod(k, cb, adaT, adab, 3, xt[1], Bxt[1])
    scale1, Bsc1 = emit_scale(k, "scale1", mod, Bmod, 8, vt[:, 0:8], Bvt)
    bg = k.sb("bg", [128, NCH], F32)
    Bbg = k.buf("bg")
    P.op("dve", lambda e: e.tensor_tensor(out=bg[:, :], in0=vt[:, 48:56], in1=mod[:, 16:24], op=ALU.mult),
         reads=[Bvt, Bmod], writes=[Bbg])
    Win = k.sb("Win", [128, 4, 4096], BF16)
    Wout = k.sb("Wout", [128, 2, 4096], BF16)
    BWin = k.bufs("Win_", 4)
    BWout = k.bufs("Wout_", 2)
    for t in range(4):
        P.dma("pool", lambda e, t=t: e.dma_start(out=Win[:, t, :], in_=win[t, :, :]), writes=[BWin[t]], key=BWin[t])
    for t in range(2):
        P.dma("pool", lambda e, t=t: e.dma_start(out=Wout[:, t, :], in_=wout[t, :, :]), writes=[BWout[t]], key=BWout[t])

    N = NormCtx(k, C)
    hT = k.sb("hT", [128, NCH, T], BF16)
    BhT = k.bufs("hT_", NCH)
    sg = N.sq
    Bsg = N.Bsq
    ub = [k.sb("ub%d" % i, [128, NCH, T + HALO], BF16) for i in range(2)]
    Bu = [k.bufs("ub%d_" % i, NCH) for i in range(2)]
    xht = k.sb("xht", [128, NCH, HALO], F32)
    Bxh = k.bufs("xht_", NCH)
    dg = [k.sb("dg%d" % i, [128, CW, 128], BF16) for i in range(2)]
    Bdg = k.bufs("dg_", 2)
    v = k.sb("v", [128, NCH, T], F32)
    Bv = k.bufs("v_", NCH)
    vb = [k.sb("vb%d" % i, [128, T], BF16) for i in range(2)]
    Bvb = k.bufs("vb_", 2)
    vsq = [k.sb("vsq%d" % i, [128, T], BF16) for i in range(2)]
    Bvsq = k.bufs("vsq_", 2)
    mean = k.sb("mean", [128, T], F32)
    Bmean = k.buf("mean")
    var = k.sb("var", [128, T], F32)
    Bvar = k.buf("var")
    t1 = [k.sb("t1_%d" % i, [128, T], F32) for i in range(2)]
    Bt1 = k.bufs("t1_", 2)
    pw = [k.bank("pw%d" % i) for i in range(3)]
    Bpw = k.bufs("pw_", 3)
    pc = [k.bank("pc%d" % i) for i in range(2)]
    Bpc = k.bufs("pc_", 2)
    pm = k.bank("pm")
    Bpm = k.buf("pm")
    pq = k.bank("pq")
    Bpq = k.buf("pq")
    cnt = {"w": 0, "c": 0}

    def front(x, Bx, w, slot, off):
        emit_norm(N, x, Bx, w, scale1, Bsc1, lambda ch: mod[:, ch:ch + 1], Bmod, hT, BhT)
        for c in range(NCH):
            for part in (1, 0):
                j = part * 8 + c
                wt, m = j // 4, j % 4
                pb = cnt["w"] % 3
                cnt["w"] += 1
                for kc in range(NCH):
                    P.op("pe", lambda e, wt=wt, m=m, kc=kc, pb=pb: e.matmul(
                        pw[pb][:, 0:w], lhsT=Win[:, wt, kc * 512 + m * 128: kc * 512 + (m + 1) * 128], rhs=hT[:, kc, 0:w],
                        start=(kc == 0), stop=(kc == NCH - 1)),
                         reads=[BWin[wt], BhT[kc]], writes=[Bpw[pb]])
                if part == 1:
                    P.op("act", lambda e, c=c, pb=pb: e.activation(out=sg[:, c, 0:w], in_=pw[pb][:, 0:w], func=AF.Sigmoid,
                                                                   bias=vt[:, 16 + c:17 + c], scale=1.0),
                         reads=[Bpw[pb], Bvt], writes=[Bsg])
                else:
                    P.op("dve", lambda e, c=c, pb=pb: e.scalar_tensor_tensor(
                        out=ub[slot][:, c, off:off + w], in0=pw[pb][:, 0:w], scalar=vt[:, 8 + c:9 + c], in1=sg[:, c, 0:w],
                        op0=ALU.add, op1=ALU.mult),
                         reads=[Bpw[pb], Bvt, Bsg], writes=[Bu[slot][c]])

    P.dma("sp", lambda e: e.dma_start(out=xht[:, :, :], in_=xh_fn(e)), writes=Bxh, key=Bxh[0])
    front(xht, Bxh, HALO, 0, 0)
    P.op("dve", lambda e: e.tensor_scalar(out=ub[0][:, :, 0:HALO], in0=ub[0][:, :, 0:HALO], scalar1=hvt[:, 0:1], scalar2=None,
                                          op0=ALU.mult), reads=Bu[0] + [Bhv], writes=Bu[0])

    for i in range(NT):
        s = i % 2
        x = xt[s]
        Bx = Bxt[s]
        P.dma("sp", lambda e, x=x, i=i: e.dma_start(out=x[:, :, :], in_=xv[:, :, i * T:(i + 1) * T]), writes=Bx, key=Bx[0])
        front(x, Bx, T, s, HALO)
        P.op("dve", lambda e, s=s: e.tensor_copy(out=ub[1 - s][:, :, 0:HALO], in_=ub[s][:, :, T:T + HALO]),
             reads=Bu[s], writes=Bu[1 - s])
        for c in range(NCH):
            ds = cnt["c"] % 2
            cb_ = cnt["c"] % 2
            cnt["c"] += 1
            P.op("pool", lambda e, c=c, ds=ds: e.tensor_tensor(
                out=dg[ds][:, :, :], in0=ident[:, :].unsqueeze(1).broadcast_to([128, CW, 128]),
                in1=vt[:, 56 + c * CW:56 + (c + 1) * CW].unsqueeze(2).broadcast_to([128, CW, 128]), op=ALU.mult),
                 reads=[Bid, Bvt], writes=[Bdg[ds]])
            for kk in range(CW):
                P.op("pe", lambda e, c=c, kk=kk, ds=ds, cb_=cb_, s=s: e.matmul(
                    pc[cb_][:, :], lhsT=dg[ds][:, kk, :], rhs=ub[s][:, c, kk:kk + T], start=(kk == 0), stop=(kk == CW - 1)),
                     reads=[Bdg[ds], Bu[s][c]], writes=[Bpc[cb_]])
            P.op("act", lambda e, c=c, cb_=cb_: e.activation(out=v[:, c, :], in_=pc[cb_][:, :], func=AF.Identity,
                                                             bias=vt[:, 24 + c:25 + c], scale=1.0),
                 reads=[Bpc[cb_], Bvt], writes=[Bv[c]])
            P.op("act", lambda e, c=c, cb_=cb_, ds=ds: e.activation(out=vsq[ds][:, :], in_=pc[cb_][:, :], func=AF.Square,
                                                                    bias=vt[:, 24 + c:25 + c], scale=1.0),
                 reads=[Bpc[cb_], Bvt], writes=[Bvsq[ds]])
            P.op("dve", lambda e, c=c, ds=ds: e.tensor_copy(out=vb[ds][:, :], in_=v[:, c, :]), reads=[Bv[c]], writes=[Bvb[ds]])
            P.op("pe", lambda e, c=c, ds=ds: e.matmul(pm[:, :], lhsT=C["ones"][:, :], rhs=vb[ds][:, :], start=(c == 0), stop=(c == NCH - 1)),
                 reads=[Bvb[ds], C["Bones"]], writes=[Bpm])
            P.op("pe", lambda e, c=c, ds=ds: e.matmul(pq[:, :], lhsT=C["ones"][:, :], rhs=vsq[ds][:, :], start=(c == 0), stop=(c == NCH - 1)),
                 reads=[Bvsq[ds], C["Bones"]], writes=[Bpq])
        P.op("act", lambda e: e.activation(out=mean[:, :], in_=pm[:, :], func=AF.Identity, scale=1.0 / D), reads=[Bpm], writes=[Bmean])
        P.op("dve", lambda e: e.tensor_tensor(out=var[:, :], in0=mean[:, :], in1=mean[:, :], op=ALU.mult), reads=[Bmean], writes=[Bvar])
        P.op("dve", lambda e: e.scalar_tensor_tensor(out=var[:, :], in0=pq[:, :], scalar=1.0 / D, in1=var[:, :],
                                                     op0=ALU.mult, op1=ALU.subtract), reads=[Bpq, Bvar], writes=[Bvar])
        P.op("act", lambda e: e.activation(out=var[:, :], in_=var[:, :], func=AF.Sqrt, bias=C["eps"][:, 0:1], scale=1.0),
             reads=[Bvar, C["Beps"]], writes=[Bvar])
        P.op("dve", lambda e: e.reciprocal(out=var[:, :], in_=var[:, :]), reads=[Bvar], writes=[Bvar])
        for c in range(NCH):
            ts_ = c % 2
            P.op("dve", lambda e, c=c, ts_=ts_: e.tensor_tensor(out=t1[ts_][:, :], in0=v[:, c, :], in1=mean[:, :], op=ALU.subtract),
                 reads=[Bv[c], Bmean], writes=[Bt1[ts_]])
            P.op("dve", lambda e, ts_=ts_: e.tensor_tensor(out=t1[ts_][:, :], in0=t1[ts_][:, :], in1=var[:, :], op=ALU.mult),
                 reads=[Bt1[ts_], Bvar], writes=[Bt1[ts_]])
            P.op("act", lambda e, c=c, ts_=ts_: e.activation(out=hT[:, c, :], in_=t1[ts_][:, :], func=AF.Silu,
                                                             bias=vt[:, 40 + c:41 + c], scale=vt[:, 32 + c:33 + c]),
                 reads=[Bt1[ts_], Bvt], writes=[BhT[c]])
        for c in range(NCH):
            wt, m = c // 4, c % 4
            pb = cnt["w"] % 3
            cnt["w"] += 1
            for kc in range(NCH):
                P.op("pe", lambda e, wt=wt, m=m, kc=kc, pb=pb: e.matmul(
                    pw[pb][:, :], lhsT=Wout[:, wt, kc * 512 + m * 128: kc * 512 + (m + 1) * 128], rhs=hT[:, kc, :],
                    start=(kc == 0), stop=(kc == NCH - 1)),
                     reads=[BWout[wt], BhT[kc]], writes=[Bpw[pb]])
            P.op("dve", lambda e, c=c, pb=pb, x=x: e.scalar_tensor_tensor(
                out=x[:, c, :], in0=pw[pb][:, :], scalar=mod[:, 16 + c:17 + c], in1=x[:, c, :], op0=ALU.mult, op1=ALU.add),
                 reads=[Bpw[pb], Bmod, Bx[c]], writes=[Bx[c]])
            P.op("dve", lambda e, c=c, x=x: e.tensor_scalar(out=x[:, c, :], in0=x[:, c, :], scalar1=bg[:, c:c + 1], scalar2=None,
                                                            op0=ALU.add), reads=[Bx[c], Bbg], writes=[Bx[c]])
        P.dma("sp", lambda e, x=x, i=i: e.dma_start(out=xov[:, :, i * T:(i + 1) * T], in_=x[:, :, :]), reads=Bx, key=Bx[1])


def conv_vec(z, j):
    cols = [pvec(z["mix_norm_g_i"]), pvec(z["conv_b_in"][j][:D]), pvec(z["conv_b_in"][j][D:]), pvec(z["conv_dw_b"][j]),
            pvec(z["conv_ln_g"][j]), pvec(z["conv_ln_b"][j]), pvec(z["conv_b_out"][j])]
    dw = z["conv_dw"][j]
    dwp = dw.reshape(CW, NCH, 128).transpose(2, 1, 0).reshape(128, NCH * CW)
    return np.ascontiguousarray(np.concatenate(cols + [dwp], axis=1).astype(np.float32))


def run_conv(xTs, halos, c, ada_w_i, ada_b_i, mix_g_i, z, j):
    nc = get_prog("conv", build_conv)
    aT = ada_rows(ada_w_i, [0, 1, 2])
    ab = ada_cols(ada_b_i, [0, 1, 2])
    zz = dict(z)
    zz["mix_norm_g_i"] = mix_g_i
    vec = conv_vec(zz, j)
    wint = tileA(z["conv_w_in"][j])
    woutt = tileA(z["conv_w_out"][j])
    ident = np.eye(128, dtype=np.float32)
    in_maps = []
    for core in range(8):
        b, t0 = core_tokens(core)
        in_maps.append({"xT": xTs[core], "xh": halos[core],
                        "hv": np.full((128, 1), 0.0 if t0 == 0 else 1.0, np.float32),
                        "cb": np.ascontiguousarray(np.broadcast_to(c[b], (128, D))),
                        "adaT": aT, "adab": ab, "vec": vec, "ident": ident, "win": wint, "wout": woutt})
    res = run_bass_kernel_spmd(nc, in_maps, core_ids=list(range(8)))
    return [r["xo"] for r in res.results]


def build_a1():
    k = KB()
    io = {"xT": k.din("xT", [D, TOK]), "ho": k.dout("ho", [D, TOK], BF16), "cb": k.din("cb", [128, D]),
          "adaT": k.din("adaT", [16, 128, D]), "adab": k.din("adab", [128, 16]), "ng": k.din("ng", [128, NCH])}
    emit_a1(k, io)
    n = k.P.finalize()
    return k.nc, n


def emit_a1(k, io):
    nc, P = k.nc, k.P
    xT, ho, cb, adaT, adab, ng = [io[n_] for n_ in ("xT", "ho", "cb", "adaT", "adab", "ng")]
    xv = xT.rearrange("(c p) t -> p c t", p=128)
    hov = ho.rearrange("(c p) t -> p c t", p=128)
    C = emit_consts(k)
    xt = [k.sb("xt%d" % i, [128, NCH, T], F32) for i in range(2)]
    Bxt = [k.bufs("xt%d_" % i, NCH) for i in range(2)]
    mod, Bmod = emit_mod(k, cb, adaT, adab, 2, xt[1], Bxt[1])
    ngt = k.sb("ngt", [128, NCH], F32)
    Bng = k.buf("ngt")
    P.dma("sp", lambda e: e.dma_start(out=ngt[:, :], in_=ng[:, :]), writes=[Bng], key=Bng)
    scale1, Bsc1 = emit_scale(k, "scale1", mod, Bmod, 8, ngt[:, :], Bng)
    N = NormCtx(k, C)
    hT = [k.sb("hT%d" % i, [128, NCH, T], BF16) for i in range(2)]
    BhT = [k.bufs("hT%d_" % i, NCH) for i in range(2)]
    for i in range(NT):
        s = i % 2
        x, Bx = xt[s], Bxt[s]
        P.dma("sp", lambda e, x=x, i=i: e.dma_start(out=x[:, :, :], in_=xv[:, :, i * T:(i + 1) * T]), writes=Bx, key=Bx[0])
        emit_norm(N, x, Bx, T, scale1, Bsc1, lambda ch: mod[:, ch:ch + 1], Bmod, hT[s], BhT[s])
        P.dma("sp", lambda e, s=s, i=i: e.dma_start(out=hov[:, :, i * T:(i + 1) * T], in_=hT[s][:, :, :]), reads=BhT[s], key=BhT[s][0])


def build_a3():
    k = KB()
    aT = k.din("aT", [D, TOK], BF16)
    av = aT.rearrange("(c p) t -> p c t", p=128)

    def a_load(P, dst, Bd, i):
        P.dma("sp", lambda e: e.dma_start(out=dst[:, :, :], in_=av[:, :, i * T:(i + 1) * T]), writes=Bd, key=Bd[0])

    io = {"xT": k.din("xT", [D, TOK]), "a_load": a_load, "xo": k.dout("xo", [D, TOK]), "cb": k.din("cb", [128, D]),
          "adaT": k.din("adaT", [8, 128, D]), "adab": k.din("adab", [128, 8]), "wo": k.din("wo", [2, 128, 4096])}
    emit_a3(k, io)
    n = k.P.finalize()
    return k.nc, n


def emit_a3(k, io):
    nc, P = k.nc, k.P
    xT, xo, cb, adaT, adab, wo = [io[n_] for n_ in ("xT", "xo", "cb", "adaT", "adab", "wo")]
    a_load = io["a_load"]
    xv = xT.rearrange("(c p) t -> p c t", p=128)
    xov = xo.rearrange("(c p) t -> p c t", p=128)
    xt = [k.sb("xt%d" % i, [128, NCH, T], F32) for i in range(2)]
    Bxt = [k.bufs("xt%d_" % i, NCH) for i in range(2)]
    mod, Bmod = emit_mod(k, cb, adaT, adab, 1, xt[1], Bxt[1])
    Wo = k.sb("Wo", [128, 2, 4096], BF16)
    BWo = k.bufs("Wo_", 2)
    for t in range(2):
        P.dma("pool", lambda e, t=t: e.dma_start(out=Wo[:, t, :], in_=wo[t, :, :]), writes=[BWo[t]], key=BWo[t])
    at = [k.sb("at%d" % i, [128, NCH, T], BF16) for i in range(2)]
    Bat = [k.bufs("at%d_" % i, NCH) for i in range(2)]
    pw = [k.bank("pw%d" % i) for i in range(2)]
    Bpw = k.bufs("pw_", 2)
    cnt = 0
    for i in range(NT):
        s = i % 2
        x, Bx = xt[s], Bxt[s]
        P.dma("sp", lambda e, x=x, i=i: e.dma_start(out=x[:, :, :], in_=xv[:, :, i * T:(i + 1) * T]), writes=Bx, key=Bx[0])
        a_load(P, at[s], Bat[s], i)
        for c in range(NCH):
            wt, m = c // 4, c % 4
            pb = cnt % 2
            cnt += 1
            for kc in range(NCH):
                P.op("pe", lambda e, wt=wt, m=m, kc=kc, pb=pb, s=s: e.matmul(
                    pw[pb][:, :], lhsT=Wo[:, wt, kc * 512 + m * 128: kc * 512 + (m + 1) * 128], rhs=at[s][:, kc, :],
                    start=(kc == 0), stop=(kc == NCH - 1)),
                     reads=[BWo[wt], Bat[s][kc]], writes=[Bpw[pb]])
            P.op("dve", lambda e, c=c, pb=pb, x=x: e.scalar_tensor_tensor(
                out=x[:, c, :], in0=pw[pb][:, :], scalar=mod[:, c:c + 1], in1=x[:, c, :], op0=ALU.mult, op1=ALU.add),
                 reads=[Bpw[pb], Bmod, Bx[c]], writes=[Bx[c]])
        P.dma("sp", lambda e, x=x, i=i: e.dma_start(out=xov[:, :, i * T:(i + 1) * T], in_=x[:, :, :]), reads=Bx, key=Bx[1])


NQS = S // T
NKT = S // 128
HG = 4


def build_a2():
    k = KB()
    hTa = k.din("hTa", [D, S], BF16)
    hv_ = hTa.rearrange("(c p) t -> p c t", p=128)
    ao = k.dout("ao", [HG * 64, S], BF16)
    io = {"h_tile": (lambda i: hv_[:, :, i * T:(i + 1) * T]),
          "ao_dst": (lambda h, qs: ao[h * 64:(h + 1) * 64, qs * T:(qs + 1) * T]),
          "wqk": k.din("wqk", [128, 4096]), "wv": k.din("wv", [128, NCH * 256]), "gqk": k.din("gqk", [128, 2]),
          "AB": k.din("AB", [128, HG * 128]), "shc": k.din("shc", [128, 16]), "kconst": k.din("kconst", [64, S], BF16),
          "cmask": k.din("cmask", [4, 128, T]), "ident": k.din("ident", [128, 128]), "bdones": k.din("bdones", [128, 128]),
          "QA": k.dint("QA", [HG, 128, S], BF16), "KA": k.dint("KA", [HG, 64, S], BF16)}
    emit_a2(k, io)
    n = k.P.finalize()
    return k.nc, n


def emit_a2(k, io):
    nc, P = k.nc, k.P
    wqk, wv, gqk, ABd, shcd, kconst, cmaskd, identd, bdd, QA, KA = [io[n_] for n_ in (
        "wqk", "wv", "gqk", "AB", "shc", "kconst", "cmask", "ident", "bdones", "QA", "KA")]
    h_tile, ao_dst = io["h_tile"], io["ao_dst"]
    BQA = k.bufs("QAd", HG)
    BKA = k.bufs("KAd", HG)

    C = emit_consts(k)
    ident = k.sb("ident_sb", [128, 128], BF16)
    Bid = k.buf("ident")
    bdo = k.sb("bdo", [128, 128], BF16)
    Bbd = k.buf("bdo")
    cmask = k.sb("cmask_sb", [128, 4, T], BF16)
    Bcm = k.buf("cmask")
    AB = k.sb("AB_sb", [128, HG * 128], F32)
    BAB = k.buf("AB")
    shc = k.sb("shc_sb", [128, 16], F32)
    Bshc = k.buf("shc")
    gq = k.sb("gqk_sb", [128, 2], F32)
    Bgq = k.buf("gqk")
    onesf = k.sb("onesf", [128, 64], F32)
    Bof = k.buf("onesf")
    Wqk = k.sb("Wqk", [128, 4096], BF16)
    BWqk = k.buf("Wqk")
    Wv = k.sb("Wv", [128, NCH * 256], BF16)
    BWv = k.buf("Wv")
    Kaug = k.sb("Kaug", [128, S], BF16)
    BKlo = k.buf("Kaug_lo")
    BKhi = k.buf("Kaug_hi")
    Vall = k.sb("Vall", [128, NKT, HG, 65], BF16)
    BV = k.bufs("Vall_", NKT)
    Bvones = k.buf("Vones")
    P.dma("pool", lambda e: e.dma_start(out=ident[:, :], in_=identd[:, :]), writes=[Bid], key=Bid)
    P.dma("pool", lambda e: e.dma_start(out=bdo[:, :], in_=bdd[:, :]), writes=[Bbd], key=Bbd)
    for d_ in range(4):
        P.dma("pool", lambda e, d_=d_: e.dma_start(out=cmask[:, d_, :], in_=cmaskd[d_, :, :]), writes=[Bcm], key=Bcm)
    P.dma("pool", lambda e: e.dma_start(out=Wqk[:, :], in_=wqk[:, :]), writes=[BWqk], key=BWqk)
    P.dma("pool", lambda e: e.dma_start(out=Wv[:, :], in_=wv[:, :]), writes=[BWv], key=BWv)
    P.dma("sp", lambda e: e.dma_start(out=AB[:, :], in_=ABd[:, :]), writes=[BAB], key=BAB)
    P.dma("sp", lambda e: e.dma_start(out=shc[:, :], in_=shcd[:, :]), writes=[Bshc], key=Bshc)
    P.dma("sp", lambda e: e.dma_start(out=gq[:, :], in_=gqk[:, :]), writes=[Bgq], key=Bgq)
    P.dma("sp", lambda e: e.dma_start(out=Kaug[64:128, :], in_=kconst[:, :]), writes=[BKhi], key=BKhi)
    P.op("pool", lambda e: e.memset(onesf[:, :], 1.0), writes=[Bof])
    P.op("pool", lambda e: e.memset(Vall[:, :, :, 64:65], 1.0), writes=[Bvones])

    banks = [k.bank("bk%d" % i) for i in range(8)]
    Bb = k.bufs("bk_", 8)

    PKB = [0, 1, 6, 7]
    PNB = [2, 3, 4, 5]
    ht = [k.sb("ht%d" % i, [128, NCH, T], BF16) for i in range(2)]
    Bht = k.bufs("ht_", 2)
    sqb = [k.sb("sqb%d" % i, [128, T], BF16) for i in range(4)]
    Bsqb = k.bufs("sqb_", 4)
    sd = [k.sb("sd%d" % i, [128, T], F32) for i in range(4)]
    Bsd = k.bufs("sd_", 4)
    nf = [k.sb("nf%d" % i, [128, T], F32) for i in range(4)]
    Bnf = k.bufs("nf_", 4)
    kb = [k.sb("kb%d" % i, [128, T], BF16) for i in range(2)]
    Bkb = k.bufs("kb_", 2)
    kmT = [k.sb("kmT%d" % i, [128, 64], F32) for i in range(2)]
    Bkm = k.bufs("kmT_", 2)
    QAt = [k.sb("QAt%d" % i, [128, T], BF16) for i in range(2)]
    BQAt = k.bufs("QAt_", 2)
    mb4 = [k.sb("mb4_%d" % i, [128, 4, 64], F32) for i in range(2)]
    Bmb4 = [k.bufs("mb4_%d_" % i, 4) for i in range(2)]
    gp4 = [k.sb("gp4_%d" % i, [128, 4, 64], F32) for i in range(2)]
    Bgp4 = [k.bufs("gp4_%d_" % i, 4) for i in range(2)]
    top8 = [k.sb("top8_%d" % i, [128, 4, 8], F32) for i in range(2)]
    Btop = [k.bufs("top8_%d_" % i, 4) for i in range(2)]
    Mbb4 = [k.sb("Mbb4_%d" % i, [128, 4, 128], BF16) for i in range(2)]
    BMbb4 = k.bufs("Mbb4_", 2)
    for r in range(2):
        P.op("pool", lambda e, r=r: e.memset(Mbb4[r][:, :, :], 0.0), writes=[BMbb4[r]])
        P.op("pool", lambda e, r=r: e.memset(kmT[r][:, :], 0.0), writes=[Bkm[r]])
    CH = [(1, 0), (1, 1), (0, 0), (0, 1)]
    for i in range(NQS):
        s = i % 2
        P.dma("sp", lambda e, s=s, i=i: e.dma_start(out=ht[s][:, :, :], in_=h_tile(i)), writes=[Bht[s]], key=Bht[s])
        for ci, (isk, hp) in enumerate(CH):
            m = isk * 2 + hp
            for kc in range(NCH):
                P.op("pe", lambda e, m=m, kc=kc, ci=ci, s=s: e.matmul(
                    banks[PKB[ci]][:, :], lhsT=Wqk[:, kc * 512 + m * 128: kc * 512 + (m + 1) * 128], rhs=ht[s][:, kc, :],
                    start=(kc == 0), stop=(kc == NCH - 1)), reads=[BWqk, Bht[s]], writes=[Bb[PKB[ci]]])
        for ci in range(4):
            P.op("act", lambda e, ci=ci: e.activation(out=sqb[ci][:, :], in_=banks[PKB[ci]][:, :], func=AF.Square),
                 reads=[Bb[PKB[ci]]], writes=[Bsqb[ci]])
        for ci in range(4):
            P.op("pe", lambda e, ci=ci: e.matmul(banks[PNB[ci]][:, :], lhsT=bdo[:, :], rhs=sqb[ci][:, :], start=True, stop=True),
                 reads=[Bbd, Bsqb[ci]], writes=[Bb[PNB[ci]]])
        for ci in range(4):
            P.op("act", lambda e, ci=ci: e.activation(out=sd[ci][:, :], in_=banks[PNB[ci]][:, :], func=AF.Sqrt, bias=C["eps"][:, 0:1],
                                                      scale=1.0 / 64), reads=[Bb[PNB[ci]], C["Beps"]], writes=[Bsd[ci]])
        for ci in range(4):
            P.op("dve", lambda e, ci=ci: e.reciprocal(out=sd[ci][:, :], in_=sd[ci][:, :]), reads=[Bsd[ci]], writes=[Bsd[ci]])
        for ci, (isk, hp) in enumerate(CH):
            P.op("dve", lambda e, ci=ci, isk=isk: e.scalar_tensor_tensor(out=nf[ci][:, :], in0=banks[PKB[ci]][:, :], scalar=gq[:, isk:isk + 1],
                                                                          in1=sd[ci][:, :], op0=ALU.mult, op1=ALU.mult),
                 reads=[Bb[PKB[ci]], Bgq, Bsd[ci]], writes=[Bnf[ci]])
        for hp in range(2):
            P.op("act", lambda e, hp=hp: e.activation(out=kb[hp][:, :], in_=nf[hp][:, :], func=AF.Identity), reads=[Bnf[hp]], writes=[Bkb[hp]])
            for hb in range(2):
                h = 2 * hp + hb
                P.dma("pool", lambda e, h=h, hb=hb, hp=hp, i=i: e.dma_start(
                    out=KA[h, :, i * T:(i + 1) * T], in_=kb[hp][hb * 64:(hb + 1) * 64, :]),
                      reads=[Bkb[hp]], writes=[BKA[h]], key=Bkb[hp])
            P.op("dve", lambda e, hp=hp, i=i: e.tensor_reduce(out=kmT[hp][:, 2 * i:2 * i + 2],
                                                               in_=nf[hp][:, :].rearrange("p (b t) -> p b t", b=2),
                                                               axis=AX.X, op=ALU.add), reads=[Bnf[hp]], writes=[Bkm[hp]])
        ms = [2 * i, 2 * i, 2 * i + 1, 2 * i + 1]
        for h in range(HG):
            hp, hb = h // 2, h % 2
            p0 = hb * 64
            qn = nf[2 + hp]
            Bqn = Bnf[2 + hp]
            qs_ = (4 * i + h) % 2
            r = h % 2
            gb = 2 + r
            tb = 4 + r
            P.op("dve", lambda e, qn=qn, p0=p0, qs_=qs_: e.tensor_copy(out=QAt[qs_][0:64, :], in_=qn[p0:p0 + 64, :]),
                 reads=[Bqn], writes=[BQAt[qs_]])
            P.op("pool", lambda e, r=r: e.memset(mb4[r][:, :, :], NEG), writes=Bmb4[r])
            act_js = [js for js in range(4) if ms[js] >= 3]
            if act_js:
                P.op("pool", lambda e, r=r: e.memset(gp4[r][:, :, :], -1e30), writes=Bgp4[r])
                for js in act_js:
                    P.op("pe", lambda e, qn=qn, p0=p0, js=js, hp=hp, gb=gb: e.matmul(
                        banks[gb][:, js * 64:(js + 1) * 64], lhsT=qn[p0:p0 + 64, js * 128:(js + 1) * 128], rhs=kmT[hp][p0:p0 + 64, 0:64],
                        start=True, stop=True), reads=[Bqn, Bkm[hp]], writes=[Bb[gb]])
                for js in act_js:
                    m = ms[js]
                    P.op("dve", lambda e, r=r, js=js, m=m, gb=gb: e.tensor_copy(out=gp4[r][:, js, 0:m], in_=banks[gb][:, js * 64:js * 64 + m]),
                         reads=[Bb[gb]], writes=[Bgp4[r][js]])
                for js in act_js:
                    m = ms[js]
                    P.op("dve", lambda e, r=r, js=js, m=m: e.max(out=top8[r][:, js, :], in_=gp4[r][:, js, 0:max(m, 8)]),
                         reads=[Bgp4[r][js]], writes=[Btop[r][js]])
                for js in act_js:
                    m = ms[js]
                    P.op("dve", lambda e, r=r, js=js, m=m: e.tensor_scalar(out=mb4[r][:, js, 0:m], in0=gp4[r][:, js, 0:m],
                                                                           scalar1=top8[r][:, js, 2:3], scalar2=NEG,
                                                                           op0=ALU.is_lt, op1=ALU.mult),
                         reads=[Bgp4[r][js], Btop[r][js]], writes=[Bmb4[r][js]])
            for js in range(4):
                m = ms[js]
                lo = 0 if m < 3 else m
                P.op("dve", lambda e, r=r, js=js, lo=lo, m=m: e.memset(mb4[r][:, js, lo:m + 1], 0.0), reads=[Bmb4[r][js]], writes=[Bmb4[r][js]])
            P.op("dve", lambda e, r=r, h=h: e.tensor_copy(out=mb4[r][:, :, 63:64], in_=shc[:, h * 4:(h + 1) * 4].unsqueeze(2)),
                 reads=Bmb4[r] + [Bshc], writes=Bmb4[r])
            P.op("dve", lambda e, r=r: e.tensor_copy(out=Mbb4[r][:, :, 64:128], in_=mb4[r][:, :, :]), reads=Bmb4[r], writes=[BMbb4[r]])
            for js in range(4):
                P.op("pe", lambda e, r=r, js=js, tb=tb: e.matmul(banks[tb][:, js * 128:(js + 1) * 128], lhsT=Mbb4[r][:, js, :], rhs=ident[:, :],
                                                                 start=True, stop=True), reads=[BMbb4[r], Bid], writes=[Bb[tb]])
            P.op("act", lambda e, qs_=qs_, tb=tb: e.activation(out=QAt[qs_][64:128, :], in_=banks[tb][64:128, :], func=AF.Identity),
                 reads=[Bb[tb]], writes=[BQAt[qs_]])
            P.dma("pool", lambda e, h=h, qs_=qs_, i=i: e.dma_start(out=QA[h, :, i * T:(i + 1) * T], in_=QAt[qs_][:, :]),
                  reads=[BQAt[qs_]], writes=[BQA[h]], key=BQAt[qs_])
        for js in range(4):
            kt = 4 * i + js
            vb_ = js % 2
            for kc in range(NCH):
                P.op("pe", lambda e, kc=kc, js=js, s=s, vb_=vb_: e.matmul(
                    banks[vb_][:, 0:256], lhsT=ht[s][:, kc, js * 128:(js + 1) * 128], rhs=Wv[:, kc * 256:(kc + 1) * 256],
                    start=(kc == 0), stop=(kc == NCH - 1)), reads=[Bht[s], BWv], writes=[Bb[vb_]])
            P.op("act", lambda e, kt=kt, vb_=vb_: e.activation(out=Vall[:, kt, :, 0:64],
                                                              in_=banks[vb_][:, 0:256].rearrange("p (h d) -> p h d", h=HG),
                                                              func=AF.Identity), reads=[Bb[vb_], Bvones], writes=[BV[kt]])

    LA = 3
    NPT = 6
    SBK = [0, 1, 3, 4]
    qa = [k.sb("qa%d" % i, [128, T], BF16) for i in range(2)]
    Bqa = k.bufs("qa_", 2)
    pT = [k.sb("pT%d" % i, [128, T], BF16) for i in range(NPT)]
    BpT = k.bufs("pT_", NPT)
    rc = k.sb("rc", [128, T], F32)
    Brc = k.buf("rc")
    bc = k.sb("bc", [64, T], F32)
    Bbc = k.buf("bc")
    aot = [k.sb("aot%d" % i, [64, T], BF16) for i in range(2)]
    Baot = k.bufs("aot_", 2)
    st = {"fin": 0}
    for h in range(io.get("nheads2", HG)):
        P.dma("sp", lambda e, h=h: e.dma_start(out=Kaug[0:64, :], in_=KA[h, :, :]), reads=[BKA[h]], writes=[BKlo], key=BKlo)
        units = [(qs, kt) for qs in range(NQS) for kt in range(4 * qs + 4)]
        NU = len(units)
        pend = []

        def emit_qk(u, h=h):
            qs, kt = units[u]
            sl = qs % 2
            if kt == 0:
                P.dma("sp", lambda e: e.dma_start(out=qa[sl][:, :], in_=QA[h, :, qs * T:(qs + 1) * T]),
                      reads=[BQA[h]], writes=[Bqa[sl]], key=Bqa[sl])
            sb_ = SBK[u % 4]
            pt_ = u % NPT
            dk = kt - 4 * qs
            P.op("pe", lambda e: e.matmul(banks[sb_][:, :], lhsT=Kaug[:, kt * 128:(kt + 1) * 128], rhs=qa[sl][:, :],
                                          start=True, stop=(dk < 0)),
                 reads=[BKlo, BKhi, Bqa[sl]], writes=[Bb[sb_]])
            if dk >= 0:
                P.op("pe", lambda e: e.matmul(banks[sb_][:, :], lhsT=ident[:, :], rhs=cmask[:, dk, :], start=False, stop=True),
                     reads=[Bid, Bcm], writes=[Bb[sb_]])
            ri = dk + 124
            P.op("act", lambda e: e.activation(out=pT[pt_][:, :], in_=banks[sb_][:, :], func=AF.Exp,
                                               bias=AB[:, h * 128 + ri:h * 128 + ri + 1], scale=0.125),
                 reads=[Bb[sb_], BAB], writes=[BpT[pt_]])

        def emit_fin(qs, h=h):
            ob = 6 + qs % 2
            ar = st["fin"] % 2
            st["fin"] += 1
            P.op("dve", lambda e: e.reciprocal(out=rc[64:65, :], in_=banks[ob][64:65, :]), reads=[Bb[ob]], writes=[Brc])
            P.op("pe", lambda e: e.matmul(banks[2][0:64, :], lhsT=onesf[64:65, 0:64], rhs=rc[64:65, :], start=True, stop=True),
                 reads=[Bof, Brc], writes=[Bb[2]])
            P.op("act", lambda e: e.activation(out=bc[:, :], in_=banks[2][0:64, :], func=AF.Identity), reads=[Bb[2]], writes=[Bbc])
            P.op("dve", lambda e: e.tensor_tensor(out=aot[ar][:, :], in0=banks[ob][0:64, :], in1=bc[:, :], op=ALU.mult),
                 reads=[Bb[ob], Bbc], writes=[Baot[ar]])
            P.dma("pool", lambda e: e.dma_start(out=ao_dst(h, qs), in_=aot[ar][:, :]), reads=[Baot[ar]], key=Baot[ar])

        def emit_pv(u, step, h=h):
            qs, kt = units[u]
            ob = 6 + qs % 2
            pt_ = u % NPT
            nk = 4 * qs + 4
            P.op("pe", lambda e: e.matmul(banks[ob][0:65, :], lhsT=Vall[:, kt, h, :], rhs=pT[pt_][:, :],
                                          start=(kt == 0), stop=(kt == nk - 1)),
                 reads=[BV[kt], Bvones, BpT[pt_]], writes=[Bb[ob]])
            if kt == nk - 1:
                pend.append((step + 2, qs))

        for step in range(NU + LA):
            if step < NU:
                emit_qk(step)
            if step >= LA:
                emit_pv(step - LA, step)
            while pend and pend[0][0] <= step:
                emit_fin(pend.pop(0)[1])
        while pend:
            emit_fin(pend.pop(0)[1])


def _bf16():
    import ml_dtypes
    return ml_dtypes.bfloat16


def a2_consts(g):
    slopes = (2.0 ** (-8.0 * (np.arange(16) + 1) / 16)).astype(np.float32)
    p = np.arange(128, dtype=np.float32)[:, None]
    ri = np.arange(128, dtype=np.float32)[None, :]
    AB = np.zeros((128, HG * 128), np.float32)
    shc = np.zeros((128, 16), np.float32)
    for hl in range(HG):
        sl = slopes[4 * g + hl]
        AB[:, hl * 128:(hl + 1) * 128] = sl * (p + 128.0 * (ri - 124.0))
        for js in range(4):
            shc[:, hl * 4 + js] = -8.0 * sl * (js * 128.0 + p[:, 0])
    return AB, shc


def a2_static():
    kc = np.zeros((64, S), np.float32)
    for n in range(63):
        kc[n, n * 256:(n + 1) * 256] = 1.0
    kc[63, :] = 1.0
    cm = np.zeros((4, 128, T), np.float32)
    col = np.arange(T)[None, :]
    for dk in range(4):
        kp = dk * 128 + np.arange(128)[:, None]
        kb_, qb_ = kp // 256, col // 256
        cm[dk] = np.where(((kb_ == qb_) & (col < kp)) | (kb_ > qb_), NEG, 0.0)
    bd = np.zeros((128, 128), np.float32)
    bd[:64, :64] = 1.0
    bd[64:, 64:] = 1.0
    return kc.astype(_bf16()), cm, bd


def run_a1(xTs, c, ada_w_i, ada_b_i, ng_i):
    nc = get_prog("a1", build_a1)
    aT = ada_rows(ada_w_i, [0, 1])
    ab = ada_cols(ada_b_i, [0, 1])
    ngp = pvec(ng_i)
    in_maps = []
    for core in range(8):
        b, _ = core_tokens(core)
        in_maps.append({"xT": xTs[core], "cb": np.ascontiguousarray(np.broadcast_to(c[b], (128, D))),
                        "adaT": aT, "adab": ab, "ng": ngp})
    res = run_bass_kernel_spmd(nc, in_maps, core_ids=list(range(8)))
    return [r["ho"] for r in res.results]


def run_a2(hTas, wqkv, gqn, gkn):
    nc = get_prog("a2", build_a2)
    kc, cm, bd = a2_static()
    ident = np.eye(128, dtype=np.float32)
    in_maps = []
    for core in range(8):
        b, g = core // 4, core % 4
        Wq = wqkv[:, g * 256:(g + 1) * 256]
        Wk = wqkv[:, D + g * 256:D + (g + 1) * 256]
        Wv = wqkv[:, 2 * D + g * 256:2 * D + (g + 1) * 256]
        wqk = tileA(np.concatenate([Wq, Wk], axis=1))[0]
        wv = np.ascontiguousarray(Wv.reshape(8, 128, 256).transpose(1, 0, 2).reshape(128, 2048))
        gqk = np.ascontiguousarray(np.stack([np.tile(gqn, 2), np.tile(gkn, 2)], axis=1).astype(np.float32))
        AB, shc = a2_consts(g)
        in_maps.append({"hTa": hTas[b], "wqk": wqk, "wv": wv, "gqk": gqk, "AB": AB, "shc": shc, "kconst": kc,
                        "cmask": cm, "ident": ident, "bdones": bd})
    res = run_bass_kernel_spmd(nc, in_maps, core_ids=list(range(8)))
    return [r["ao"] for r in res.results]


def run_a3(xTs, aTs, c, ada_w_i, ada_b_i, wo_i):
    nc = get_prog("a3", build_a3)
    aT = ada_rows(ada_w_i, [2])
    ab = ada_cols(ada_b_i, [2])
    wot = tileA(wo_i)
    in_maps = []
    for core in range(8):
        b, _ = core_tokens(core)
        in_maps.append({"xT": xTs[core], "aT": aTs[core], "cb": np.ascontiguousarray(np.broadcast_to(c[b], (128, D))),
                        "adaT": aT, "adab": ab, "wo": wot})
    res = run_bass_kernel_spmd(nc, in_maps, core_ids=list(range(8)))
    return [r["xo"] for r in res.results]


def build_fused(upto=4):
    k = KB(fused=True)
    nc, P = k.nc, k.P
    x0 = k.din("xT", [D, TOK])
    xh0 = k.din("xh", [D, HALO])
    hv = k.din("hv", [128, 1])
    cb = k.din("cb", [128, D])
    identd = k.din("ident", [128, 128])
    ABd = k.din("AB", [128, HG * 128])
    shcd = k.din("shc", [128, 16])
    kconst = k.din("kconst", [64, S], BF16)
    cmaskd = k.din("cmask", [4, 128, T])
    bdd = k.din("bdones", [128, 128])
    adaT = [k.din("adaT%d" % i, [48, 128, D]) for i in range(4)]
    adab = [k.din("adab%d" % i, [128, 48]) for i in range(4)]
    ngm = [k.din("ngm%d" % i, [128, NCH]) for i in range(4)]
    w1 = [k.din("w1_%d" % i, [8, 128, 4096]) for i in range(4)]
    w2 = [k.din("w2_%d" % i, [8, 128, 4096]) for i in range(4)]
    vec = [k.din("vec%d" % j, [128, NVC]) for j in range(2)]
    win = [k.din("win%d" % j, [4, 128, 4096]) for j in range(2)]
    wout = [k.din("wout%d" % j, [2, 128, 4096]) for j in range(2)]
    nga = [k.din("nga%d" % j, [128, NCH]) for j in range(2)]
    wqk = [k.din("wqk%d" % j, [128, 4096]) for j in range(2)]
    wv = [k.din("wv%d" % j, [128, NCH * 256]) for j in range(2)]
    gqk = [k.din("gqk%d" % j, [128, 2]) for j in range(2)]
    wo = [k.din("wo%d" % j, [2, 128, 4096]) for j in range(2)]
    xo = k.dout("xo", [D, TOK])
    xa = k.dint("xa", [D, TOK])
    xb = k.dint("xb", [D, TOK])
    hown = k.dint("hown", [D, TOK], BF16)
    hG = k.dint("hG", [4 * D, TOK], BF16)
    aown = k.dint("aown", [4 * 256, TOK], BF16)
    aoG = k.dint("aoG", [4 * D, TOK], BF16)
    amine = k.dint("amine", [D, TOK], BF16)
    hl = k.dint("hl", [D, HALO])
    hlG = k.dint("hlG", [8 * D, HALO])
    QA = k.dint("QA", [HG, 128, S], BF16)
    KA = k.dint("KA", [HG, 64, S], BF16)
    npass = [0]
    dyn = {}

    def newpass(name):
        P.barrier()
        k.begin_pass("p%d%s_" % (npass[0], name))
        npass[0] += 1

    def mlp(i, xin, xout):
        newpass("mlp")
        emit_mlp(k, {"xT": xin, "xo": xout, "cb": cb, "adaT": adaT[i][24:48], "adab": adab[i][:, 24:48], "ng": ngm[i],
                     "w1": w1[i], "w2": w2[i]})

    def conv(i, j, xin, xout, xh_fn):
        newpass("conv")
        emit_conv(k, {"xT": xin, "xh_fn": xh_fn, "hv": hv, "xo": xout, "cb": cb, "adaT": adaT[i][0:24], "adab": adab[i][:, 0:24],
                      "vec": vec[j], "ident": identd, "win": win[j], "wout": wout[j]})

    def attn(i, j, xin, xout):
        newpass("a1")
        emit_a1(k, {"xT": xin, "ho": hown, "cb": cb, "adaT": adaT[i][0:16], "adab": adab[i][:, 0:16], "ng": nga[j]})
        newpass("ag1")
        for c_ in range(NCH):
            Bg = k.buf("hG%d" % c_)
            P.dma("pool", lambda e, c_=c_: e.collective_compute(
                "AllGather", ALU.bypass, replica_groups=[[0, 1, 2, 3], [4, 5, 6, 7]],
                ins=[hown[c_ * 128:(c_ + 1) * 128, :]], outs=[hG[c_ * 512:(c_ + 1) * 512, :]]), writes=[Bg], key=Bg, inc=1)
        newpass("a2")

        hGv = hG.rearrange("(c r p) t -> r p c t", c=NCH, r=4, p=128)

        def h_tile(ti):
            r, cc = ti // NT, ti % NT
            return hGv[r][:, :, cc * T:(cc + 1) * T]

        def ao_dst(h, qs):
            q, cc = qs // NT, qs % NT
            return aown[q * 256 + h * 64:q * 256 + (h + 1) * 64, cc * T:(cc + 1) * T]

        emit_a2(k, {"h_tile": h_tile, "ao_dst": ao_dst, "wqk": wqk[j], "wv": wv[j], "gqk": gqk[j], "AB": ABd, "shc": shcd,
                    "kconst": kconst, "cmask": cmaskd, "ident": identd, "bdones": bdd, "QA": QA, "KA": KA})
        newpass("ag2")
        for m_ in range(8):
            Bg2 = k.buf("aoG%d" % m_)
            P.dma("pool", lambda e, m_=m_: e.collective_compute(
                "AllGather", ALU.bypass, replica_groups=[[0, 1, 2, 3], [4, 5, 6, 7]],
                ins=[aown[m_ * 128:(m_ + 1) * 128, :]], outs=[aoG[m_ * 512:(m_ + 1) * 512, :]]), writes=[Bg2], key=Bg2, inc=1)
        newpass("a3")

        Bam = k.buf("amine")
        for g_ in range(4):
            for fh in range(2):
                def fn(e, g_=g_, fh=fh):
                    if "q1024" not in dyn:
                        dyn["q1024"] = e.snap((e.partition_id() % 4) * 1024)
                    base = fh * 512 + g_ * 128
                    return e.dma_start(out=amine[(g_ * 2 + fh) * 128:(g_ * 2 + fh + 1) * 128, :],
                                       in_=aoG[base:base + 3200, :][bass.ds(dyn["q1024"], 128), :])
                P.dma("sp", fn, writes=[Bam], key=Bam)
        amv = amine.rearrange("(c p) t -> p c t", p=128)

        def a_load(P_, dst, Bd, ti):
            P_.dma("sp", lambda e: e.dma_start(out=dst[:, :, :], in_=amv[:, :, ti * T:(ti + 1) * T]), reads=[Bam], writes=Bd, key=Bd[0])

        emit_a3(k, {"xT": xin, "a_load": a_load, "xo": xout, "cb": cb, "adaT": adaT[i][16:24], "adab": adab[i][:, 16:24], "wo": wo[j]})

    conv(0, 0, x0, xa, lambda e: xh0.rearrange("(c p) t -> p c t", p=128))
    mlp(0, xa, xb if upto > 1 else xo)
    if upto == 1:
        n = P.finalize()
        return nc, n
    attn(1, 0, xb, xa)
    mlp(1, xa, xb if upto > 2 else xo)
    if upto == 2:
        n = P.finalize()
        return nc, n
    newpass("halo")
    Bhl = k.buf("hl")
    BhlG = k.buf("hlG")
    P.dma("sp", lambda e: e.dma_start(out=hl[:, :], in_=xb[:, TOK - HALO:TOK]), writes=[Bhl], key=Bhl)
    P.dma("pool", lambda e: e.collective_compute("AllGather", ALU.bypass, replica_groups=[list(range(8))],
                                                  ins=[hl[:, :]], outs=[hlG[:, :]]), reads=[Bhl], writes=[BhlG], key=BhlG, inc=1)
    def xh2(e):
        if "prev" not in dyn:
            dyn["prev"] = e.snap(((e.partition_id() + 7) % 8) * D)
        return hlG[bass.ds(dyn["prev"], D), :].rearrange("(c p) t -> p c t", p=128)

    conv(2, 1, xb, xa, xh2)
    mlp(2, xa, xb)
    attn(3, 1, xb, xa)
    mlp(3, xa, xo)
    n = P.finalize()
    return nc, n


def kernel(x, c, ada_w, ada_b, mix_norm_g, mlp_norm_g,
           conv_w_in, conv_b_in, conv_dw, conv_dw_b, conv_ln_g, conv_ln_b, conv_w_out, conv_b_out,
           attn_w_qkv, attn_q_norm_g, attn_k_norm_g, attn_w_o, mlp_w1, mlp_w2):
    f = lambda a: np.asarray(a, dtype=np.float32)
    x, c, ada_w, ada_b = f(x), f(c), f(ada_w), f(ada_b)
    z = {"conv_w_in": f(conv_w_in), "conv_b_in": f(conv_b_in), "conv_dw": f(conv_dw), "conv_dw_b": f(conv_dw_b),
         "conv_ln_g": f(conv_ln_g), "conv_ln_b": f(conv_ln_b), "conv_w_out": f(conv_w_out), "conv_b_out": f(conv_b_out)}
    mix_norm_g, mlp_norm_g = f(mix_norm_g), f(mlp_norm_g)
    attn_w_qkv, attn_w_o = f(attn_w_qkv), f(attn_w_o)
    gqn, gkn = f(attn_q_norm_g), f(attn_k_norm_g)
    mlp_w1, mlp_w2 = f(mlp_w1), f(mlp_w2)
    nc = get_prog("fused%d" % UPTO, lambda: build_fused(UPTO))
    kc, cm, bd = a2_static()
    ident = np.eye(128, dtype=np.float32)
    shared = {"ident": ident, "kconst": kc, "cmask": cm, "bdones": bd}
    for i in range(4):
        shared["adaT%d" % i] = ada_rows(ada_w[i], [0, 1, 2, 3, 4, 5])
        shared["adab%d" % i] = ada_cols(ada_b[i], [0, 1, 2, 3, 4, 5])
        shared["ngm%d" % i] = pvec(mlp_norm_g[i])
        shared["w1_%d" % i] = tileA(mlp_w1[i])
        shared["w2_%d" % i] = tileB(mlp_w2[i])
    for j in range(2):
        zz = dict(z)
        zz["mix_norm_g_i"] = mix_norm_g[2 * j]
        shared["vec%d" % j] = conv_vec(zz, j)
        shared["win%d" % j] = tileA(z["conv_w_in"][j])
        shared["wout%d" % j] = tileA(z["conv_w_out"][j])
        shared["nga%d" % j] = pvec(mix_norm_g[2 * j + 1])
        shared["gqk%d" % j] = np.ascontiguousarray(np.stack([np.tile(gqn[j], 2), np.tile(gkn[j], 2)], axis=1).astype(np.float32))
        shared["wo%d" % j] = tileA(attn_w_o[j])
    per_g = []
    for g in range(4):
        d_ = {}
        AB, shc = a2_consts(g)
        d_["AB"], d_["shc"] = AB, shc
        for j in range(2):
            W = attn_w_qkv[j]
            Wq = W[:, g * 256:(g + 1) * 256]
            Wk = W[:, D + g * 256:D + (g + 1) * 256]
            Wv = W[:, 2 * D + g * 256:2 * D + (g + 1) * 256]
            d_["wqk%d" % j] = tileA(np.concatenate([Wq, Wk], axis=1))[0]
            d_["wv%d" % j] = np.ascontiguousarray(Wv.reshape(8, 128, 256).transpose(1, 0, 2).reshape(128, 2048))
        per_g.append(d_)
    in_maps = []
    for core in range(8):
        b, q = core // 4, core % 4
        m = dict(shared)
        m.update(per_g[q])
        m["xT"] = np.ascontiguousarray(x[b, q * TOK:(q + 1) * TOK].T)
        m["xh"] = np.ascontiguousarray(x[b, q * TOK - HALO:q * TOK].T) if q > 0 else np.zeros((D, HALO), np.float32)
        m["hv"] = np.full((128, 1), 0.0 if q == 0 else 1.0, np.float32)
        m["cb"] = np.ascontiguousarray(np.broadcast_to(c[b], (128, D)))
        in_maps.append(m)
    res = run_bass_kernel_spmd(nc, in_maps, core_ids=list(range(8)))
    out = np.empty((NB, S, D), np.float32)
    for core in range(8):
        b, q = core // 4, core % 4
        out[b, q * TOK:(q + 1) * TOK] = res.results[core]["xo"].T
    return out


def kernel_unfused(x, c, ada_w, ada_b, mix_norm_g, mlp_norm_g,
           conv_w_in, conv_b_in, conv_dw, conv_dw_b, conv_ln_g, conv_ln_b, conv_w_out, conv_b_out,
           attn_w_qkv, attn_q_norm_g, attn_k_norm_g, attn_w_o, mlp_w1, mlp_w2):
    f = lambda a: np.asarray(a, dtype=np.float32)
    x, c, ada_w, ada_b = f(x), f(c), f(ada_w), f(ada_b)
    z = {"conv_w_in": f(conv_w_in), "conv_b_in": f(conv_b_in), "conv_dw": f(conv_dw), "conv_dw_b": f(conv_dw_b),
         "conv_ln_g": f(conv_ln_g), "conv_ln_b": f(conv_ln_b), "conv_w_out": f(conv_w_out), "conv_b_out": f(conv_b_out)}
    mix_norm_g, mlp_norm_g = f(mix_norm_g), f(mlp_norm_g)
    attn_w_qkv, attn_w_o = f(attn_w_qkv), f(attn_w_o)
    gqn, gkn = f(attn_q_norm_g), f(attn_k_norm_g)
    mlp_w1, mlp_w2 = f(mlp_w1), f(mlp_w2)
    xTs = [np.ascontiguousarray(x[core // 4, (core % 4) * TOK:(core % 4 + 1) * TOK].T) for core in range(8)]
    for i in range(4):
        j = i // 2
        if i % 2 == 0:
            halos = []
            for core in range(8):
                if core % 4 == 0:
                    halos.append(np.zeros((D, HALO), np.float32))
                else:
                    halos.append(np.ascontiguousarray(xTs[core - 1][:, TOK - HALO:]))
            xTs = run_conv(xTs, halos, c, ada_w[i], ada_b[i], mix_norm_g[i], z, j)
        else:
            hos = run_a1(xTs, c, ada_w[i], ada_b[i], mix_norm_g[i])
            hTas = [np.ascontiguousarray(np.concatenate(hos[4 * b:4 * b + 4], axis=1)) for b in range(NB)]
            aos = run_a2(hTas, attn_w_qkv[j], gqn[j], gkn[j])
            aTs = []
            for core in range(8):
                b, q = core // 4, core % 4
                aTs.append(np.ascontiguousarray(np.concatenate([aos[4 * b + g][:, q * TOK:(q + 1) * TOK] for g in range(4)], axis=0)))
            xTs = run_a3(xTs, aTs, c, ada_w[i], ada_b[i], attn_w_o[j])
        xTs = run_mlp(xTs, c, ada_w[i], ada_b[i], mlp_norm_g[i], mlp_w1[i], mlp_w2[i])
    out = np.empty((NB, S, D), np.float32)
    for core in range(8):
        b, q = core // 4, core % 4
        out[b, q * TOK:(q + 1) * TOK] = xTs[core].T
    return out
```
